# Optimizing a Trainium2 kernel written in Bass

```python
import jax, jax.numpy as jnp
from jax import lax
import numpy as np

D_MODEL = 1024
BATCH = 32
SEQ = 256
DEPTH = 2
DEC_BATCH = 8
DEC_SEQ = 4096
PAST_LEN = 512

GRID_W = 64
N_RET_HEADS = 8
RET_DK = 128
RET_DV = 256
RET_QK_W = N_RET_HEADS * RET_DK
RET_V_W = N_RET_HEADS * RET_DV
RET_CHUNK = 128
N_FOURIER_GROUPS = 4
FOURIER_GROUP_W = 256
FOURIER_W = N_FOURIER_GROUPS * FOURIER_GROUP_W
IN_W = 2 * RET_QK_W + 2 * RET_V_W + FOURIER_W + 2 * D_MODEL
N_EXPERTS = 32
TOP_K = 4
D_EXPERT = 1024
SWIGLU_LIMIT = 7.0
SWIGLU_ALPHA = 1.702
MOE_BLOCK = 128
ROPE_BASE = 10000.0
NORM_EPS = 1e-6
GN_EPS = 1e-5

kernel_name = "hybrid_retention_fourier_moe_diffusion_step"

_SPLITS = list(np.cumsum([RET_QK_W, RET_QK_W, RET_V_W, RET_V_W, FOURIER_W, D_MODEL]))


def rms_norm(x, g):
    xf = x.astype(jnp.float32)
    y = xf * lax.rsqrt(jnp.mean(xf * xf, axis=-1, keepdims=True) + NORM_EPS)
    return (y * g.astype(jnp.float32)).astype(x.dtype)


def axial_rotary(x):
    n = x.shape[2]
    rows = n // GRID_W
    row = jnp.repeat(jnp.arange(rows), GRID_W).astype(jnp.float32)
    col = jnp.tile(jnp.arange(GRID_W), rows).astype(jnp.float32)
    n_freq = RET_DK // 4
    inv_freq = ROPE_BASE ** (-jnp.arange(n_freq, dtype=jnp.float32) / n_freq)
    ang = jnp.concatenate([row[:, None] * inv_freq, col[:, None] * inv_freq], axis=-1)
    cos, sin = jnp.cos(ang), jnp.sin(ang)
    x1, x2 = x[..., : RET_DK // 2], x[..., RET_DK // 2:]
    return jnp.concatenate([x1 * cos - x2 * sin, x1 * sin + x2 * cos], axis=-1)


def retention_scan(q, k, v, log_g, s0):
    b, h, n, _ = q.shape
    dv = v.shape[-1]
    nc = n // RET_CHUNK

    def chunks(t):
        return t.reshape(b, h, nc, RET_CHUNK, t.shape[-1]).transpose(2, 0, 1, 3, 4)

    i = jnp.arange(RET_CHUNK, dtype=jnp.float32)
    diff = i[:, None] - i[None, :]
    lg = log_g[:, None, None]
    intra = jnp.where(diff >= 0, jnp.exp(jnp.maximum(diff, 0.0) * lg), 0.0)
    q_dec = jnp.exp((i + 1.0)[:, None] * lg)
    k_dec = jnp.exp((RET_CHUNK - 1.0 - i)[:, None] * lg)
    c_dec = jnp.exp(RET_CHUNK * lg)

    def step(s, qkv):
        qc, kc, vc = qkv
        scores = jnp.einsum("bhid,bhjd->bhij", qc, kc) * intra
        o = (jnp.einsum("bhij,bhjv->bhiv", scores, vc)
             + jnp.einsum("bhid,bhdv->bhiv", qc * q_dec, s))
        s = s * c_dec + jnp.einsum("bhjd,bhjv->bhdv", kc * k_dec, vc)
        return s, o

    s_n, o = lax.scan(step, s0, (chunks(q), chunks(k), chunks(v)))
    o = o.transpose(1, 2, 0, 3, 4).reshape(b, h, n, dv)
    return o, s_n


def bidir_retention(q, k, v, log_g, s0_f, s0_b):
    o_f, s_f = retention_scan(q, k, v, log_g[0], s0_f)
    flip = lambda t: jnp.flip(t, axis=2)
    o_b, s_b = retention_scan(flip(q), flip(k), flip(v), log_g[1], s0_b)
    return o_f + flip(o_b), s_f, s_b


def moe(h, w_router, b_router, w1, b1, w2, b2):
    b, n, d = h.shape
    t = b * n
    xt = h.reshape(t, d)
    logits = (xt @ w_router + b_router).astype(jnp.float32)
    top_v, top_i = lax.top_k(logits, TOP_K)
    wts = jax.nn.softmax(top_v, axis=-1)
    m = t * TOP_K
    flat_e = top_i.reshape(m)
    order = jnp.argsort(flat_e)
    sorted_e = flat_e[order]
    counts = jnp.bincount(flat_e, length=N_EXPERTS)
    padded = ((counts + MOE_BLOCK - 1) // MOE_BLOCK) * MOE_BLOCK
    p_end = jnp.cumsum(padded)
    p_start = p_end - padded
    s_start = jnp.cumsum(counts) - counts
    ppos = p_start[sorted_e] + jnp.arange(m) - s_start[sorted_e]
    p_total = ((m + MOE_BLOCK - 1) // MOE_BLOCK) * MOE_BLOCK + N_EXPERTS * MOE_BLOCK
    n_blk = p_total // MOE_BLOCK
    xp = jnp.zeros((p_total, d), xt.dtype).at[ppos].set(xt[order // TOP_K])
    blk_e = jnp.minimum(jnp.searchsorted(p_end, jnp.arange(n_blk) * MOE_BLOCK, side="right"),
                        N_EXPERTS - 1)

    def expert_block(args):
        xb, e = args
        hh = xb @ w1[e] + b1[e]
        gate, lin = hh[:, :D_EXPERT], hh[:, D_EXPERT:]
        gate = jnp.minimum(gate, SWIGLU_LIMIT)
        lin = jnp.clip(lin, -SWIGLU_LIMIT, SWIGLU_LIMIT)
        act = (lin + 1.0) * (gate * jax.nn.sigmoid(SWIGLU_ALPHA * gate))
        return act @ w2[e] + b2[e]

    yb = lax.map(expert_block, (xp.reshape(n_blk, MOE_BLOCK, d), blk_e))
    y_sorted = yb.reshape(p_total, d)[ppos]
    y_flat = jnp.zeros((m, d), y_sorted.dtype).at[order].set(y_sorted)
    out = jnp.einsum("tk,tkd->td", wts.astype(y_flat.dtype), y_flat.reshape(t, TOP_K, d))
    return out.reshape(b, n, d)


def trunk_layer(x, mod, s0_f, s0_b, latent, norm1_g, norm2_g, w_in, decay_logit, w_ret_o,
                w_four, w_out, w_router, b_router, w1, b1, w2, b2):
    b, n, _ = x.shape
    shift1, scale1, gate1, shift2, scale2, gate2 = [mod[:, j][:, None, :] for j in range(6)]
    h = rms_norm(x, norm1_g) * (1.0 + scale1) + shift1
    u = h @ w_in
    q, k, v, g_ret, u_four, g_a, g_b = jnp.split(u, _SPLITS, axis=-1)

    q = q.reshape(b, n, N_RET_HEADS, RET_DK).transpose(0, 2, 1, 3).astype(jnp.float32) * (RET_DK ** -0.5)
    k = k.reshape(b, n, N_RET_HEADS, RET_DK).transpose(0, 2, 1, 3).astype(jnp.float32)
    v = v.reshape(b, n, N_RET_HEADS, RET_DV).transpose(0, 2, 1, 3).astype(jnp.float32)
    if latent:
        q, k = axial_rotary(q), axial_rotary(k)
    log_g = jax.nn.log_sigmoid(decay_logit.astype(jnp.float32))
    o, s_f, s_b = bidir_retention(q, k, v, log_g, s0_f.astype(jnp.float32), s0_b.astype(jnp.float32))
    mu = jnp.mean(o, axis=-1, keepdims=True)
    var = jnp.mean(jnp.square(o - mu), axis=-1, keepdims=True)
    o = (o - mu) * lax.rsqrt(var + GN_EPS)
    o = o.transpose(0, 2, 1, 3).reshape(b, n, RET_V_W).astype(x.dtype)
    branch_a = (jax.nn.silu(g_ret) * o) @ w_ret_o

    uf = u_four.astype(jnp.float32).reshape(b, n, N_FOURIER_GROUPS, FOURIER_GROUP_W)
    uf = jnp.fft.fft2(uf, axes=(1, 3), norm="ortho").real
    branch_b = uf.reshape(b, n, FOURIER_W).astype(x.dtype) @ w_four

    merged = jax.nn.sigmoid(g_a) * branch_a + jax.nn.sigmoid(g_b) * branch_b
    x = x + gate1 * (merged @ w_out)

    h2 = rms_norm(x, norm2_g) * (1.0 + scale2) + shift2
    x = x + gate2 * moe(h2, w_router, b_router, w1, b1, w2, b2)
    return x, s_f, s_b


def setup_inputs(seed: int = 0) -> dict:
    key = jax.random.key(seed)
    ks = jax.random.split(key, 24)
    f32 = jnp.float32
    nrm = lambda kk, shape, s: jax.random.normal(kk, shape, f32) * s
    base = np.log(2.0 ** (5.0 + np.arange(N_RET_HEADS)) - 1.0).astype(np.float32)
    return {
        "x_prompt": nrm(ks[0], (BATCH, SEQ, D_MODEL), 1.0),
        "x_sample": nrm(ks[1], (DEC_BATCH, DEC_SEQ, D_MODEL), 1.0),
        "state_ret": nrm(ks[2], (DEC_BATCH, DEPTH, 2, N_RET_HEADS, RET_DK, RET_DV), 1.0),
        "c": nrm(ks[3], (DEC_BATCH, D_MODEL), 1.0),
        "c_ctx": nrm(ks[4], (D_MODEL,), 1.0),
        "w_ada": nrm(ks[5], (DEPTH, D_MODEL, 6 * D_MODEL), 0.5 * D_MODEL ** -0.5),
        "b_ada": nrm(ks[6], (DEPTH, 6 * D_MODEL), 0.02),
        "norm1_g": 1.0 + nrm(ks[7], (DEPTH, D_MODEL), 0.02),
        "norm2_g": 1.0 + nrm(ks[8], (DEPTH, D_MODEL), 0.02),
        "w_in": nrm(ks[9], (DEPTH, D_MODEL, IN_W), D_MODEL ** -0.5),
        "ret_decay_logit": jnp.asarray(base)[None, None, :] + nrm(ks[10], (DEPTH, 2, N_RET_HEADS), 0.1),
        "w_ret_o": nrm(ks[11], (DEPTH, RET_V_W, D_MODEL), RET_V_W ** -0.5),
        "w_four": nrm(ks[12], (DEPTH, FOURIER_W, D_MODEL), FOURIER_W ** -0.5),
        "w_out": nrm(ks[13], (DEPTH, D_MODEL, D_MODEL), D_MODEL ** -0.5),
        "w_router": nrm(ks[14], (DEPTH, D_MODEL, N_EXPERTS), D_MODEL ** -0.5),
        "b_router": nrm(ks[15], (DEPTH, N_EXPERTS), 0.01),
        "w1": nrm(ks[16], (DEPTH, N_EXPERTS, D_MODEL, 2 * D_EXPERT), D_MODEL ** -0.5),
        "b1": nrm(ks[17], (DEPTH, N_EXPERTS, 2 * D_EXPERT), 0.02),
        "w2": nrm(ks[18], (DEPTH, N_EXPERTS, D_EXPERT, D_MODEL), D_EXPERT ** -0.5),
        "b2": nrm(ks[19], (DEPTH, N_EXPERTS, D_MODEL), 0.02),
        "final_g": 1.0 + nrm(ks[20], (D_MODEL,), 0.02),
    }


def reference(x_prompt, x_sample, state_ret, c, c_ctx, w_ada, b_ada, norm1_g, norm2_g, w_in,
              ret_decay_logit, w_ret_o, w_four, w_out, w_router, b_router, w1, b1, w2, b2, final_g):
    hp, hs = x_prompt, x_sample
    zero_state = jnp.zeros((x_prompt.shape[0], N_RET_HEADS, RET_DK, RET_DV), jnp.float32)
    layer_states = []
    for l in range(DEPTH):
        mod_ctx = (jax.nn.silu(c_ctx)[None, :] @ w_ada[l] + b_ada[l]).reshape(1, 6, D_MODEL)
        mod_lat = (jax.nn.silu(c) @ w_ada[l] + b_ada[l]).reshape(-1, 6, D_MODEL)
        lw = (norm1_g[l], norm2_g[l], w_in[l], ret_decay_logit[l], w_ret_o[l], w_four[l], w_out[l],
              w_router[l], b_router[l], w1[l], b1[l], w2[l], b2[l])
        hp, s_f, s_b = trunk_layer(hp, mod_ctx, zero_state, zero_state, False, *lw)
        layer_states.append(jnp.stack([s_f, s_b], axis=1))
        hs, _, _ = trunk_layer(hs, mod_lat, state_ret[:, l, 0], state_ret[:, l, 1], True, *lw)
    new_state_ret = jnp.stack(layer_states, axis=1).astype(x_prompt.dtype)
    y_prompt = rms_norm(hp, final_g)
    y_sample = rms_norm(hs, final_g)
    return (y_prompt, y_sample, new_state_ret)
```

```python
import os
from contextlib import ExitStack
import numpy as np
import ml_dtypes
import concourse.bass as bass
import concourse.mybir as mybir
from concourse.bass_utils import run_bass_kernel_spmd

F32 = mybir.dt.float32
BF16 = mybir.dt.bfloat16
I32 = mybir.dt.int32
U32 = mybir.dt.uint32
AF = mybir.ActivationFunctionType
ALU = mybir.AluOpType
AX = mybir.AxisListType

D = 1024
NTOK = 5120
NT = 40
NS = 4096
NH = 8
DK = 128
DV = 256
INW = 9216
NE = 32
DE = 1024
BLK = 512
NBLK = 72
PT = NBLK * BLK
EPS = 1e-6
GN_EPS = 1e-5
DEPTH = 2

C_ID, C_DF, C_TF, C_DB, C_TB, C_IP1, C_IB, C_LTRI, C_ONES = [i * 128 for i in range(9)]
C_KF = 9 * 128
C_KB = C_KF + 1
C_IOTA = C_KB + 1
C_BC = C_IOTA + 32
C_KCP = C_BC + NBLK
C_N = C_KCP + 8


def _consts():
    c = np.zeros((128, C_N), np.float32)
    p = np.arange(128)
    j = p[:, None].astype(np.float64)
    i = p[None, :].astype(np.float64)
    c[:, C_ID:C_ID + 128] = np.eye(128)
    c[:, C_DF:C_DF + 128] = np.maximum(i - j, 0)
    c[:, C_TF:C_TF + 128] = (i >= j)
    c[:, C_DB:C_DB + 128] = np.maximum(j - i, 0)
    c[:, C_TB:C_TB + 128] = (j >= i)
    c[:, C_IP1:C_IP1 + 128] = i + 1
    c[:, C_IB:C_IB + 128] = 128 - i
    c[:, C_LTRI:C_LTRI + 128] = (j < i)
    c[:, C_ONES:C_ONES + 128] = 1.0
    c[:, C_KF] = 127 - p
    c[:, C_KB] = p
    c[:, C_IOTA:C_IOTA + 32] = np.arange(32)[None, :]
    c[:, C_BC:C_BC + NBLK] = (np.arange(NBLK) * BLK)[None, :]
    c[:, C_KCP:C_KCP + 8] = np.arange(8)[None, :] * 128 + p[:, None]
    return c


def _rot_tables():
    t = np.arange(NS)
    row = (t // 64).astype(np.float32)
    col = (t % 64).astype(np.float32)
    nf = 32
    inv = (np.float32(10000.0) ** (-(np.arange(nf, dtype=np.float32)) / np.float32(nf))).astype(np.float32)
    ang = np.concatenate([row[:, None] * inv[None, :], col[:, None] * inv[None, :]], axis=1)
    ang = ang.astype(np.float64)
    cs = np.cos(ang).T
    sn = np.sin(ang).T
    tab = np.zeros((128, 2, NS), np.float32)
    tab[:64, 0] = cs
    tab[64:, 0] = cs
    tab[:64, 1] = sn
    tab[64:, 1] = sn
    return tab


def _dft_chan():
    c = np.arange(256)
    m = (c[:, None] * c[None, :]) % 256
    a = 2.0 * np.pi * m / 256.0
    cs = np.cos(a) / 16.0
    sn = -np.sin(a) / 16.0
    full = np.concatenate([cs, sn], axis=1)
    return full.reshape(2, 128, 512).transpose(1, 0, 2).astype(ml_dtypes.bfloat16)


def _dft_seq(n):
    nch = n // 128
    idx = np.arange(n, dtype=np.int64)
    m = (idx[:, None] * idx[None, :]) % n
    a = 2.0 * np.pi * m.astype(np.float64) / n
    sc = 1.0 / np.sqrt(n)
    out = np.zeros((nch, 128, 2, nch, 128), ml_dtypes.bfloat16)
    for cs, f in ((0, np.cos), (1, np.sin)):
        mat = (f(a) * sc).astype(np.float32)
        out[:, :, cs] = mat.reshape(nch, 128, nch, 128).transpose(2, 1, 0, 3).astype(ml_dtypes.bfloat16)
    return out


class Prog:
    CE = ("pe", "act", "dve", "pool")
    ALL = ("pe", "act", "dve", "pool", "sp")
    R = 10

    def __init__(self, nc, stack):
        self.nc = nc
        self.stack = stack
        self.ops = {e: [] for e in self.ALL}
        self.sems = {}
        self.phase = 0
        self.cnt = {e: 0 for e in self.CE}
        self.dcnt = {"sp": 0, "pool": 0}
        self.lastw = {}
        self.readers = {}
        self.waited = {e: {} for e in self.ALL}
        self.nsem = 0
        self.pool_init = None
        for q in self.dcnt:
            for s in range(self.R):
                self._mk(("d", q, s))
        self._new_phase()

    def _mk(self, key):
        self.sems[key] = self.stack.enter_context(self.nc.semaphore("s%d" % self.nsem))
        self.nsem += 1

    def _new_phase(self):
        self.phase += 1
        for e in self.CE:
            self.cnt[e] = 0
            self._mk(("c", e, self.phase))

    def _ck(self, e):
        return ("c", e, self.phase)

    def _deps(self, eng, reads, writes):
        deps = {}

        def add(ev):
            if ev is None:
                return
            k, v = ev
            if deps.get(k, 0) < v:
                deps[k] = v
        for t in reads:
            add(self.lastw.get(t))
        for t in writes:
            add(self.lastw.get(t))
            for k, v in self.readers.get(t, {}).items():
                add((k, v))
        waits = []
        for k, v in deps.items():
            if eng == "pe" and k == self._ck("pe"):
                continue
            if self.waited[eng].get(k, 0) >= v:
                continue
            self.waited[eng][k] = v
            waits.append((k, v))
        return waits

    def _post(self, ev, reads, writes):
        k, v = ev
        for t in reads:
            r = self.readers.setdefault(t, {})
            if r.get(k, 0) < v:
                r[k] = v
        for t in writes:
            self.lastw[t] = ev
            self.readers[t] = {}

    def op(self, eng, fn, reads=(), writes=()):
        waits = self._deps(eng, reads, writes)
        self.cnt[eng] += 1
        ev = (self._ck(eng), self.cnt[eng])
        self.ops[eng].append((waits, fn, ev[0], 1))
        self._post(ev, reads, writes)

    def dma(self, q, fn, reads=(), writes=()):
        waits = self._deps(q, reads, writes)
        j = self.dcnt[q]
        self.dcnt[q] += 1
        slot, gen = j % self.R, j // self.R
        key = ("d", q, slot)
        if gen > 0 and self.waited[q].get(key, 0) < 16 * gen:
            self.waited[q][key] = 16 * gen
            waits.append((key, 16 * gen))
        ev = (key, 16 * (gen + 1))
        self.ops[q].append((waits, fn, key, 16))
        self._post(ev, reads, writes)

    def barrier(self):
        evs = []
        for e in self.CE:
            if self.cnt[e] > 0:
                evs.append((self._ck(e), self.cnt[e]))
        for q, n in self.dcnt.items():
            for s in range(self.R):
                if n > s:
                    last = ((n - 1 - s) // self.R) + 1
                    evs.append((("d", q, s), 16 * last))
        for e in self.ALL:
            waits = []
            for k, v in evs:
                if self.waited[e].get(k, 0) >= v:
                    continue
                self.waited[e][k] = v
                waits.append((k, v))
            if waits:
                self.ops[e].append((waits, None, None, 0))
        self.lastw = {}
        self.readers = {}
        self._new_phase()

    def emit(self):
        nc = self.nc
        engs = {"pe": nc.tensor, "act": nc.scalar, "dve": nc.vector, "pool": nc.gpsimd, "sp": nc.sync}
        with nc.Block() as block:
            def run(name):
                eng = engs[name]
                for waits, fn, key, inc in self.ops[name]:
                    for k, v in waits:
                        eng.wait_ge(self.sems[k], v)
                    if fn is not None:
                        ins = fn()
                        ins.then_inc(self.sems[key], inc)

            @block.tensor
            def _(e):
                run("pe")

            @block.scalar
            def _(e):
                run("act")

            @block.vector
            def _(e):
                run("dve")

            @block.gpsimd
            def _(e):
                if self.pool_init is not None:
                    self.pool_init()
                run("pool")

            @block.sync
            def _(e):
                run("sp")


def build(debug=False, stop_after=None, dbg_names=()):
    nc = bass.Bass("TRN2", target_bir_lowering=False)
    stack = ExitStack()
    dbg_kind = "ExternalOutput" if debug else "Internal"

    def din(name, shape, dt=F32):
        return nc.dram_tensor(name, list(shape), dt, kind="ExternalInput").ap()

    def dscr(name, shape, dt, dbg=False):
        return nc.dram_tensor(name, list(shape), dt, kind=("ExternalOutput" if name in dbg_names else "Internal")).ap()

    x_in = din("x_in", [NTOK, D])
    s0_in = din("s0", [DEPTH, 2, NH, DK, DV])
    cT_in = din("cT", [128, 8, 2])
    w_ada = din("w_ada", [DEPTH, D, 6 * D])
    b_adaT = din("b_adaT", [DEPTH, 128, 48])
    g1T = din("g1T", [DEPTH, 128, 8])
    g2T = din("g2T", [DEPTH, 128, 8])
    w_in = din("w_in", [DEPTH, D, INW])
    decay = din("decay", [DEPTH, 16])
    w_ret_o = din("w_ret_o", [DEPTH, 2048, D])
    w_four = din("w_four", [DEPTH, D, D])
    w_out = din("w_out", [DEPTH, D, D])
    w_router = din("w_router", [DEPTH, D, NE])
    b_router = din("b_router", [DEPTH, NE])
    w1 = din("w1", [DEPTH, NE, D, 2 * DE])
    b1T = din("b1T", [DEPTH, NE, 128, 16])
    w2 = din("w2", [DEPTH, NE, DE, D])
    b2 = din("b2", [DEPTH, NE, D])
    final_g = din("final_g", [1, D])
    cst_in = din("cst", [128, C_N])
    rot_in = din("rot", [128, 2, NS])
    dftc_in = din("dftc", [128, 2, 512], BF16)
    dfts_in = din("dfts", [32, 128, 2, 32, 128], BF16)
    dftp_in = din("dftp", [2, 128, 2, 2, 128], BF16)

    y_out = nc.dram_tensor("y_out", [NTOK, D], F32, kind="ExternalOutput").ap()
    ns_out = nc.dram_tensor("ns_out", [4, DEPTH, 2, NH, DK, DV], F32, kind="ExternalOutput").ap()

    Xs = dscr("Xs", [NTOK, D], F32, True)
    QT = dscr("QT", [1024, NTOK], BF16, True)
    KT = dscr("KT", [1024, NTOK], BF16, True)
    Vd = dscr("Vd", [NTOK, 2048], BF16, True)
    GR = dscr("GR", [NTOK, 2048], BF16, True)
    XFT = dscr("XFT", [1024, NTOK], BF16, True)
    GAT = dscr("GAT", [1024, NTOK], BF16, True)
    GBT = dscr("GBT", [1024, NTOK], BF16, True)
    OGT = dscr("OGT", [2048, NTOK], BF16, True)
    UFT = dscr("UFT", [1024, NTOK], BF16, True)
    H2 = dscr("H2", [NTOK, D], BF16, True)
    XP = dscr("XP", [PT, D], BF16)
    Yd = dscr("Yd", [PT, D], F32)
    LG = dscr("LGd", [128, NT * 32], F32, True)
    SLd = dscr("SLd", [128, NT * 4], I32, True)

    ARENA = 48640
    arena = stack.enter_context(nc.sbuf_tensor("arena", [128, ARENA], F32))
    psum = stack.enter_context(nc.psum_tensor("ps", [128, 8, 512], F32))

    class Alloc:
        def __init__(self, base=0):
            self.off = base

        def get(self, shape, dt):
            n = int(np.prod(shape[1:]))
            esz = 2 if dt == BF16 else 4
            words = (n * esz + 3) // 4
            words = (words + 7) // 8 * 8
            a = arena[:, self.off:self.off + words]
            self.off += words
            assert self.off <= ARENA, ("SBUF overflow", self.off)
            if dt != F32:
                a = a.bitcast(dt)
            a = a[:, 0:n]
            if len(shape) == 3:
                a = a.rearrange("p (a b) -> p a b", a=shape[1])
            elif len(shape) == 4:
                a = a.rearrange("p (a b c) -> p a b c", a=shape[1], b=shape[2])
            return a

    def ps_f32(b, n=512):
        return psum[:, b, 0:n]

    def ps_bf(b):
        return psum[:, b, :].bitcast(BF16)

    P = Prog(nc, stack)
    REG = {}

    def _pool_init():
        REG["xp"] = nc.gpsimd.to_reg(PT - 1)
        REG["w"] = nc.gpsimd.to_reg(DEPTH * NE * 1024 - 1)
        REG["b1"] = nc.gpsimd.to_reg(DEPTH * NE * 128 - 1)
        REG["b2"] = nc.gpsimd.to_reg(DEPTH * NE - 1)
    P.pool_init = _pool_init
    V, A, G, T, SP = nc.vector, nc.scalar, nc.gpsimd, nc.tensor, nc.sync

    pa = Alloc(0)
    cst = pa.get([128, C_N], F32)
    identb = pa.get([128, 128], BF16)
    scT = pa.get([128, 8, 2], F32)
    modT = pa.get([128, 48, 2], F32)
    A1 = pa.get([128, 2, 8], F32)
    epsT = pa.get([128, 2], F32)
    PBASE = pa.off

    ident = cst[:, C_ID:C_ID + 128]
    ones = cst[:, C_ONES:C_ONES + 128]

    P.dma("sp", lambda: SP.dma_start(out=cst, in_=cst_in), writes=["cst"])
    P.dma("sp", lambda: SP.dma_start(out=scT, in_=cT_in), writes=["scT"])
    P.op("act", lambda: A.activation(out=scT, in_=scT, func=AF.Silu), reads=["scT"], writes=["scT"])
    P.op("dve", lambda: V.tensor_copy(out=identb, in_=ident), reads=["cst"], writes=["identb"])
    P.op("dve", lambda: V.memset(epsT[:, 0:1], EPS), writes=["eps0"])
    P.op("dve", lambda: V.memset(epsT[:, 1:2], GN_EPS), writes=["eps1"])
    P.barrier()
    if debug or stop_after:
        dbgo = nc.dram_tensor("dbgo", [128, 4096], F32, kind="ExternalOutput").ap()
    if stop_after == "init":
        P.dma("sp", lambda: SP.dma_start(out=dbgo[:, 0:C_N], in_=cst), writes=["dbgo"])
        P.dma("sp", lambda: SP.dma_start(out=dbgo[:, 2048:2064], in_=scT.rearrange("p a b -> p (a b)")), writes=["dbgo2"])
        P.barrier()
        P.emit()
        return nc, stack

    seqs = [(0, NS, 32, True, 0, None)] + [(NS + 256 * s, 256, 2, False, 1, s) for s in range(4)]

    def bcast_row(dst, vecT, tmpd, psb):
        for kc in range(8):
            P.op("dve", lambda kc=kc: V.tensor_scalar(out=tmpd, in0=ident, scalar1=vecT[:, kc:kc + 1],
                                                      scalar2=None, op0=ALU.mult),
                 reads=["vecsrc"], writes=["bc_tmp"])
            P.op("pe", lambda kc=kc: T.matmul(ps_f32(psb, 128), lhsT=ones, rhs=tmpd, start=True, stop=True),
                 reads=["bc_tmp"], writes=[("ps", psb)])
            P.op("act", lambda kc=kc: A.copy(out=dst[:, kc * 128:(kc + 1) * 128], in_=ps_f32(psb, 128)),
                 reads=[("ps", psb)], writes=["bc_dst"])

    def do_layer(l):
        Xsrc = x_in if l == 0 else Xs
        al = Alloc(PBASE)
        hT = al.get([128, 8, NTOK], BF16)
        xt = [al.get([128, D], F32) for _ in range(2)]
        xn = [al.get([128, D], F32) for _ in range(2)]
        junk = al.get([128, D], F32)
        ss = al.get([128, 4], F32)
        P1END = al.off
        wa = [al.get([128, 8, 512], F32) for _ in range(2)]
        bada = al.get([128, 48], F32)
        g1t = al.get([128, 8], F32)
        P.dma("sp", lambda: SP.dma_start(out=bada, in_=b_adaT[l]), writes=["bada"])
        P.dma("sp", lambda: SP.dma_start(out=g1t, in_=g1T[l]), writes=["g1t"])
        for nb in range(12):
            wb = wa[nb % 2]
            P.dma("sp", lambda nb=nb, wb=wb: SP.dma_start(
                out=wb, in_=w_ada[l].rearrange("(kc p) c -> p kc c", p=128)[:, :, nb * 512:(nb + 1) * 512]),
                writes=[("wa", nb % 2)])
            for sub in range(4):
                ch = nb * 4 + sub
                for kc in range(8):
                    P.op("pe", lambda kc=kc, sub=sub, ch=ch, wb=wb: T.matmul(
                        psum[:, 0, ch * 2:ch * 2 + 2], lhsT=wb[:, kc, sub * 128:(sub + 1) * 128],
                        rhs=scT[:, kc, :], start=(kc == 0), stop=(kc == 7)),
                        reads=[("wa", nb % 2)], writes=[("ps", 0)])
        P.op("dve", lambda: V.tensor_tensor(
            out=modT, in0=psum[:, 0, 0:96].rearrange("p (c j) -> p c j", j=2),
            in1=bada.unsqueeze(2).to_broadcast([128, 48, 2]), op=ALU.add),
            reads=[("ps", 0), "bada"], writes=["modT"])
        for j in range(2):
            P.op("dve", lambda j=j: V.scalar_tensor_tensor(
                out=A1[:, j, :], in0=modT[:, 8:16, j], scalar=1.0, in1=g1t, op0=ALU.add, op1=ALU.mult),
                reads=["modT", "g1t"], writes=["A1"])

        if stop_after == "p0":
            P.dma("sp", lambda: SP.dma_start(out=dbgo[:, 0:96], in_=modT.rearrange("p a b -> p (a b)")), reads=["modT"], writes=["dbgo"])
            P.dma("sp", lambda: SP.dma_start(out=dbgo[:, 128:144], in_=A1.rearrange("p a b -> p (a b)")), reads=["A1"], writes=["dbgo2"])
            P.barrier()
            return True
        for tt in range(NT):
            j = 0 if tt < 32 else 1
            xb, xnb = xt[tt % 2], xn[tt % 2]
            P.dma("sp", lambda tt=tt, xb=xb: SP.dma_start(out=xb, in_=Xsrc[tt * 128:(tt + 1) * 128, :]),
                  writes=[("xt", tt % 2)])
            P.op("act", lambda xb=xb: A.activation(out=junk, in_=xb, func=AF.Square, accum_out=ss[:, 0:1]),
                 reads=[("xt", tt % 2)], writes=["junk", "ss"])
            P.op("act", lambda: A.activation(out=ss[:, 1:2], in_=ss[:, 0:1], func=AF.Sqrt, scale=1.0 / D, bias=epsT[:, 0:1]),
                 reads=["ss"], writes=["ss1"])
            P.op("dve", lambda: V.reciprocal(out=ss[:, 2:3], in_=ss[:, 1:2]), reads=["ss1"], writes=["ss2"])
            P.op("dve", lambda xb=xb, xnb=xnb: V.tensor_scalar(out=xnb, in0=xb, scalar1=ss[:, 2:3], scalar2=None,
                                                               op0=ALU.mult),
                 reads=[("xt", tt % 2), "ss2"], writes=[("xn", tt % 2)])
            for half in range(2):
                bnk = 1 + half + 2 * (tt % 2)
                for q in range(4):
                    kc = half * 4 + q
                    P.op("pe", lambda kc=kc, q=q, bnk=bnk, xnb=xnb: T.matmul(
                        psum[:, bnk, q * 128:(q + 1) * 128], lhsT=xnb[:, kc * 128:(kc + 1) * 128], rhs=ident,
                        start=True, stop=True),
                        reads=[("xn", tt % 2)], writes=[("ps", bnk)])
                for q in range(4):
                    kc = half * 4 + q
                    P.op("act", lambda kc=kc, q=q, bnk=bnk, tt=tt, j=j: A.activation(
                        out=hT[:, kc, tt * 128:(tt + 1) * 128], in_=psum[:, bnk, q * 128:(q + 1) * 128],
                        func=AF.Identity, scale=A1[:, j, kc:kc + 1], bias=modT[:, kc, j:j + 1]),
                        reads=[("ps", bnk), "A1", "modT"], writes=[("hT", tt)])
        if stop_after == "p1":
            P.dma("sp", lambda: SP.dma_start(out=dbgo.bitcast(BF16)[:, 0:8 * 128].rearrange("p (k t) -> p k t", k=8), in_=hT[:, :, 0:128]), reads=[("hT", 0)], writes=["dbgo"])
            P.dma("sp", lambda: SP.dma_start(out=dbgo.bitcast(BF16)[:, 4096:4096 + 8 * 128].rearrange("p (k t) -> p k t", k=8), in_=hT[:, :, 4992:5120]), reads=[("hT", 39)], writes=["dbgo1"])
        P.barrier()
        if stop_after == "p1":
            return True

        al = Alloc(P1END)
        wbf = [al.get([128, 8, 512], BF16) for _ in range(2)]
        wrot = al.get([128, 8, 512], BF16)
        rot = al.get([128, 2, NS], F32)
        osb = [al.get([128, 4, 512], BF16) for _ in range(2)]
        t1 = [al.get([128, 512], F32) for _ in range(2)]
        t2 = [al.get([128, 512], F32) for _ in range(2)]
        P.dma("sp", lambda: SP.dma_start(out=rot, in_=rot_in), writes=["rot"])
        oi = 0
        for cb in range(18):
            wb = wbf[cb % 2]
            wtok = ("wbf", cb % 2)
            P.dma("pool", lambda cb=cb, wb=wb: G.dma_start(
                out=wb, in_=w_in[l].rearrange("(kc p) c -> p kc c", p=128)[:, :, cb * 512:(cb + 1) * 512]),
                writes=[wtok])
            kind = ("q", "q", "k", "k", "v", "v", "v", "v", "g", "g", "g", "g", "x", "x", "a", "a", "b", "b")[cb]
            if kind in ("q", "k"):
                wv = wb.rearrange("p k (h two f) -> p k h two f", two=2, f=64)
                rv = wrot.rearrange("p k (h two f) -> p k h two f", two=2, f=64)
                P.op("act", lambda wv=wv, rv=rv: A.mul(out=rv[:, :, :, 0, :], in_=wv[:, :, :, 1, :], mul=-1.0),
                     reads=[wtok], writes=["wrot"])
                P.op("dve", lambda wv=wv, rv=rv: V.tensor_copy(out=rv[:, :, :, 1, :], in_=wv[:, :, :, 0, :]),
                     reads=[wtok], writes=["wrot2"])
            for tg in range(10):
                ob = osb[oi % 2]
                otok = ("osb", oi % 2)
                oi += 1
                tsl = slice(tg * 512, (tg + 1) * 512)
                for sub in range(4):
                    bA = (sub % 2) * 2
                    bB = bA + 1
                    if kind in ("v", "g"):
                        tk = tg * 512 + sub * 128
                        for kc in range(8):
                            P.op("pe", lambda kc=kc, bA=bA, tk=tk, wb=wb: T.matmul(
                                ps_f32(bA), lhsT=hT[:, kc, tk:tk + 128], rhs=wb[:, kc, :],
                                start=(kc == 0), stop=(kc == 7)), reads=[wtok], writes=[("ps", bA)])
                        fn = AF.Copy if kind == "v" else AF.Silu
                        P.op("act", lambda bA=bA, ob=ob, sub=sub, fn=fn: A.activation(
                            out=ob[:, sub, :], in_=ps_f32(bA), func=fn),
                            reads=[("ps", bA)], writes=[otok])
                    else:
                        csl = slice(sub * 128, (sub + 1) * 128)
                        for kc in range(8):
                            P.op("pe", lambda kc=kc, bA=bA, wb=wb, csl=csl, tsl=tsl: T.matmul(
                                ps_f32(bA), lhsT=wb[:, kc, csl], rhs=hT[:, kc, tsl],
                                start=(kc == 0), stop=(kc == 7)), reads=[wtok], writes=[("ps", bA)])
                        sc = (DK ** -0.5) if kind == "q" else 1.0
                        if kind in ("q", "k") and tg < 8:
                            for kc in range(8):
                                P.op("pe", lambda kc=kc, bB=bB, csl=csl, tsl=tsl: T.matmul(
                                    ps_f32(bB), lhsT=wrot[:, kc, csl], rhs=hT[:, kc, tsl],
                                    start=(kc == 0), stop=(kc == 7)), reads=["wrot", "wrot2"], writes=[("ps", bB)])
                            ta, tb = t1[sub % 2], t2[sub % 2]
                            P.op("dve", lambda bA=bA, ta=ta, tsl=tsl, sc=sc: V.scalar_tensor_tensor(
                                out=ta, in0=ps_f32(bA), scalar=sc, in1=rot[:, 0, tsl], op0=ALU.mult, op1=ALU.mult),
                                reads=[("ps", bA), "rot"], writes=[("t1", sub % 2)])
                            P.op("dve", lambda bB=bB, tb=tb, tsl=tsl, sc=sc: V.scalar_tensor_tensor(
                                out=tb, in0=ps_f32(bB), scalar=sc, in1=rot[:, 1, tsl], op0=ALU.mult, op1=ALU.mult),
                                reads=[("ps", bB), "rot"], writes=[("t2", sub % 2)])
                            P.op("pool", lambda ta=ta, tb=tb, ob=ob, sub=sub: G.tensor_tensor(
                                out=ob[:, sub, :], in0=ta, in1=tb, op=ALU.add),
                                reads=[("t1", sub % 2), ("t2", sub % 2)], writes=[otok])
                        else:
                            if kind in ("a", "b"):
                                P.op("act", lambda bA=bA, ob=ob, sub=sub: A.activation(
                                    out=ob[:, sub, :], in_=ps_f32(bA), func=AF.Sigmoid),
                                    reads=[("ps", bA)], writes=[otok])
                            else:
                                P.op("act", lambda bA=bA, ob=ob, sub=sub, sc=sc: A.activation(
                                    out=ob[:, sub, :], in_=ps_f32(bA), func=AF.Copy, scale=sc),
                                    reads=[("ps", bA)], writes=[otok])
                if kind in ("v", "g"):
                    dst = (Vd if kind == "v" else GR)
                    c0 = (cb - (4 if kind == "v" else 8)) * 512
                    P.dma("sp", lambda dst=dst, c0=c0, tg=tg, ob=ob: SP.dma_start(
                        out=dst[tg * 512:(tg + 1) * 512, c0:c0 + 512].rearrange("(s p) c -> p s c", p=128), in_=ob),
                        reads=[otok], writes=[("u", kind)])
                else:
                    dst, cb0 = {"q": (QT, 0), "k": (KT, 2), "x": (XFT, 12), "a": (GAT, 14), "b": (GBT, 16)}[kind]
                    r0 = (cb - cb0) * 512
                    P.dma("sp", lambda dst=dst, r0=r0, tsl=tsl, ob=ob: SP.dma_start(
                        out=dst[r0:r0 + 512, tsl].rearrange("(s p) t -> p s t", p=128), in_=ob),
                        reads=[otok], writes=[("u", kind)])
        P.barrier()
        if stop_after == "p2a":
            return True

        al = Alloc(PBASE)
        dl = al.get([128, 16], F32)
        MC = al.get([128, 8, 128], F32)
        QDF = al.get([128, 8, 128], F32)
        QDB = al.get([128, 8, 128], F32)
        KD = al.get([128, 2, 8], F32)
        CDEC = al.get([128, 16], F32)
        mtmp = al.get([128, 2, 128], F32)
        QTh = al.get([128, NS], BF16)
        KTh = al.get([128, NS], BF16)
        Vh = al.get([128, 32, 256], BF16)
        GRh = al.get([128, 32, 256], BF16)
        Kf = al.get([128, 32, 128], BF16)
        Kb = al.get([128, 32, 128], BF16)
        QfT = al.get([128, 32, 128], BF16)
        QbT = al.get([128, 32, 128], BF16)
        SbAll = al.get([128, 33, 256], BF16)
        OGh = al.get([128, 2, NS], BF16)
        Sf32 = al.get([128, 256], F32)
        Sb32 = al.get([128, 256], F32)
        Sfbf = [al.get([128, 256], BF16) for _ in range(2)]
        scm = [al.get([128, 128], BF16) for _ in range(2)]
        onb = [al.get([128, 256], F32) for _ in range(2)]
        ogb = [al.get([128, 256], BF16) for _ in range(2)]
        st6 = al.get([128, 8], F32)
        mv = al.get([128, 4], F32)

        P.dma("sp", lambda: SP.dma_start(out=dl, in_=decay[l:l + 1, :].to_broadcast([128, 16])), writes=["dl"])
        P.op("act", lambda: A.activation(out=dl, in_=dl, func=AF.Exp, scale=-1.0), reads=["dl"], writes=["dl"])
        P.op("act", lambda: A.activation(out=dl, in_=dl, func=AF.Ln, bias=1.0), reads=["dl"], writes=["dl"])
        P.op("dve", lambda: V.tensor_scalar(out=dl, in0=dl, scalar1=-1.0, scalar2=None, op0=ALU.mult),
             reads=["dl"], writes=["dl"])
        P.op("act", lambda: A.activation(out=CDEC, in_=dl, func=AF.Exp, scale=128.0), reads=["dl"], writes=["CDEC"])
        for h in range(NH):
            lf = dl[:, h:h + 1]
            lb = dl[:, 8 + h:9 + h]
            P.op("act", lambda lf=lf: A.activation(out=mtmp[:, 0, :], in_=cst[:, C_DF:C_DF + 128], func=AF.Exp, scale=lf),
                 reads=["dl"], writes=["mtmp0"])
            P.op("act", lambda lb=lb: A.activation(out=mtmp[:, 1, :], in_=cst[:, C_DB:C_DB + 128], func=AF.Exp, scale=lb),
                 reads=["dl"], writes=["mtmp1"])
            P.op("dve", lambda: V.tensor_tensor(out=mtmp[:, 0, :], in0=mtmp[:, 0, :], in1=cst[:, C_TF:C_TF + 128], op=ALU.mult),
                 reads=["mtmp0"], writes=["mtmp0"])
            P.op("dve", lambda: V.tensor_tensor(out=mtmp[:, 1, :], in0=mtmp[:, 1, :], in1=cst[:, C_TB:C_TB + 128], op=ALU.mult),
                 reads=["mtmp1"], writes=["mtmp1"])
            P.op("dve", lambda h=h: V.tensor_tensor(out=MC[:, h, :], in0=mtmp[:, 0, :], in1=mtmp[:, 1, :], op=ALU.add),
                 reads=["mtmp0", "mtmp1"], writes=["MC"])
            P.op("act", lambda h=h, lf=lf: A.activation(out=QDF[:, h, :], in_=cst[:, C_IP1:C_IP1 + 128], func=AF.Exp, scale=lf),
                 reads=["dl"], writes=["QD"])
            P.op("act", lambda h=h, lb=lb: A.activation(out=QDB[:, h, :], in_=cst[:, C_IB:C_IB + 128], func=AF.Exp, scale=lb),
                 reads=["dl"], writes=["QD"])
            P.op("act", lambda h=h, lf=lf: A.activation(out=KD[:, 0, h:h + 1], in_=cst[:, C_KF:C_KF + 1], func=AF.Exp, scale=lf),
                 reads=["dl"], writes=["KD"])
            P.op("act", lambda h=h, lb=lb: A.activation(out=KD[:, 1, h:h + 1], in_=cst[:, C_KB:C_KB + 1], func=AF.Exp, scale=lb),
                 reads=["dl"], writes=["KD"])

        for (r0, N, nch, latent, jt, pidx) in seqs:
            for h in range(NH):
                P.dma("sp", lambda h=h, r0=r0, N=N: SP.dma_start(out=QTh[:, 0:N], in_=QT[h * 128:(h + 1) * 128, r0:r0 + N]),
                      writes=["QTh"])
                P.dma("sp", lambda h=h, r0=r0, N=N: SP.dma_start(out=KTh[:, 0:N], in_=KT[h * 128:(h + 1) * 128, r0:r0 + N]),
                      writes=["KTh"])
                P.dma("sp", lambda h=h, r0=r0, N=N, nch=nch: SP.dma_start(
                    out=Vh[:, 0:nch, :], in_=Vd[r0:r0 + N, h * 256:(h + 1) * 256].rearrange("(c p) v -> p c v", p=128)),
                    writes=["Vh"])
                P.dma("sp", lambda h=h, r0=r0, N=N, nch=nch: SP.dma_start(
                    out=GRh[:, 0:nch, :], in_=GR[r0:r0 + N, h * 256:(h + 1) * 256].rearrange("(c p) v -> p c v", p=128)),
                    writes=["GRh"])
                if latent:
                    P.dma("sp", lambda h=h: SP.dma_start(out=Sf32, in_=s0_in[l, 0, h]), writes=["Sf32"])
                    P.dma("sp", lambda h=h: SP.dma_start(out=Sb32, in_=s0_in[l, 1, h]), writes=["Sb32"])
                else:
                    P.op("pool", lambda: G.memset(Sf32, 0.0), writes=["Sf32"])
                    P.op("pool", lambda: G.memset(Sb32, 0.0), writes=["Sb32"])
                for c0 in range(0, nch, 8):
                    n8 = min(8, nch - c0)
                    bnk = 4 + (c0 // 8) % 2
                    for cc in range(n8):
                        c = c0 + cc
                        P.op("pe", lambda c=c, cc=cc, bnk=bnk: T.transpose(
                            ps_bf(bnk)[:, cc * 128:(cc + 1) * 128], KTh[:, c * 128:(c + 1) * 128], identb),
                            reads=["KTh"], writes=[("ps", bnk)])
                    P.op("dve", lambda c0=c0, n8=n8, bnk=bnk, h=h: V.tensor_scalar(
                        out=Kf[:, c0:c0 + n8, :], in0=ps_bf(bnk)[:, 0:n8 * 128].rearrange("p (c d) -> p c d", d=128),
                        scalar1=KD[:, 0, h:h + 1], scalar2=None, op0=ALU.mult),
                        reads=[("ps", bnk), "KD"], writes=["Kf"])
                    P.op("act", lambda c0=c0, n8=n8, bnk=bnk, h=h: A.activation(
                        out=Kb[:, c0:c0 + n8, :], in_=ps_bf(bnk)[:, 0:n8 * 128].rearrange("p (c d) -> p c d", d=128),
                        func=AF.Copy, scale=KD[:, 1, h:h + 1]),
                        reads=[("ps", bnk), "KD", "Kf"], writes=["Kb"])
                P.op("dve", lambda h=h, nch=nch, N=N: V.tensor_tensor(
                    out=QfT[:, 0:nch, :], in0=QTh[:, 0:N].rearrange("p (c i) -> p c i", i=128),
                    in1=QDF[:, h:h + 1, :].to_broadcast([128, nch, 128]), op=ALU.mult),
                    reads=["QTh", "QD"], writes=["QfT"])
                P.op("pool", lambda h=h, nch=nch, N=N: G.tensor_tensor(
                    out=QbT[:, 0:nch, :], in0=QTh[:, 0:N].rearrange("p (c i) -> p c i", i=128),
                    in1=QDB[:, h:h + 1, :].to_broadcast([128, nch, 128]), op=ALU.mult),
                    reads=["QTh", "QD"], writes=["QbT"])
                P.op("act", lambda nch=nch: A.copy(out=SbAll[:, nch, :], in_=Sb32), reads=["Sb32"], writes=[("SbAll", nch)])
                for c in range(nch - 1, -1, -1):
                    bnk = 6 + (c % 2)
                    P.op("pe", lambda c=c, bnk=bnk: T.matmul(ps_f32(bnk, 256), lhsT=Kb[:, c, :], rhs=Vh[:, c, :],
                                                             start=True, stop=True),
                         reads=["Kb", "Vh"], writes=[("ps", bnk)])
                    P.op("dve", lambda bnk=bnk, h=h: V.scalar_tensor_tensor(
                        out=Sb32, in0=Sb32, scalar=CDEC[:, 8 + h:9 + h], in1=ps_f32(bnk, 256), op0=ALU.mult, op1=ALU.add),
                        reads=[("ps", bnk), "Sb32", "CDEC"], writes=["Sb32"])
                    P.op("act", lambda c=c: A.copy(out=SbAll[:, c, :], in_=Sb32), reads=["Sb32"], writes=[("SbAll", c)])
                if not latent:
                    P.dma("sp", lambda h=h, pidx=pidx: SP.dma_start(out=ns_out[pidx, l, 1, h], in_=Sb32),
                          reads=["Sb32"], writes=[("ns", pidx, 1, h)])
                P.op("act", lambda: A.copy(out=Sfbf[0], in_=Sf32), reads=["Sf32"], writes=[("Sfbf", 0)])
                for c in range(nch):
                    csl = slice(c * 128, (c + 1) * 128)
                    b_sc = c % 2
                    b_o = 2 + c % 2
                    sm, onx, ogx = scm[c % 2], onb[c % 2], ogb[c % 2]
                    P.op("pe", lambda csl=csl, b_sc=b_sc: T.matmul(ps_f32(b_sc, 128), lhsT=KTh[:, csl], rhs=QTh[:, csl],
                                                                 start=True, stop=True),
                         reads=["KTh", "QTh"], writes=[("ps", b_sc)])
                    P.op("dve", lambda b_sc=b_sc, sm=sm, h=h: V.tensor_tensor(out=sm, in0=ps_f32(b_sc, 128), in1=MC[:, h, :],
                                                                              op=ALU.mult),
                         reads=[("ps", b_sc), "MC"], writes=[("scm", c % 2)])
                    P.op("pe", lambda c=c, b_o=b_o, sm=sm: T.matmul(ps_f32(b_o, 256), lhsT=sm, rhs=Vh[:, c, :],
                                                                    start=True, stop=False),
                         reads=[("scm", c % 2), "Vh"], writes=[("ps", b_o)])
                    P.op("pe", lambda c=c, b_o=b_o: T.matmul(ps_f32(b_o, 256), lhsT=QfT[:, c, :], rhs=Sfbf[c % 2],
                                                             start=False, stop=False),
                         reads=["QfT", ("Sfbf", c % 2)], writes=[("ps", b_o)])
                    P.op("pe", lambda c=c, b_o=b_o: T.matmul(ps_f32(b_o, 256), lhsT=QbT[:, c, :], rhs=SbAll[:, c + 1, :],
                                                             start=False, stop=True),
                         reads=["QbT", ("SbAll", c + 1)], writes=[("ps", b_o)])
                    P.op("dve", lambda b_o=b_o: V.bn_stats(out=st6[:, 0:6], in_=ps_f32(b_o, 256)),
                         reads=[("ps", b_o)], writes=["st6"])
                    P.op("dve", lambda: V.bn_aggr(out=mv[:, 0:2], in_=st6[:, 0:6]), reads=["st6"], writes=["mv"])
                    P.op("act", lambda: A.activation(out=mv[:, 3:4], in_=mv[:, 1:2], func=AF.Sqrt, bias=epsT[:, 1:2]),
                         reads=["mv"], writes=["mv3"])
                    P.op("dve", lambda: V.reciprocal(out=mv[:, 2:3], in_=mv[:, 3:4]), reads=["mv3"], writes=["mv2"])
                    P.op("dve", lambda b_o=b_o, onx=onx: V.tensor_scalar(out=onx, in0=ps_f32(b_o, 256), scalar1=mv[:, 0:1],
                                                                         scalar2=mv[:, 2:3], op0=ALU.subtract, op1=ALU.mult),
                         reads=[("ps", b_o), "mv", "mv2"], writes=[("on", c % 2)])
                    P.op("pool", lambda c=c, onx=onx, ogx=ogx: G.tensor_tensor(out=ogx, in0=onx, in1=GRh[:, c, :], op=ALU.mult),
                         reads=[("on", c % 2), "GRh"], writes=[("og", c % 2)])
                    b_t = 4 + c % 2
                    for hf in range(2):
                        P.op("pe", lambda hf=hf, b_t=b_t, ogx=ogx: T.transpose(
                            ps_bf(b_t)[:, hf * 128:(hf + 1) * 128], ogx[:, hf * 128:(hf + 1) * 128], identb),
                            reads=[("og", c % 2)], writes=[("ps", b_t)])
                    P.op("act", lambda b_t=b_t, csl=csl: A.copy(
                        out=OGh[:, :, csl], in_=ps_bf(b_t)[:, 0:256].rearrange("p (a t) -> p a t", a=2)),
                        reads=[("ps", b_t)], writes=["OGh"])
                    b_s = 6 + c % 2
                    P.op("pe", lambda c=c, b_s=b_s: T.matmul(ps_f32(b_s, 256), lhsT=Kf[:, c, :], rhs=Vh[:, c, :],
                                                             start=True, stop=True),
                         reads=["Kf", "Vh"], writes=[("ps", b_s)])
                    P.op("dve", lambda b_s=b_s, h=h: V.scalar_tensor_tensor(
                        out=Sf32, in0=Sf32, scalar=CDEC[:, h:h + 1], in1=ps_f32(b_s, 256), op0=ALU.mult, op1=ALU.add),
                        reads=[("ps", b_s), "Sf32", "CDEC"], writes=["Sf32"])
                    P.op("act", lambda c=c: A.copy(out=Sfbf[(c + 1) % 2], in_=Sf32),
                         reads=["Sf32"], writes=[("Sfbf", (c + 1) % 2)])
                if not latent:
                    P.dma("sp", lambda h=h, pidx=pidx: SP.dma_start(out=ns_out[pidx, l, 0, h], in_=Sf32),
                          reads=["Sf32"], writes=[("ns", pidx, 0, h)])
                P.dma("sp", lambda h=h, r0=r0, N=N: SP.dma_start(
                    out=OGT[h * 256:(h + 1) * 256, r0:r0 + N].rearrange("(a p) t -> p a t", p=128), in_=OGh[:, :, 0:N]),
                    reads=["OGh"], writes=["OGT"])
        P.barrier()
        if stop_after == "p2b":
            return True

        al = Alloc(PBASE)
        dftc = al.get([128, 2, 512], BF16)
        dftp = al.get([128, 2, 2, 2, 128], BF16) if False else None
        dftp_t = [al.get([128, 2, 2, 128], BF16) for _ in range(2)]
        XFp = al.get([128, 4, NS], BF16)
        PQ = al.get([128, 2, 32, 512], BF16)
        Dp = [al.get([128, 2, 32, 128], BF16) for _ in range(2)]
        ufb = [al.get([128, 512], BF16) for _ in range(2)]
        P.dma("sp", lambda: SP.dma_start(out=dftc, in_=dftc_in), writes=["dftc"])
        for mt in range(2):
            P.dma("sp", lambda mt=mt: SP.dma_start(out=dftp_t[mt], in_=dftp_in[mt]), writes=["dftp"])
        di = 0
        for (r0, N, nch, latent, jt, pidx) in seqs:
            for pair in range(2):
                P.dma("sp", lambda pair=pair, r0=r0, N=N: SP.dma_start(
                    out=XFp[:, :, 0:N], in_=XFT[pair * 512:(pair + 1) * 512, r0:r0 + N].rearrange("(a p) t -> p a t", p=128)),
                    writes=["XFp"])
                for c in range(nch):
                    for gg in range(2):
                        bnk = (c * 2 + gg) % 2
                        for k2 in range(2):
                            P.op("pe", lambda c=c, gg=gg, k2=k2, bnk=bnk: T.matmul(
                                ps_f32(bnk), lhsT=XFp[:, gg * 2 + k2, c * 128:(c + 1) * 128], rhs=dftc[:, k2, :],
                                start=(k2 == 0), stop=(k2 == 1)), reads=["XFp", "dftc"], writes=[("ps", bnk)])
                        P.op("act", lambda c=c, gg=gg, bnk=bnk: A.copy(
                            out=PQ[:, :, c, gg * 256:(gg + 1) * 256], in_=ps_f32(bnk).rearrange("p (a f) -> p a f", a=2)),
                            reads=[("ps", bnk)], writes=["PQ"])
                for mt in range(nch):
                    if latent:
                        dp = Dp[di % 2]
                        dtok = ("Dp", di % 2)
                        di += 1
                        P.dma("sp", lambda mt=mt, dp=dp: SP.dma_start(out=dp, in_=dfts_in[mt]), writes=[dtok])
                    else:
                        dp = dftp_t[mt]
                        dtok = "dftp"
                    bnk = 2 + mt % 2
                    n_mm = 2 * nch
                    i_mm = 0
                    for cs in range(2):
                        for ncn in range(nch):
                            P.op("pe", lambda cs=cs, ncn=ncn, bnk=bnk, dp=dp, i_mm=i_mm, n_mm=n_mm: T.matmul(
                                ps_f32(bnk), lhsT=dp[:, cs, ncn, :], rhs=PQ[:, cs, ncn, :],
                                start=(i_mm == 0), stop=(i_mm == n_mm - 1)), reads=[dtok, "PQ"], writes=[("ps", bnk)])
                            i_mm += 1
                    ub = ufb[mt % 2]
                    P.op("act", lambda bnk=bnk, ub=ub: A.copy(out=ub, in_=ps_f32(bnk)), reads=[("ps", bnk)],
                         writes=[("ufb", mt % 2)])
                    bt = 4 + mt % 2
                    for q4 in range(4):
                        P.op("pe", lambda q4=q4, bt=bt, ub=ub: T.transpose(
                            ps_bf(bt)[:, q4 * 128:(q4 + 1) * 128], ub[:, q4 * 128:(q4 + 1) * 128], identb),
                            reads=[("ufb", mt % 2)], writes=[("ps", bt)])
                    P.op("dve", lambda mt=mt, bt=bt: V.tensor_copy(
                        out=XFp[:, :, mt * 128:(mt + 1) * 128], in_=ps_bf(bt)[:, 0:512].rearrange("p (a t) -> p a t", a=4)),
                        reads=[("ps", bt)], writes=["XFp"])
                P.dma("sp", lambda pair=pair, r0=r0, N=N: SP.dma_start(
                    out=UFT[pair * 512:(pair + 1) * 512, r0:r0 + N].rearrange("(a p) t -> p a t", p=128), in_=XFp[:, :, 0:N]),
                    reads=["XFp"], writes=["UFT"])
        P.barrier()
        if stop_after == "p2c":
            return True

        al = Alloc(PBASE)
        wro = al.get([128, 16, D], BF16)
        wfo = al.get([128, 8, D], BF16)
        wou = al.get([128, 8, D], BF16)
        wr = al.get([128, 8, NE], F32)
        brt = al.get([128, NE], F32)
        g2t = al.get([128, 8], F32)
        vtmp = al.get([128, 2, 8], F32)
        bct = al.get([128, 128], F32)
        G1 = [al.get([128, D], F32) for _ in range(2)]
        A2 = [al.get([128, D], F32) for _ in range(2)]
        B2 = [al.get([128, D], F32) for _ in range(2)]
        OGt = al.get([128, 16, 512], BF16)
        UFt = al.get([128, 8, 512], BF16)
        GAt = al.get([128, 8, 512], BF16)
        GBt = al.get([128, 8, 512], BF16)
        ta_ = [al.get([128, 512], F32)] * 2
        tb_ = [al.get([128, 512], F32)] * 2
        mT = al.get([128, 8, 512], BF16)
        xt2 = [al.get([128, D], F32) for _ in range(2)]
        yt2 = al.get([128, D], F32)
        h2 = al.get([128, D], F32)
        h2b = [al.get([128, D], BF16)] * 2
        h2T = al.get([128, 8, 128], F32)
        junk2 = yt2
        ss2 = al.get([128, 4], F32)
        Lsb = al.get([128, NT, NE], F32)
        P.dma("pool", lambda: G.dma_start(out=wro, in_=w_ret_o[l].rearrange("(kc p) c -> p kc c", p=128)), writes=["wro"])
        P.dma("pool", lambda: G.dma_start(out=wfo, in_=w_four[l].rearrange("(kc p) c -> p kc c", p=128)), writes=["wfo"])
        P.dma("pool", lambda: G.dma_start(out=wou, in_=w_out[l].rearrange("(kc p) c -> p kc c", p=128)), writes=["wou"])
        P.dma("sp", lambda: SP.dma_start(out=wr, in_=w_router[l].rearrange("(kc p) c -> p kc c", p=128)), writes=["wr"])
        P.dma("sp", lambda: SP.dma_start(out=brt, in_=b_router[l:l + 1, :].to_broadcast([128, NE])), writes=["brt"])
        P.dma("sp", lambda: SP.dma_start(out=g2t, in_=g2T[l]), writes=["g2t"])
        for j in range(2):
            P.op("dve", lambda j=j: V.tensor_copy(out=vtmp[:, 0, :], in_=modT[:, 16:24, j]), reads=["bc_dst"], writes=["vecsrc"])
            bcast_row(G1[j], vtmp[:, 0, :], bct, 7)
            P.op("dve", lambda j=j: V.scalar_tensor_tensor(out=vtmp[:, 0, :], in0=modT[:, 32:40, j], scalar=1.0, in1=g2t,
                                                           op0=ALU.add, op1=ALU.mult), reads=["bc_dst", "g2t"], writes=["vecsrc"])
            bcast_row(A2[j], vtmp[:, 0, :], bct, 7)
            P.op("dve", lambda j=j: V.tensor_copy(out=vtmp[:, 0, :], in_=modT[:, 24:32, j]), reads=["bc_dst"], writes=["vecsrc"])
            bcast_row(B2[j], vtmp[:, 0, :], bct, 7)
        for tg in range(10):
            j = 0 if tg < 8 else 1
            tsl = slice(tg * 512, (tg + 1) * 512)
            P.dma("sp", lambda tsl=tsl: SP.dma_start(out=OGt, in_=OGT[:, tsl].rearrange("(kc p) t -> p kc t", p=128)), writes=["OGt"])
            P.dma("sp", lambda tsl=tsl: SP.dma_start(out=UFt, in_=UFT[:, tsl].rearrange("(kc p) t -> p kc t", p=128)), writes=["UFt"])
            P.dma("sp", lambda tsl=tsl: SP.dma_start(out=GAt, in_=GAT[:, tsl].rearrange("(kc p) t -> p kc t", p=128)), writes=["GAt"])
            P.dma("sp", lambda tsl=tsl: SP.dma_start(out=GBt, in_=GBT[:, tsl].rearrange("(kc p) t -> p kc t", p=128)), writes=["GBt"])
            for dc in range(8):
                dsl = slice(dc * 128, (dc + 1) * 128)
                bA, bB = (dc % 2) * 2, (dc % 2) * 2 + 1
                for kc in range(16):
                    P.op("pe", lambda kc=kc, bA=bA, dsl=dsl: T.matmul(ps_f32(bA), lhsT=wro[:, kc, dsl], rhs=OGt[:, kc, :],
                                                                      start=(kc == 0), stop=(kc == 15)),
                         reads=["wro", "OGt"], writes=[("ps", bA)])
                for kc in range(8):
                    P.op("pe", lambda kc=kc, bB=bB, dsl=dsl: T.matmul(ps_f32(bB), lhsT=wfo[:, kc, dsl], rhs=UFt[:, kc, :],
                                                                      start=(kc == 0), stop=(kc == 7)),
                         reads=["wfo", "UFt"], writes=[("ps", bB)])
                ta, tb = ta_[dc % 2], tb_[dc % 2]
                P.op("dve", lambda bA=bA, ta=ta, dc=dc: V.tensor_tensor(out=ta, in0=ps_f32(bA), in1=GAt[:, dc, :], op=ALU.mult),
                     reads=[("ps", bA), "GAt"], writes=["ta"])
                P.op("dve", lambda bB=bB, tb=tb, dc=dc: V.tensor_tensor(out=tb, in0=ps_f32(bB), in1=GBt[:, dc, :], op=ALU.mult),
                     reads=[("ps", bB), "GBt"], writes=["tb"])
                P.op("pool", lambda ta=ta, tb=tb, dc=dc: G.tensor_tensor(out=mT[:, dc, :], in0=ta, in1=tb, op=ALU.add),
                     reads=["ta", "tb"], writes=["mT"])
            for ts in range(4):
                tt = tg * 4 + ts
                xb = xt2[tt % 2]
                hb = h2b[tt % 2]
                P.dma("sp", lambda tt=tt, xb=xb: SP.dma_start(out=xb, in_=Xsrc[tt * 128:(tt + 1) * 128, :]),
                      reads=[("Xs", tt)], writes=[("xt2", tt % 2)])
                for hf in range(2):
                    for kc in range(8):
                        P.op("pe", lambda kc=kc, hf=hf, ts=ts: T.matmul(
                            ps_f32(4 + hf), lhsT=mT[:, kc, ts * 128:(ts + 1) * 128], rhs=wou[:, kc, hf * 512:(hf + 1) * 512],
                            start=(kc == 0), stop=(kc == 7)), reads=["mT", "wou"], writes=[("ps", 4 + hf)])
                P.op("dve", lambda j=j: V.tensor_tensor(out=yt2.rearrange("p (a f) -> p a f", a=2), in0=psum[:, 4:6, :],
                                                        in1=G1[j].rearrange("p (a f) -> p a f", a=2), op=ALU.mult),
                     reads=[("ps", 4), ("ps", 5), "bc_dst"], writes=["yt2"])
                P.op("pool", lambda xb=xb: G.tensor_tensor(out=xb, in0=xb, in1=yt2, op=ALU.add),
                     reads=["yt2", ("xt2", tt % 2)], writes=[("xt2", tt % 2)])
                P.dma("sp", lambda tt=tt, xb=xb: SP.dma_start(out=Xs[tt * 128:(tt + 1) * 128, :], in_=xb),
                      reads=[("xt2", tt % 2)], writes=[("Xs", tt)])
                P.op("act", lambda xb=xb: A.activation(out=junk2, in_=xb, func=AF.Square, accum_out=ss2[:, 0:1]),
                     reads=[("xt2", tt % 2)], writes=["yt2", "ss2"])
                P.op("act", lambda: A.activation(out=ss2[:, 1:2], in_=ss2[:, 0:1], func=AF.Sqrt, scale=1.0 / D, bias=epsT[:, 0:1]),
                     reads=["ss2"], writes=["ss2b"])
                P.op("dve", lambda: V.reciprocal(out=ss2[:, 2:3], in_=ss2[:, 1:2]), reads=["ss2b"], writes=["ss2c"])
                P.op("dve", lambda xb=xb, j=j: V.scalar_tensor_tensor(out=h2, in0=xb, scalar=ss2[:, 2:3], in1=A2[j],
                                                                      op0=ALU.mult, op1=ALU.mult),
                     reads=[("xt2", tt % 2), "ss2c", "bc_dst"], writes=["h2"])
                P.op("pool", lambda j=j: G.tensor_tensor(out=h2, in0=h2, in1=B2[j], op=ALU.add),
                     reads=["h2", "bc_dst"], writes=["h2"])
                P.op("act", lambda hb=hb: A.copy(out=hb, in_=h2), reads=["h2"], writes=["h2b"])
                P.dma("sp", lambda tt=tt, hb=hb: SP.dma_start(out=H2[tt * 128:(tt + 1) * 128, :], in_=hb),
                      reads=["h2b"], writes=["H2"])
                for kc in range(8):
                    P.op("pe", lambda kc=kc: T.matmul(psum[:, 6 + kc // 4, (kc % 4) * 128:(kc % 4 + 1) * 128],
                                                      lhsT=h2[:, kc * 128:(kc + 1) * 128], rhs=ident, start=True, stop=True),
                         reads=["h2"], writes=[("ps", 6 + kc // 4)])
                P.op("dve", lambda: V.tensor_copy(out=h2T.rearrange("p (a k) t -> p a (k t)", a=2), in_=psum[:, 6:8, :]),
                     reads=[("ps", 6), ("ps", 7)], writes=["h2T"])
                for kc in range(8):
                    P.op("pe", lambda kc=kc: T.matmul(psum[:, 6, 0:NE], lhsT=h2T[:, kc, :], rhs=wr[:, kc, :],
                                                      start=(kc == 0), stop=(kc == 7)),
                         reads=["h2T", "wr"], writes=[("ps", 6)])
                P.op("dve", lambda tt=tt: V.tensor_tensor(out=Lsb[:, tt, :], in0=psum[:, 6, 0:NE], in1=brt, op=ALU.add),
                     reads=[("ps", 6), "brt"], writes=["Lsb"])
        P.dma("sp", lambda: SP.dma_start(out=LG, in_=Lsb.rearrange("p a b -> p (a b)")), reads=["Lsb"], writes=["LG"])
        P.barrier()
        if stop_after == "p2d":
            return True

        al = Alloc(PBASE)
        WK = al.get([128, NT, 4], F32)
        SLI = al.get([128, NT, 4], I32)
        BLKI = al.get([128, NBLK], I32)
        IDXW = al.get([128, NBLK, 8], I32)
        IDXB1 = al.get([128, NBLK], I32)
        IDXB2 = al.get([128, NBLK], I32)
        R3END = al.off
        idxf = al.get([128, NBLK, 8], F32)
        idxg = al.get([128, NBLK], F32)
        Lr = al.get([128, NT, NE], F32)
        v8 = al.get([128, NT, 8], F32)
        i8 = al.get([128, NT, 8], U32)
        i8f = al.get([128, NT, 8], F32)
        e4 = al.get([128, NT, 4], F32)
        s4 = al.get([128, NT], F32)
        mask = al.get([128, NT, NE], F32)
        pos = al.get([128, NT, NE], F32)
        tot = al.get([128, NT, NE], F32)
        carry = al.get([128, NT + 1, NE], F32)
        cntv = al.get([128, 4, NE], F32)
        pend = al.get([128, NE], F32)
        slot = al.get([128, NT, NE], F32)
        oh = al.get([128, NT, NE], F32)
        slk = al.get([128, NT, 4], F32)
        cmpb = al.get([128, NBLK, NE], F32)
        blkf = al.get([128, NBLK], F32)
        P.dma("sp", lambda: SP.dma_start(out=Lr.rearrange("p a b -> p (a b)"), in_=LG), writes=["Lr"])
        for tt in range(NT):
            P.op("dve", lambda tt=tt: V.max(out=v8[:, tt, :], in_=Lr[:, tt, :]), reads=["Lr"], writes=["v8"])
            P.op("dve", lambda tt=tt: V.max_index(out=i8[:, tt, :], in_max=v8[:, tt, :], in_values=Lr[:, tt, :]),
                 reads=["Lr", "v8"], writes=["i8"])
        P.op("dve", lambda: V.tensor_copy(out=i8f, in_=i8), reads=["i8"], writes=["i8f"])
        P.op("dve", lambda: V.tensor_tensor(out=e4, in0=v8[:, :, 0:4], in1=v8[:, :, 0:1].to_broadcast([128, NT, 4]),
                                            op=ALU.subtract), reads=["v8"], writes=["e4"])
        P.op("act", lambda: A.activation(out=e4, in_=e4, func=AF.Exp), reads=["e4"], writes=["e4"])
        P.op("dve", lambda: V.tensor_reduce(out=s4, in_=e4, axis=AX.X, op=ALU.add), reads=["e4"], writes=["s4"])
        P.op("dve", lambda: V.reciprocal(out=s4, in_=s4), reads=["s4"], writes=["s4"])
        P.op("dve", lambda: V.tensor_tensor(out=WK, in0=e4, in1=s4.unsqueeze(2).to_broadcast([128, NT, 4]), op=ALU.mult),
             reads=["e4", "s4"], writes=["WK"])
        P.op("dve", lambda: V.tensor_tensor(out=mask, in0=Lr, in1=v8[:, :, 3:4].to_broadcast([128, NT, NE]), op=ALU.is_ge),
             reads=["Lr", "v8"], writes=["mask"])
        mflat = mask.rearrange("p a b -> p (a b)")
        for (c0, cn, bnk) in ((0, 512, 0), (512, 512, 1), (1024, 256, 2)):
            P.op("pe", lambda c0=c0, cn=cn, bnk=bnk: T.matmul(ps_f32(bnk, cn), lhsT=cst[:, C_LTRI:C_LTRI + 128],
                                                              rhs=mflat[:, c0:c0 + cn], start=True, stop=True),
                 reads=["mask"], writes=[("ps", bnk)])
            P.op("act", lambda c0=c0, cn=cn, bnk=bnk: A.copy(out=pos.rearrange("p a b -> p (a b)")[:, c0:c0 + cn],
                                                             in_=ps_f32(bnk, cn)), reads=[("ps", bnk)], writes=["pos"])
            P.op("pe", lambda c0=c0, cn=cn, bnk=bnk: T.matmul(ps_f32(bnk + 3, cn), lhsT=ones, rhs=mflat[:, c0:c0 + cn],
                                                              start=True, stop=True),
                 reads=["mask"], writes=[("ps", bnk + 3)])
            P.op("act", lambda c0=c0, cn=cn, bnk=bnk: A.copy(out=tot.rearrange("p a b -> p (a b)")[:, c0:c0 + cn],
                                                             in_=ps_f32(bnk + 3, cn)), reads=[("ps", bnk + 3)], writes=["tot"])
        P.op("dve", lambda: V.memset(carry[:, 0, :], 0.0), writes=["carry"])
        for tt in range(NT):
            P.op("dve", lambda tt=tt: V.tensor_tensor(out=carry[:, tt + 1, :], in0=carry[:, tt, :], in1=tot[:, tt, :], op=ALU.add),
                 reads=["tot", "carry"], writes=["carry"])
        cnt_ = carry[:, NT, :]
        cnti = cntv.bitcast(I32)
        P.op("dve", lambda: V.tensor_copy(out=cnti[:, 0, :], in_=cnt_), reads=["carry"], writes=["cv0"])
        P.op("dve", lambda: V.tensor_scalar(out=cnti[:, 1, :], in0=cnti[:, 0, :], scalar1=BLK - 1, scalar2=None, op0=ALU.add),
             reads=["cv0"], writes=["cv1"])
        P.op("dve", lambda: V.tensor_scalar(out=cnti[:, 2, :], in0=cnti[:, 1, :], scalar1=9, scalar2=9,
                                            op0=ALU.arith_shift_right, op1=ALU.logical_shift_left), reads=["cv1"], writes=["cv2"])
        P.op("dve", lambda: V.tensor_copy(out=cntv[:, 3, :], in_=cnti[:, 2, :]), reads=["cv2"], writes=["cv3"])
        P.op("dve", lambda: V.tensor_copy(out=pend[:, 0:1], in_=cntv[:, 3, 0:1]), reads=["cv3"], writes=["pend"])
        for e in range(1, NE):
            P.op("dve", lambda e=e: V.tensor_tensor(out=pend[:, e:e + 1], in0=pend[:, e - 1:e], in1=cntv[:, 3, e:e + 1], op=ALU.add),
                 reads=["pend", "cv3"], writes=["pend"])
        P.op("dve", lambda: V.tensor_tensor(out=cntv[:, 0, :], in0=pend, in1=cntv[:, 3, :], op=ALU.subtract),
             reads=["pend", "cv3", "cv1"], writes=["cv0"])
        P.op("dve", lambda: V.tensor_tensor(out=pos, in0=pos, in1=carry[:, 0:NT, :], op=ALU.add),
             reads=["pos", "carry"], writes=["pos"])
        P.op("dve", lambda: V.tensor_tensor(out=slot, in0=pos, in1=cntv[:, 0:1, :].to_broadcast([128, NT, NE]), op=ALU.add),
             reads=["pos", "cv0"], writes=["slot"])
        for k in range(4):
            P.op("dve", lambda k=k: V.tensor_tensor(
                out=oh, in0=cst[:, C_IOTA:C_IOTA + NE].unsqueeze(1).to_broadcast([128, NT, NE]),
                in1=i8f[:, :, k:k + 1].to_broadcast([128, NT, NE]), op=ALU.is_equal), reads=["i8f", "slk"], writes=["oh"])
            P.op("dve", lambda: V.tensor_tensor(out=oh, in0=oh, in1=slot, op=ALU.mult), reads=["oh", "slot"], writes=["oh"])
            P.op("dve", lambda k=k: V.tensor_reduce(out=slk[:, :, k], in_=oh, axis=AX.X, op=ALU.add),
                 reads=["oh"], writes=["slk"])
        P.op("dve", lambda: V.tensor_copy(out=SLI, in_=slk), reads=["slk"], writes=["SLI"])
        P.op("dve", lambda: V.tensor_tensor(
            out=cmpb, in0=pend.unsqueeze(1).to_broadcast([128, NBLK, NE]),
            in1=cst[:, C_BC:C_BC + NBLK].unsqueeze(2).to_broadcast([128, NBLK, NE]), op=ALU.is_le),
            reads=["pend"], writes=["cmpb"])
        P.op("dve", lambda: V.tensor_reduce(out=blkf, in_=cmpb, axis=AX.X, op=ALU.add), reads=["cmpb"], writes=["blkf"])
        P.op("dve", lambda: V.tensor_scalar(out=blkf, in0=blkf, scalar1=float(NE - 1), scalar2=None, op0=ALU.min),
             reads=["blkf"], writes=["blkf"])
        P.op("dve", lambda: V.tensor_copy(out=BLKI, in_=blkf), reads=["blkf"], writes=["BLKI"])
        P.op("dve", lambda: V.tensor_scalar(out=idxg, in0=blkf, scalar1=1024.0, scalar2=float(l * NE * 1024),
                                            op0=ALU.mult, op1=ALU.add), reads=["blkf"], writes=["idxg"])
        P.op("dve", lambda: V.tensor_tensor(out=idxf, in0=idxg.unsqueeze(2).to_broadcast([128, NBLK, 8]),
                                            in1=cst[:, C_KCP:C_KCP + 8].unsqueeze(1).to_broadcast([128, NBLK, 8]), op=ALU.add),
             reads=["idxg"], writes=["idxf"])
        P.op("dve", lambda: V.tensor_copy(out=IDXW, in_=idxf), reads=["idxf"], writes=["IDXW"])
        P.op("dve", lambda: V.tensor_scalar(out=idxg, in0=blkf, scalar1=128.0, scalar2=float(l * NE * 128),
                                            op0=ALU.mult, op1=ALU.add), reads=["blkf", "idxf"], writes=["idxg"])
        P.op("dve", lambda: V.tensor_tensor(out=idxg, in0=idxg, in1=cst[:, C_KB:C_KB + 1].to_broadcast([128, NBLK]), op=ALU.add),
             reads=["idxg"], writes=["idxg"])
        P.op("dve", lambda: V.tensor_copy(out=IDXB1, in_=idxg), reads=["idxg"], writes=["IDXB1"])
        P.op("dve", lambda: V.tensor_scalar(out=idxg, in0=blkf, scalar1=float(l * NE), scalar2=None, op0=ALU.add),
             reads=["blkf", "IDXB1"], writes=["idxg"])
        P.op("dve", lambda: V.tensor_copy(out=IDXB2, in_=idxg), reads=["idxg"], writes=["IDXB2"])
        P.dma("sp", lambda: SP.dma_start(out=SLd, in_=SLI.rearrange("p a b -> p (a b)")), reads=["SLI"], writes=["SLd"])

        htk = [al.get([128, D], BF16) for _ in range(4)]
        R3C = R3END
        for tt in range(NT):
            hb = htk[tt % 4]
            P.dma("sp", lambda tt=tt, hb=hb: SP.dma_start(out=hb, in_=H2[tt * 128:(tt + 1) * 128, :]), writes=[("htk", tt % 4)])
            for k in range(4):
                P.dma("pool", lambda tt=tt, k=k, hb=hb: G.indirect_dma_start(
                    out=XP, out_offset=bass.IndirectOffsetOnAxis(ap=SLI[:, tt, k:k + 1], axis=0),
                    in_=hb, in_offset=None, bounds_check=REG["xp"], oob_is_err=False),
                    reads=[("htk", tt % 4), "SLI"], writes=["XP"])
        if stop_after in ("p3c", "p3d"):
            di = dbgo.bitcast(I32)
            P.dma("sp", lambda: SP.dma_start(out=dbgo[:, 0:160], in_=WK.rearrange("p a b -> p (a b)")), reads=["WK"], writes=["dbgo"])
            P.dma("sp", lambda: SP.dma_start(out=di[:, 256:256 + NBLK], in_=BLKI), reads=["BLKI"], writes=["dbgo1"])
            P.dma("sp", lambda: SP.dma_start(out=di[:, 512:512 + NBLK * 8], in_=IDXW.rearrange("p a b -> p (a b)")), reads=["IDXW"], writes=["dbgo2"])
            P.dma("sp", lambda: SP.dma_start(out=di[:, 1100:1100 + NBLK], in_=IDXB1), reads=["IDXB1"], writes=["dbgo3"])
            P.dma("sp", lambda: SP.dma_start(out=di[:, 1200:1200 + NBLK], in_=IDXB2), reads=["IDXB2"], writes=["dbgo4"])
            P.dma("sp", lambda: SP.dma_start(out=dbgo[:, 1300:1332], in_=pend), reads=["pend"], writes=["dbgo5"])
            P.dma("sp", lambda: SP.dma_start(out=dbgo[:, 1400:1432], in_=carry[:, NT, :]), reads=["carry"], writes=["dbgo6"])
        P.barrier()
        if stop_after == "p3c":
            return True

        al = Alloc(R3C)
        w1b = [al.get([128, 8, 2 * DE], BF16) for _ in range(2)]
        w2b = [al.get([128, 8, D], BF16) for _ in range(2)]
        b1t = [al.get([128, 16], F32) for _ in range(2)]
        b2t = [al.get([128, D], F32) for _ in range(2)]
        xtok = al.get([128, 4, D], BF16)
        xbT = al.get([128, 8, 512], BF16)
        actT = al.get([128, 8, 512], BF16)
        gA = [al.get([128, 512], F32) for _ in range(2)]
        sA = [al.get([128, 512], F32) for _ in range(2)]
        lA = [al.get([128, 512], F32) for _ in range(2)]
        ytk = al.get([128, 4, D], F32)
        for b in range(NBLK):
            wi = b % 2

            for kc in range(8):
                P.dma("pool", lambda b=b, wi=wi, kc=kc: G.indirect_dma_start(
                    out=w1b[wi][:, kc, :], out_offset=None, in_=w1.rearrange("l e r c -> (l e r) c"),
                    in_offset=bass.IndirectOffsetOnAxis(ap=IDXW[:, b, kc:kc + 1], axis=0),
                    bounds_check=REG["w"], oob_is_err=False), writes=[("w1b", wi, kc // 2)])
            for kc in range(8):
                P.dma("pool", lambda b=b, wi=wi, kc=kc: G.indirect_dma_start(
                    out=w2b[wi][:, kc, :], out_offset=None, in_=w2.rearrange("l e r c -> (l e r) c"),
                    in_offset=bass.IndirectOffsetOnAxis(ap=IDXW[:, b, kc:kc + 1], axis=0),
                    bounds_check=REG["w"], oob_is_err=False), writes=[("w2b", wi, kc // 4)])
            P.dma("pool", lambda b=b, wi=wi: G.indirect_dma_start(
                out=b1t[wi], out_offset=None, in_=b1T.rearrange("l e p c -> (l e p) c"),
                in_offset=bass.IndirectOffsetOnAxis(ap=IDXB1[:, b:b + 1], axis=0),
                bounds_check=REG["b1"], oob_is_err=False), writes=[("b1t", wi)])
            P.dma("pool", lambda b=b, wi=wi: G.indirect_dma_start(
                out=b2t[wi], out_offset=None, in_=b2.rearrange("l e c -> (l e) c"),
                in_offset=bass.IndirectOffsetOnAxis(ap=IDXB2[:, b:b + 1], axis=0),
                bounds_check=REG["b2"], oob_is_err=False), writes=[("b2t", wi)])
            P.dma("sp", lambda b=b: SP.dma_start(out=xtok, in_=XP[b * BLK:(b + 1) * BLK, :].rearrange("(s p) d -> p s d", p=128)),
                  writes=["xtok"])
            for s in range(4):
                bnk = s % 2
                for kc in range(8):
                    P.op("pe", lambda s=s, kc=kc, bnk=bnk: T.transpose(
                        ps_bf(bnk)[:, kc * 128:(kc + 1) * 128], xtok[:, s, kc * 128:(kc + 1) * 128], identb),
                        reads=["xtok"], writes=[("ps", bnk)])
                P.op("act", lambda s=s, bnk=bnk: A.copy(out=xbT[:, :, s * 128:(s + 1) * 128],
                                                        in_=ps_bf(bnk).rearrange("p (k t) -> p k t", k=8)),
                     reads=[("ps", bnk)], writes=["xbT"])
            w1r = [("w1b", wi, q) for q in range(4)]
            w2r = [("w2b", wi, q) for q in range(2)]
            for mc in range(8):
                bG, bL = 2 + (mc % 2) * 2, 3 + (mc % 2) * 2
                g_, s_, l_ = gA[mc % 2], sA[mc % 2], lA[mc % 2]
                for kc in range(8):
                    P.op("pe", lambda kc=kc, mc=mc, bG=bG, wi=wi: T.matmul(
                        ps_f32(bG), lhsT=w1b[wi][:, kc, mc * 128:(mc + 1) * 128], rhs=xbT[:, kc, :],
                        start=(kc == 0), stop=(kc == 7)), reads=w1r + ["xbT"], writes=[("ps", bG)])
                for kc in range(8):
                    P.op("pe", lambda kc=kc, mc=mc, bL=bL, wi=wi: T.matmul(
                        ps_f32(bL), lhsT=w1b[wi][:, kc, DE + mc * 128:DE + (mc + 1) * 128], rhs=xbT[:, kc, :],
                        start=(kc == 0), stop=(kc == 7)), reads=w1r + ["xbT"], writes=[("ps", bL)])
                P.op("dve", lambda mc=mc, bG=bG, g_=g_, wi=wi: V.tensor_scalar(
                    out=g_, in0=ps_f32(bG), scalar1=b1t[wi][:, mc:mc + 1], scalar2=7.0, op0=ALU.add, op1=ALU.min),
                    reads=[("ps", bG), ("b1t", wi)], writes=[("gA", mc % 2)])
                P.op("act", lambda g_=g_, s_=s_: A.activation(out=s_, in_=g_, func=AF.Sigmoid, scale=1.702),
                     reads=[("gA", mc % 2)], writes=[("sA", mc % 2)])
                P.op("pool", lambda g_=g_, s_=s_: G.tensor_tensor(out=s_, in0=g_, in1=s_, op=ALU.mult),
                     reads=[("gA", mc % 2), ("sA", mc % 2)], writes=[("sA", mc % 2)])
                P.op("dve", lambda mc=mc, bL=bL, l_=l_, wi=wi: V.tensor_scalar(
                    out=l_, in0=ps_f32(bL), scalar1=b1t[wi][:, 8 + mc:9 + mc], scalar2=7.0, op0=ALU.add, op1=ALU.min),
                    reads=[("ps", bL), ("b1t", wi)], writes=[("lA", mc % 2)])
                P.op("dve", lambda l_=l_: V.tensor_scalar(out=l_, in0=l_, scalar1=-7.0, scalar2=1.0, op0=ALU.max, op1=ALU.add),
                     reads=[("lA", mc % 2)], writes=[("lA", mc % 2)])
                P.op("pool", lambda mc=mc, l_=l_, s_=s_: G.tensor_tensor(out=actT[:, mc, :], in0=l_, in1=s_, op=ALU.mult),
                     reads=[("lA", mc % 2), ("sA", mc % 2)], writes=["actT"])
            for s in range(4):
                b0 = 6
                for hf in range(2):
                    for kc in range(8):
                        P.op("pe", lambda kc=kc, hf=hf, s=s, wi=wi: T.matmul(
                            ps_f32(6 + hf), lhsT=actT[:, kc, s * 128:(s + 1) * 128], rhs=w2b[wi][:, kc, hf * 512:(hf + 1) * 512],
                            start=(kc == 0), stop=(kc == 7)), reads=w2r + ["actT"], writes=[("ps", 6 + hf)])
                P.op("dve", lambda s=s, wi=wi: V.tensor_tensor(out=ytk[:, s, :].rearrange("p (a f) -> p a f", a=2),
                                                               in0=psum[:, 6:8, :],
                                                               in1=b2t[wi].rearrange("p (a f) -> p a f", a=2), op=ALU.add),
                     reads=[("ps", 6), ("ps", 7), ("b2t", wi)], writes=["ytk"])
            P.dma("sp", lambda b=b: SP.dma_start(out=Yd[b * BLK:(b + 1) * BLK, :].rearrange("(s p) d -> p s d", p=128), in_=ytk),
                  reads=["ytk"], writes=["Yd"])
        P.barrier()
        if stop_after == "p3d":
            return True

        al = Alloc(R3END)
        g2row = al.get([128, 2, 8], F32)
        bct2 = al.get([128, 128], F32)
        G2 = [al.get([128, D], F32) for _ in range(2)]
        FG = al.get([128, D], F32)
        yk = [al.get([128, 4, D], F32) for _ in range(2)]
        xt3 = [al.get([128, D], F32) for _ in range(2)]
        acc = al.get([128, D], F32)
        junk3 = al.get([128, D], F32)
        ss3 = al.get([128, 4], F32)
        for j in range(2):
            P.op("dve", lambda j=j: V.tensor_copy(out=g2row[:, 0, :], in_=modT[:, 40:48, j]), reads=["bc_dst"], writes=["vecsrc"])
            bcast_row(G2[j], g2row[:, 0, :], bct2, 7)
        if l == DEPTH - 1:
            P.dma("sp", lambda: SP.dma_start(out=FG, in_=final_g.to_broadcast([128, D])), writes=["FG"])
        for tt in range(NT):
            j = 0 if tt < 32 else 1
            ykb = yk[tt % 2]
            xb = xt3[tt % 2]
            for k in range(4):
                P.dma("pool", lambda tt=tt, k=k, ykb=ykb: G.indirect_dma_start(
                    out=ykb[:, k, :], out_offset=None, in_=Yd,
                    in_offset=bass.IndirectOffsetOnAxis(ap=SLI[:, tt, k:k + 1], axis=0),
                    bounds_check=REG["xp"], oob_is_err=False), writes=[("yk", tt % 2, k)])
            P.dma("sp", lambda tt=tt, xb=xb: SP.dma_start(out=xb, in_=Xs[tt * 128:(tt + 1) * 128, :]),
                  reads=[("Xs", tt)], writes=[("xt3", tt % 2)])
            P.op("dve", lambda tt=tt, ykb=ykb: V.tensor_scalar(out=acc, in0=ykb[:, 0, :], scalar1=WK[:, tt, 0:1], scalar2=None,
                                                               op0=ALU.mult), reads=[("yk", tt % 2, 0)], writes=["acc"])
            for k in range(1, 4):
                P.op("dve", lambda tt=tt, k=k, ykb=ykb: V.scalar_tensor_tensor(
                    out=acc, in0=ykb[:, k, :], scalar=WK[:, tt, k:k + 1], in1=acc, op0=ALU.mult, op1=ALU.add),
                    reads=[("yk", tt % 2, k), "acc"], writes=["acc"])
            P.op("pool", lambda j=j: G.tensor_tensor(out=acc, in0=acc, in1=G2[j], op=ALU.mult), reads=["acc", "bc_dst"], writes=["acc"])
            P.op("dve", lambda xb=xb: V.tensor_tensor(out=xb, in0=xb, in1=acc, op=ALU.add),
                 reads=["acc", ("xt3", tt % 2)], writes=[("xt3", tt % 2)])
            if l < DEPTH - 1:
                P.dma("sp", lambda tt=tt, xb=xb: SP.dma_start(out=Xs[tt * 128:(tt + 1) * 128, :], in_=xb),
                      reads=[("xt3", tt % 2)], writes=[("Xs", tt)])
            else:
                P.op("act", lambda xb=xb: A.activation(out=junk3, in_=xb, func=AF.Square, accum_out=ss3[:, 0:1]),
                     reads=[("xt3", tt % 2)], writes=["junk3", "ss3"])
                P.op("act", lambda: A.activation(out=ss3[:, 1:2], in_=ss3[:, 0:1], func=AF.Sqrt, scale=1.0 / D, bias=epsT[:, 0:1]),
                     reads=["ss3"], writes=["ss3b"])
                P.op("dve", lambda: V.reciprocal(out=ss3[:, 2:3], in_=ss3[:, 1:2]), reads=["ss3b"], writes=["ss3c"])
                P.op("dve", lambda xb=xb: V.scalar_tensor_tensor(out=xb, in0=xb, scalar=ss3[:, 2:3], in1=FG,
                                                                 op0=ALU.mult, op1=ALU.mult),
                     reads=[("xt3", tt % 2), "ss3c", "FG"], writes=[("xt3", tt % 2)])
                P.dma("sp", lambda tt=tt, xb=xb: SP.dma_start(out=y_out[tt * 128:(tt + 1) * 128, :], in_=xb),
                      reads=[("xt3", tt % 2)], writes=[("yo", tt)])
        P.barrier()
        return False

    for l in range(DEPTH):
        if do_layer(l):
            break
    P.barrier()
    P.emit()
    return nc, stack


_CONST_CACHE = {}


def _host_consts():
    if not _CONST_CACHE:
        _CONST_CACHE["cst"] = _consts()
        _CONST_CACHE["rot"] = _rot_tables()
        _CONST_CACHE["dftc"] = _dft_chan()
        _CONST_CACHE["dfts"] = _dft_seq(NS)
        _CONST_CACHE["dftp"] = _dft_seq(256)
    return _CONST_CACHE


def make_in_maps(inp, ncores=8):
    f = lambda a: np.ascontiguousarray(np.asarray(a, dtype=np.float32))
    hc = _host_consts()
    shared = {
        "w_ada": f(inp["w_ada"]),
        "b_adaT": f(np.asarray(inp["b_ada"]).reshape(DEPTH, 48, 128).transpose(0, 2, 1)),
        "g1T": f(np.asarray(inp["norm1_g"]).reshape(DEPTH, 8, 128).transpose(0, 2, 1)),
        "g2T": f(np.asarray(inp["norm2_g"]).reshape(DEPTH, 8, 128).transpose(0, 2, 1)),
        "w_in": f(inp["w_in"]),
        "decay": f(np.asarray(inp["ret_decay_logit"]).reshape(DEPTH, 16)),
        "w_ret_o": f(inp["w_ret_o"]),
        "w_four": f(inp["w_four"]),
        "w_out": f(inp["w_out"]),
        "w_router": f(inp["w_router"]),
        "b_router": f(inp["b_router"]),
        "w1": f(inp["w1"]),
        "b1T": f(np.asarray(inp["b1"]).reshape(DEPTH, NE, 16, 128).transpose(0, 1, 3, 2)),
        "w2": f(inp["w2"]),
        "b2": f(inp["b2"]),
        "final_g": f(np.asarray(inp["final_g"]).reshape(1, D)),
        "cst": hc["cst"], "rot": hc["rot"], "dftc": hc["dftc"], "dfts": hc["dfts"], "dftp": hc["dftp"],
    }
    xp = np.asarray(inp["x_prompt"], np.float32)
    xs = np.asarray(inp["x_sample"], np.float32)
    st = np.asarray(inp["state_ret"], np.float32)
    c = np.asarray(inp["c"], np.float32)
    cc = np.asarray(inp["c_ctx"], np.float32)
    maps = []
    for i in range(ncores):
        m = dict(shared)
        m["x_in"] = np.ascontiguousarray(np.concatenate([xs[i], xp[4 * i:4 * i + 4].reshape(1024, D)], axis=0))
        m["s0"] = np.ascontiguousarray(st[i])
        cT = np.stack([c[i].reshape(8, 128).T, cc.reshape(8, 128).T], axis=-1)
        m["cT"] = np.ascontiguousarray(cT.astype(np.float32))
        maps.append(m)
    return maps


def kernel(**inputs):
    nc, stack = build()
    try:
        maps = make_in_maps(inputs, 8)
        res = run_bass_kernel_spmd(nc, maps, core_ids=list(range(8)))
    finally:
        stack.close()
    y_prompt = np.zeros((32, 256, D), np.float32)
    y_sample = np.zeros((8, NS, D), np.float32)
    new_state = np.zeros((32, DEPTH, 2, NH, DK, DV), np.float32)
    for i, r in enumerate(res.results):
        yo = np.asarray(r["y_out"])
        y_sample[i] = yo[:NS]
        y_prompt[4 * i:4 * i + 4] = yo[NS:].reshape(4, 256, D)
        new_state[4 * i:4 * i + 4] = np.asarray(r["ns_out"])
    return (y_prompt, y_sample, new_state)
```

```python
import os
from contextlib import ExitStack
import numpy as np
import ml_dtypes
import concourse.bass as bass
import concourse.mybir as mybir
from concourse.bass_utils import run_bass_kernel_spmd

F32 = mybir.dt.float32
BF16 = mybir.dt.bfloat16
I32 = mybir.dt.int32
U32 = mybir.dt.uint32
AF = mybir.ActivationFunctionType
ALU = mybir.AluOpType
AX = mybir.AxisListType

D = 1024
NTOK = 5120
NT = 40
NS = 4096
NH = 8
DK = 128
DV = 256
INW = 9216
NE = 32
DE = 1024
BLK = 512
NBLK = 72
PT = NBLK * BLK
EPS = 1e-6
GN_EPS = 1e-5
DEPTH = 2

C_ID, C_DF, C_TF, C_DB, C_TB, C_IP1, C_IB, C_LTRI, C_ONES = [i * 128 for i in range(9)]
C_KF = 9 * 128
C_KB = C_KF + 1
C_IOTA = C_KB + 1
C_BC = C_IOTA + 32
C_KCP = C_BC + NBLK
C_N = C_KCP + 8


def _consts():
    c = np.zeros((128, C_N), np.float32)
    p = np.arange(128)
    j = p[:, None].astype(np.float64)
    i = p[None, :].astype(np.float64)
    c[:, C_ID:C_ID + 128] = np.eye(128)
    c[:, C_DF:C_DF + 128] = np.maximum(i - j, 0)
    c[:, C_TF:C_TF + 128] = (i >= j)
    c[:, C_DB:C_DB + 128] = np.maximum(j - i, 0)
    c[:, C_TB:C_TB + 128] = (j >= i)
    c[:, C_IP1:C_IP1 + 128] = i + 1
    c[:, C_IB:C_IB + 128] = 128 - i
    c[:, C_LTRI:C_LTRI + 128] = (j < i)
    c[:, C_ONES:C_ONES + 128] = 1.0
    c[:, C_KF] = 127 - p
    c[:, C_KB] = p
    c[:, C_IOTA:C_IOTA + 32] = np.arange(32)[None, :]
    c[:, C_BC:C_BC + NBLK] = (np.arange(NBLK) * BLK)[None, :]
    c[:, C_KCP:C_KCP + 8] = np.arange(8)[None, :] * 128 + p[:, None]
    return c


def _rot_tables():
    t = np.arange(NS)
    row = (t // 64).astype(np.float32)
    col = (t % 64).astype(np.float32)
    nf = 32
    inv = (np.float32(10000.0) ** (-(np.arange(nf, dtype=np.float32)) / np.float32(nf))).astype(np.float32)
    ang = np.concatenate([row[:, None] * inv[None, :], col[:, None] * inv[None, :]], axis=1)
    ang = ang.astype(np.float64)
    cs = np.cos(ang).T
    sn = np.sin(ang).T
    tab = np.zeros((128, 2, NS), np.float32)
    tab[:64, 0] = cs
    tab[64:, 0] = cs
    tab[:64, 1] = sn
    tab[64:, 1] = sn
    return tab


def _dft_chan():
    c = np.arange(256)
    m = (c[:, None] * c[None, :]) % 256
    a = 2.0 * np.pi * m / 256.0
    cs = np.cos(a) / 16.0
    sn = -np.sin(a) / 16.0
    full = np.concatenate([cs, sn], axis=1)
    return full.reshape(2, 128, 512).transpose(1, 0, 2).astype(ml_dtypes.bfloat16)


def _dft_seq(n):
    nch = n // 128
    idx = np.arange(n, dtype=np.int64)
    m = (idx[:, None] * idx[None, :]) % n
    a = 2.0 * np.pi * m.astype(np.float64) / n
    sc = 1.0 / np.sqrt(n)
    out = np.zeros((nch, 128, 2, nch, 128), ml_dtypes.bfloat16)
    for cs, f in ((0, np.cos), (1, np.sin)):
        mat = (f(a) * sc).astype(np.float32)
        out[:, :, cs] = mat.reshape(nch, 128, nch, 128).transpose(2, 1, 0, 3).astype(ml_dtypes.bfloat16)
    return out


class Prog:
    CE = ("pe", "act", "dve", "pool")
    ALL = ("pe", "act", "dve", "pool", "sp")
    R = 10

    def __init__(self, nc, stack):
        self.nc = nc
        self.stack = stack
        self.ops = {e: [] for e in self.ALL}
        self.sems = {}
        self.phase = 0
        self.cnt = {e: 0 for e in self.CE}
        self.dcnt = {"sp": 0, "pool": 0}
        self.lastw = {}
        self.readers = {}
        self.waited = {e: {} for e in self.ALL}
        self.nsem = 0
        self.pool_init = None
        for q in self.dcnt:
            for s in range(self.R):
                self._mk(("d", q, s))
        self._new_phase()

    def _mk(self, key):
        self.sems[key] = self.stack.enter_context(self.nc.semaphore("s%d" % self.nsem))
        self.nsem += 1

    def _new_phase(self):
        self.phase += 1
        for e in self.CE:
            self.cnt[e] = 0
            self._mk(("c", e, self.phase))

    def _ck(self, e):
        return ("c", e, self.phase)

    def _deps(self, eng, reads, writes):
        deps = {}

        def add(ev):
            if ev is None:
                return
            k, v = ev
            if deps.get(k, 0) < v:
                deps[k] = v
        for t in reads:
            add(self.lastw.get(t))
        for t in writes:
            add(self.lastw.get(t))
            for k, v in self.readers.get(t, {}).items():
                add((k, v))
        waits = []
        for k, v in deps.items():
            if eng == "pe" and k == self._ck("pe"):
                continue
            if self.waited[eng].get(k, 0) >= v:
                continue
            self.waited[eng][k] = v
            waits.append((k, v))
        return waits

    def _post(self, ev, reads, writes):
        k, v = ev
        for t in reads:
            r = self.readers.setdefault(t, {})
            if r.get(k, 0) < v:
                r[k] = v
        for t in writes:
            self.lastw[t] = ev
            self.readers[t] = {}

    def op(self, eng, fn, reads=(), writes=()):
        waits = self._deps(eng, reads, writes)
        self.cnt[eng] += 1
        ev = (self._ck(eng), self.cnt[eng])
        self.ops[eng].append((waits, fn, ev[0], 1))
        self._post(ev, reads, writes)

    def dma(self, q, fn, reads=(), writes=()):
        waits = self._deps(q, reads, writes)
        j = self.dcnt[q]
        self.dcnt[q] += 1
        slot, gen = j % self.R, j // self.R
        key = ("d", q, slot)
        if gen > 0 and self.waited[q].get(key, 0) < 16 * gen:
            self.waited[q][key] = 16 * gen
            waits.append((key, 16 * gen))
        ev = (key, 16 * (gen + 1))
        self.ops[q].append((waits, fn, key, 16))
        self._post(ev, reads, writes)

    def barrier(self):
        evs = []
        for e in self.CE:
            if self.cnt[e] > 0:
                evs.append((self._ck(e), self.cnt[e]))
        for q, n in self.dcnt.items():
            for s in range(self.R):
                if n > s:
                    last = ((n - 1 - s) // self.R) + 1
                    evs.append((("d", q, s), 16 * last))
        for e in self.ALL:
            waits = []
            for k, v in evs:
                if self.waited[e].get(k, 0) >= v:
                    continue
                self.waited[e][k] = v
                waits.append((k, v))
            if waits:
                self.ops[e].append((waits, None, None, 0))
        self.lastw = {}
        self.readers = {}
        self._new_phase()

    def emit(self):
        nc = self.nc
        engs = {"pe": nc.tensor, "act": nc.scalar, "dve": nc.vector, "pool": nc.gpsimd, "sp": nc.sync}
        with nc.Block() as block:
            def run(name):
                eng = engs[name]
                for waits, fn, key, inc in self.ops[name]:
                    for k, v in waits:
                        eng.wait_ge(self.sems[k], v)
                    if fn is not None:
                        ins = fn()
                        ins.then_inc(self.sems[key], inc)

            @block.tensor
            def _(e):
                run("pe")

            @block.scalar
            def _(e):
                run("act")

            @block.vector
            def _(e):
                run("dve")

            @block.gpsimd
            def _(e):
                if self.pool_init is not None:
                    self.pool_init()
                run("pool")

            @block.sync
            def _(e):
                run("sp")


def build(debug=False, stop_after=None, dbg_names=()):
    nc = bass.Bass("TRN2", target_bir_lowering=False)
    stack = ExitStack()
    dbg_kind = "ExternalOutput" if debug else "Internal"

    def din(name, shape, dt=F32):
        return nc.dram_tensor(name, list(shape), dt, kind="ExternalInput").ap()

    def dscr(name, shape, dt, dbg=False):
        return nc.dram_tensor(name, list(shape), dt, kind=("ExternalOutput" if name in dbg_names else "Internal")).ap()

    x_in = din("x_in", [NTOK, D])
    s0_in = din("s0", [DEPTH, 2, NH, DK, DV])
    cT_in = din("cT", [128, 8, 2])
    w_ada = din("w_ada", [DEPTH, D, 6 * D])
    b_adaT = din("b_adaT", [DEPTH, 128, 48])
    g1T = din("g1T", [DEPTH, 128, 8])
    g2T = din("g2T", [DEPTH, 128, 8])
    w_in = din("w_in", [DEPTH, D, INW])
    decay = din("decay", [DEPTH, 16])
    w_ret_o = din("w_ret_o", [DEPTH, 2048, D])
    w_four = din("w_four", [DEPTH, D, D])
    w_out = din("w_out", [DEPTH, D, D])
    w_router = din("w_router", [DEPTH, D, NE])
    b_router = din("b_router", [DEPTH, NE])
    w1 = din("w1", [DEPTH, NE, D, 2 * DE])
    b1T = din("b1T", [DEPTH, NE, 128, 16])
    w2 = din("w2", [DEPTH, NE, DE, D])
    b2 = din("b2", [DEPTH, NE, D])
    final_g = din("final_g", [1, D])
    cst_in = din("cst", [128, C_N])
    rot_in = din("rot", [128, 2, NS])
    dftc_in = din("dftc", [128, 2, 512], BF16)
    dfts_in = din("dfts", [32, 128, 2, 32, 128], BF16)
    dftp_in = din("dftp", [2, 128, 2, 2, 128], BF16)

    y_out = nc.dram_tensor("y_out", [NTOK, D], F32, kind="ExternalOutput").ap()
    ns_out = nc.dram_tensor("ns_out", [4, DEPTH, 2, NH, DK, DV], F32, kind="ExternalOutput").ap()

    Xs = dscr("Xs", [NTOK, D], F32, True)
    QT = dscr("QT", [1024, NTOK], BF16, True)
    KT = dscr("KT", [1024, NTOK], BF16, True)
    Vd = dscr("Vd", [NTOK, 2048], BF16, True)
    GR = dscr("GR", [NTOK, 2048], BF16, True)
    XFT = dscr("XFT", [1024, NTOK], BF16, True)
    GAT = dscr("GAT", [1024, NTOK], BF16, True)
    GBT = dscr("GBT", [1024, NTOK], BF16, True)
    OGT = dscr("OGT", [2048, NTOK], BF16, True)
    UFT = dscr("UFT", [1024, NTOK], BF16, True)
    H2 = dscr("H2", [NTOK, D], BF16, True)
    XP = dscr("XP", [PT, D], BF16)
    Yd = dscr("Yd", [PT, D], F32)
    LG = dscr("LGd", [128, NT * 32], F32, True)
    SLd = dscr("SLd", [128, NT * 4], I32, True)

    ARENA = 48640
    arena = stack.enter_context(nc.sbuf_tensor("arena", [128, ARENA], F32))
    psum = stack.enter_context(nc.psum_tensor("ps", [128, 8, 512], F32))

    class Alloc:
        def __init__(self, base=0):
            self.off = base

        def get(self, shape, dt):
            n = int(np.prod(shape[1:]))
            esz = 2 if dt == BF16 else 4
            words = (n * esz + 3) // 4
            words = (words + 7) // 8 * 8
            a = arena[:, self.off:self.off + words]
            self.off += words
            assert self.off <= ARENA, ("SBUF overflow", self.off)
            if dt != F32:
                a = a.bitcast(dt)
            a = a[:, 0:n]
            if len(shape) == 3:
                a = a.rearrange("p (a b) -> p a b", a=shape[1])
            elif len(shape) == 4:
                a = a.rearrange("p (a b c) -> p a b c", a=shape[1], b=shape[2])
            return a

    def ps_f32(b, n=512):
        return psum[:, b, 0:n]

    def ps_bf(b):
        return psum[:, b, :].bitcast(BF16)

    P = Prog(nc, stack)
    REG = {}

    def _pool_init():
        REG["xp"] = nc.gpsimd.to_reg(PT - 1)
        REG["w"] = nc.gpsimd.to_reg(DEPTH * NE * 1024 - 1)
        REG["b1"] = nc.gpsimd.to_reg(DEPTH * NE * 128 - 1)
        REG["b2"] = nc.gpsimd.to_reg(DEPTH * NE - 1)
    P.pool_init = _pool_init
    V, A, G, T, SP = nc.vector, nc.scalar, nc.gpsimd, nc.tensor, nc.sync

    pa = Alloc(0)
    cst = pa.get([128, C_N], F32)
    identb = pa.get([128, 128], BF16)
    scT = pa.get([128, 8, 2], F32)
    modT = pa.get([128, 48, 2], F32)
    A1 = pa.get([128, 2, 8], F32)
    epsT = pa.get([128, 2], F32)
    PBASE = pa.off

    ident = cst[:, C_ID:C_ID + 128]
    ones = cst[:, C_ONES:C_ONES + 128]

    P.dma("sp", lambda: SP.dma_start(out=cst, in_=cst_in), writes=["cst"])
    P.dma("sp", lambda: SP.dma_start(out=scT, in_=cT_in), writes=["scT"])
    P.op("act", lambda: A.activation(out=scT, in_=scT, func=AF.Silu), reads=["scT"], writes=["scT"])
    P.op("dve", lambda: V.tensor_copy(out=identb, in_=ident), reads=["cst"], writes=["identb"])
    P.op("dve", lambda: V.memset(epsT[:, 0:1], EPS), writes=["eps0"])
    P.op("dve", lambda: V.memset(epsT[:, 1:2], GN_EPS), writes=["eps1"])
    P.barrier()
    if debug or stop_after:
        dbgo = nc.dram_tensor("dbgo", [128, 4096], F32, kind="ExternalOutput").ap()
    if stop_after == "init":
        P.dma("sp", lambda: SP.dma_start(out=dbgo[:, 0:C_N], in_=cst), writes=["dbgo"])
        P.dma("sp", lambda: SP.dma_start(out=dbgo[:, 2048:2064], in_=scT.rearrange("p a b -> p (a b)")), writes=["dbgo2"])
        P.barrier()
        P.emit()
        return nc, stack

    seqs = [(0, NS, 32, True, 0, None)] + [(NS + 256 * s, 256, 2, False, 1, s) for s in range(4)]

    def bcast_row(dst, vecT, tmpd, psb):
        for kc in range(8):
            P.op("dve", lambda kc=kc: V.tensor_scalar(out=tmpd, in0=ident, scalar1=vecT[:, kc:kc + 1],
                                                      scalar2=None, op0=ALU.mult),
                 reads=["vecsrc"], writes=["bc_tmp"])
            P.op("pe", lambda kc=kc: T.matmul(ps_f32(psb, 128), lhsT=ones, rhs=tmpd, start=True, stop=True),
                 reads=["bc_tmp"], writes=[("ps", psb)])
            P.op("act", lambda kc=kc: A.copy(out=dst[:, kc * 128:(kc + 1) * 128], in_=ps_f32(psb, 128)),
                 reads=[("ps", psb)], writes=["bc_dst"])

    def do_layer(l):
        Xsrc = x_in if l == 0 else Xs
        al = Alloc(PBASE)
        hT = al.get([128, 8, NTOK], BF16)
        xt = [al.get([128, D], F32) for _ in range(2)]
        xn = [al.get([128, D], F32) for _ in range(2)]
        junk = al.get([128, D], F32)
        ss = al.get([128, 4], F32)
        P1END = al.off
        wa = [al.get([128, 8, 512], F32) for _ in range(2)]
        bada = al.get([128, 48], F32)
        g1t = al.get([128, 8], F32)
        P.dma("sp", lambda: SP.dma_start(out=bada, in_=b_adaT[l]), writes=["bada"])
        P.dma("sp", lambda: SP.dma_start(out=g1t, in_=g1T[l]), writes=["g1t"])
        for nb in range(12):
            wb = wa[nb % 2]
            P.dma("sp", lambda nb=nb, wb=wb: SP.dma_start(
                out=wb, in_=w_ada[l].rearrange("(kc p) c -> p kc c", p=128)[:, :, nb * 512:(nb + 1) * 512]),
                writes=[("wa", nb % 2)])
            for sub in range(4):
                ch = nb * 4 + sub
                for kc in range(8):
                    P.op("pe", lambda kc=kc, sub=sub, ch=ch, wb=wb: T.matmul(
                        psum[:, 0, ch * 2:ch * 2 + 2], lhsT=wb[:, kc, sub * 128:(sub + 1) * 128],
                        rhs=scT[:, kc, :], start=(kc == 0), stop=(kc == 7)),
                        reads=[("wa", nb % 2)], writes=[("ps", 0)])
        P.op("dve", lambda: V.tensor_tensor(
            out=modT, in0=psum[:, 0, 0:96].rearrange("p (c j) -> p c j", j=2),
            in1=bada.unsqueeze(2).to_broadcast([128, 48, 2]), op=ALU.add),
            reads=[("ps", 0), "bada"], writes=["modT"])
        for j in range(2):
            P.op("dve", lambda j=j: V.scalar_tensor_tensor(
                out=A1[:, j, :], in0=modT[:, 8:16, j], scalar=1.0, in1=g1t, op0=ALU.add, op1=ALU.mult),
                reads=["modT", "g1t"], writes=["A1"])

        if stop_after == "p0":
            P.dma("sp", lambda: SP.dma_start(out=dbgo[:, 0:96], in_=modT.rearrange("p a b -> p (a b)")), reads=["modT"], writes=["dbgo"])
            P.dma("sp", lambda: SP.dma_start(out=dbgo[:, 128:144], in_=A1.rearrange("p a b -> p (a b)")), reads=["A1"], writes=["dbgo2"])
            P.barrier()
            return True
        for tt in range(NT):
            j = 0 if tt < 32 else 1
            xb, xnb = xt[tt % 2], xn[tt % 2]
            P.dma("sp", lambda tt=tt, xb=xb: SP.dma_start(out=xb, in_=Xsrc[tt * 128:(tt + 1) * 128, :]),
                  writes=[("xt", tt % 2)])
            P.op("act", lambda xb=xb: A.activation(out=junk, in_=xb, func=AF.Square, accum_out=ss[:, 0:1]),
                 reads=[("xt", tt % 2)], writes=["junk", "ss"])
            P.op("act", lambda: A.activation(out=ss[:, 1:2], in_=ss[:, 0:1], func=AF.Sqrt, scale=1.0 / D, bias=epsT[:, 0:1]),
                 reads=["ss"], writes=["ss1"])
            P.op("dve", lambda: V.reciprocal(out=ss[:, 2:3], in_=ss[:, 1:2]), reads=["ss1"], writes=["ss2"])
            P.op("dve", lambda xb=xb, xnb=xnb: V.tensor_scalar(out=xnb, in0=xb, scalar1=ss[:, 2:3], scalar2=None,
                                                               op0=ALU.mult),
                 reads=[("xt", tt % 2), "ss2"], writes=[("xn", tt % 2)])
            for half in range(2):
                bnk = 1 + half + 2 * (tt % 2)
                for q in range(4):
                    kc = half * 4 + q
                    P.op("pe", lambda kc=kc, q=q, bnk=bnk, xnb=xnb: T.matmul(
                        psum[:, bnk, q * 128:(q + 1) * 128], lhsT=xnb[:, kc * 128:(kc + 1) * 128], rhs=ident,
                        start=True, stop=True),
                        reads=[("xn", tt % 2)], writes=[("ps", bnk)])
                for q in range(4):
                    kc = half * 4 + q
                    P.op("act", lambda kc=kc, q=q, bnk=bnk, tt=tt, j=j: A.activation(
                        out=hT[:, kc, tt * 128:(tt + 1) * 128], in_=psum[:, bnk, q * 128:(q + 1) * 128],
                        func=AF.Identity, scale=A1[:, j, kc:kc + 1], bias=modT[:, kc, j:j + 1]),
                        reads=[("ps", bnk), "A1", "modT"], writes=[("hT", tt)])
        if stop_after == "p1":
            P.dma("sp", lambda: SP.dma_start(out=dbgo.bitcast(BF16)[:, 0:8 * 128].rearrange("p (k t) -> p k t", k=8), in_=hT[:, :, 0:128]), reads=[("hT", 0)], writes=["dbgo"])
            P.dma("sp", lambda: SP.dma_start(out=dbgo.bitcast(BF16)[:, 4096:4096 + 8 * 128].rearrange("p (k t) -> p k t", k=8), in_=hT[:, :, 4992:5120]), reads=[("hT", 39)], writes=["dbgo1"])
        P.barrier()
        if stop_after == "p1":
            return True

        al = Alloc(P1END)
        wbf = [al.get([128, 8, 512], BF16) for _ in range(2)]
        wrot = al.get([128, 8, 512], BF16)
        rot = al.get([128, 2, NS], F32)
        osb = [al.get([128, 4, 512], BF16) for _ in range(2)]
        t1 = [al.get([128, 512], F32) for _ in range(2)]
        t2 = [al.get([128, 512], F32) for _ in range(2)]
        P.dma("sp", lambda: SP.dma_start(out=rot, in_=rot_in), writes=["rot"])
        oi = 0
        for cb in range(18):
            wb = wbf[cb % 2]
            wtok = ("wbf", cb % 2)
            P.dma("pool", lambda cb=cb, wb=wb: G.dma_start(
                out=wb, in_=w_in[l].rearrange("(kc p) c -> p kc c", p=128)[:, :, cb * 512:(cb + 1) * 512]),
                writes=[wtok])
            kind = ("q", "q", "k", "k", "v", "v", "v", "v", "g", "g", "g", "g", "x", "x", "a", "a", "b", "b")[cb]
            if kind in ("q", "k"):
                wv = wb.rearrange("p k (h two f) -> p k h two f", two=2, f=64)
                rv = wrot.rearrange("p k (h two f) -> p k h two f", two=2, f=64)
                P.op("act", lambda wv=wv, rv=rv: A.mul(out=rv[:, :, :, 0, :], in_=wv[:, :, :, 1, :], mul=-1.0),
                     reads=[wtok], writes=["wrot"])
                P.op("dve", lambda wv=wv, rv=rv: V.tensor_copy(out=rv[:, :, :, 1, :], in_=wv[:, :, :, 0, :]),
                     reads=[wtok], writes=["wrot2"])
            for tg in range(10):
                ob = osb[oi % 2]
                otok = ("osb", oi % 2)
                oi += 1
                tsl = slice(tg * 512, (tg + 1) * 512)
                for sub in range(4):
                    bA = (sub % 2) * 2
                    bB = bA + 1
                    if kind in ("v", "g"):
                        tk = tg * 512 + sub * 128
                        for kc in range(8):
                            P.op("pe", lambda kc=kc, bA=bA, tk=tk, wb=wb: T.matmul(
                                ps_f32(bA), lhsT=hT[:, kc, tk:tk + 128], rhs=wb[:, kc, :],
                                start=(kc == 0), stop=(kc == 7)), reads=[wtok], writes=[("ps", bA)])
                        fn = AF.Copy if kind == "v" else AF.Silu
                        P.op("act", lambda bA=bA, ob=ob, sub=sub, fn=fn: A.activation(
                            out=ob[:, sub, :], in_=ps_f32(bA), func=fn),
                            reads=[("ps", bA)], writes=[otok])
                    else:
                        csl = slice(sub * 128, (sub + 1) * 128)
                        for kc in range(8):
                            P.op("pe", lambda kc=kc, bA=bA, wb=wb, csl=csl, tsl=tsl: T.matmul(
                                ps_f32(bA), lhsT=wb[:, kc, csl], rhs=hT[:, kc, tsl],
                                start=(kc == 0), stop=(kc == 7)), reads=[wtok], writes=[("ps", bA)])
                        sc = (DK ** -0.5) if kind == "q" else 1.0
                        if kind in ("q", "k") and tg < 8:
                            for kc in range(8):
                                P.op("pe", lambda kc=kc, bB=bB, csl=csl, tsl=tsl: T.matmul(
                                    ps_f32(bB), lhsT=wrot[:, kc, csl], rhs=hT[:, kc, tsl],
                                    start=(kc == 0), stop=(kc == 7)), reads=["wrot", "wrot2"], writes=[("ps", bB)])
                            ta, tb = t1[sub % 2], t2[sub % 2]
                            P.op("dve", lambda bA=bA, ta=ta, tsl=tsl, sc=sc: V.scalar_tensor_tensor(
                                out=ta, in0=ps_f32(bA), scalar=sc, in1=rot[:, 0, tsl], op0=ALU.mult, op1=ALU.mult),
                                reads=[("ps", bA), "rot"], writes=[("t1", sub % 2)])
                            P.op("dve", lambda bB=bB, tb=tb, tsl=tsl, sc=sc: V.scalar_tensor_tensor(
                                out=tb, in0=ps_f32(bB), scalar=sc, in1=rot[:, 1, tsl], op0=ALU.mult, op1=ALU.mult),
                                reads=[("ps", bB), "rot"], writes=[("t2", sub % 2)])
                            P.op("pool", lambda ta=ta, tb=tb, ob=ob, sub=sub: G.tensor_tensor(
                                out=ob[:, sub, :], in0=ta, in1=tb, op=ALU.add),
                                reads=[("t1", sub % 2), ("t2", sub % 2)], writes=[otok])
                        else:
                            if kind in ("a", "b"):
                                P.op("act", lambda bA=bA, ob=ob, sub=sub: A.activation(
                                    out=ob[:, sub, :], in_=ps_f32(bA), func=AF.Sigmoid),
                                    reads=[("ps", bA)], writes=[otok])
                            else:
                                P.op("act", lambda bA=bA, ob=ob, sub=sub, sc=sc: A.activation(
                                    out=ob[:, sub, :], in_=ps_f32(bA), func=AF.Copy, scale=sc),
                                    reads=[("ps", bA)], writes=[otok])
                if kind in ("v", "g"):
                    dst = (Vd if kind == "v" else GR)
                    c0 = (cb - (4 if kind == "v" else 8)) * 512
                    P.dma("sp", lambda dst=dst, c0=c0, tg=tg, ob=ob: SP.dma_start(
                        out=dst[tg * 512:(tg + 1) * 512, c0:c0 + 512].rearrange("(s p) c -> p s c", p=128), in_=ob),
                        reads=[otok], writes=[("u", kind)])
                else:
                    dst, cb0 = {"q": (QT, 0), "k": (KT, 2), "x": (XFT, 12), "a": (GAT, 14), "b": (GBT, 16)}[kind]
                    r0 = (cb - cb0) * 512
                    P.dma("sp", lambda dst=dst, r0=r0, tsl=tsl, ob=ob: SP.dma_start(
                        out=dst[r0:r0 + 512, tsl].rearrange("(s p) t -> p s t", p=128), in_=ob),
                        reads=[otok], writes=[("u", kind)])
        P.barrier()
        if stop_after == "p2a":
            return True

        al = Alloc(PBASE)
        dl = al.get([128, 16], F32)
        MC = al.get([128, 8, 128], F32)
        QDF = al.get([128, 8, 128], F32)
        QDB = al.get([128, 8, 128], F32)
        KD = al.get([128, 2, 8], F32)
        CDEC = al.get([128, 16], F32)
        mtmp = al.get([128, 2, 128], F32)
        QTh = al.get([128, NS], BF16)
        KTh = al.get([128, NS], BF16)
        Vh = al.get([128, 32, 256], BF16)
        GRh = al.get([128, 32, 256], BF16)
        Kf = al.get([128, 32, 128], BF16)
        Kb = al.get([128, 32, 128], BF16)
        QfT = al.get([128, 32, 128], BF16)
        QbT = al.get([128, 32, 128], BF16)
        SbAll = al.get([128, 33, 256], BF16)
        OGh = al.get([128, 2, NS], BF16)
        Sf32 = al.get([128, 256], F32)
        Sb32 = al.get([128, 256], F32)
        Sfbf = [al.get([128, 256], BF16) for _ in range(2)]
        scm = [al.get([128, 128], BF16) for _ in range(2)]
        onb = [al.get([128, 256], F32) for _ in range(2)]
        ogb = [al.get([128, 256], BF16) for _ in range(2)]
        st6 = al.get([128, 8], F32)
        mv = al.get([128, 4], F32)

        P.dma("sp", lambda: SP.dma_start(out=dl, in_=decay[l:l + 1, :].to_broadcast([128, 16])), writes=["dl"])
        P.op("act", lambda: A.activation(out=dl, in_=dl, func=AF.Exp, scale=-1.0), reads=["dl"], writes=["dl"])
        P.op("act", lambda: A.activation(out=dl, in_=dl, func=AF.Ln, bias=1.0), reads=["dl"], writes=["dl"])
        P.op("dve", lambda: V.tensor_scalar(out=dl, in0=dl, scalar1=-1.0, scalar2=None, op0=ALU.mult),
             reads=["dl"], writes=["dl"])
        P.op("act", lambda: A.activation(out=CDEC, in_=dl, func=AF.Exp, scale=128.0), reads=["dl"], writes=["CDEC"])
        for h in range(NH):
            lf = dl[:, h:h + 1]
            lb = dl[:, 8 + h:9 + h]
            P.op("act", lambda lf=lf: A.activation(out=mtmp[:, 0, :], in_=cst[:, C_DF:C_DF + 128], func=AF.Exp, scale=lf),
                 reads=["dl"], writes=["mtmp0"])
            P.op("act", lambda lb=lb: A.activation(out=mtmp[:, 1, :], in_=cst[:, C_DB:C_DB + 128], func=AF.Exp, scale=lb),
                 reads=["dl"], writes=["mtmp1"])
            P.op("dve", lambda: V.tensor_tensor(out=mtmp[:, 0, :], in0=mtmp[:, 0, :], in1=cst[:, C_TF:C_TF + 128], op=ALU.mult),
                 reads=["mtmp0"], writes=["mtmp0"])
            P.op("dve", lambda: V.tensor_tensor(out=mtmp[:, 1, :], in0=mtmp[:, 1, :], in1=cst[:, C_TB:C_TB + 128], op=ALU.mult),
                 reads=["mtmp1"], writes=["mtmp1"])
            P.op("dve", lambda h=h: V.tensor_tensor(out=MC[:, h, :], in0=mtmp[:, 0, :], in1=mtmp[:, 1, :], op=ALU.add),
                 reads=["mtmp0", "mtmp1"], writes=["MC"])
            P.op("act", lambda h=h, lf=lf: A.activation(out=QDF[:, h, :], in_=cst[:, C_IP1:C_IP1 + 128], func=AF.Exp, scale=lf),
                 reads=["dl"], writes=["QD"])
            P.op("act", lambda h=h, lb=lb: A.activation(out=QDB[:, h, :], in_=cst[:, C_IB:C_IB + 128], func=AF.Exp, scale=lb),
                 reads=["dl"], writes=["QD"])
            P.op("act", lambda h=h, lf=lf: A.activation(out=KD[:, 0, h:h + 1], in_=cst[:, C_KF:C_KF + 1], func=AF.Exp, scale=lf),
                 reads=["dl"], writes=["KD"])
            P.op("act", lambda h=h, lb=lb: A.activation(out=KD[:, 1, h:h + 1], in_=cst[:, C_KB:C_KB + 1], func=AF.Exp, scale=lb),
                 reads=["dl"], writes=["KD"])

        for (r0, N, nch, latent, jt, pidx) in seqs:
            for h in range(NH):
                P.dma("sp", lambda h=h, r0=r0, N=N: SP.dma_start(out=QTh[:, 0:N], in_=QT[h * 128:(h + 1) * 128, r0:r0 + N]),
                      writes=["QTh"])
                P.dma("sp", lambda h=h, r0=r0, N=N: SP.dma_start(out=KTh[:, 0:N], in_=KT[h * 128:(h + 1) * 128, r0:r0 + N]),
                      writes=["KTh"])
                P.dma("sp", lambda h=h, r0=r0, N=N, nch=nch: SP.dma_start(
                    out=Vh[:, 0:nch, :], in_=Vd[r0:r0 + N, h * 256:(h + 1) * 256].rearrange("(c p) v -> p c v", p=128)),
                    writes=["Vh"])
                P.dma("sp", lambda h=h, r0=r0, N=N, nch=nch: SP.dma_start(
                    out=GRh[:, 0:nch, :], in_=GR[r0:r0 + N, h * 256:(h + 1) * 256].rearrange("(c p) v -> p c v", p=128)),
                    writes=["GRh"])
                if latent:
                    P.dma("sp", lambda h=h: SP.dma_start(out=Sf32, in_=s0_in[l, 0, h]), writes=["Sf32"])
                    P.dma("sp", lambda h=h: SP.dma_start(out=Sb32, in_=s0_in[l, 1, h]), writes=["Sb32"])
                else:
                    P.op("pool", lambda: G.memset(Sf32, 0.0), writes=["Sf32"])
                    P.op("pool", lambda: G.memset(Sb32, 0.0), writes=["Sb32"])
                for c0 in range(0, nch, 8):
                    n8 = min(8, nch - c0)
                    bnk = 4 + (c0 // 8) % 2
                    for cc in range(n8):
                        c = c0 + cc
                        P.op("pe", lambda c=c, cc=cc, bnk=bnk: T.transpose(
                            ps_bf(bnk)[:, cc * 128:(cc + 1) * 128], KTh[:, c * 128:(c + 1) * 128], identb),
                            reads=["KTh"], writes=[("ps", bnk)])
                    P.op("dve", lambda c0=c0, n8=n8, bnk=bnk, h=h: V.tensor_scalar(
                        out=Kf[:, c0:c0 + n8, :], in0=ps_bf(bnk)[:, 0:n8 * 128].rearrange("p (c d) -> p c d", d=128),
                        scalar1=KD[:, 0, h:h + 1], scalar2=None, op0=ALU.mult),
                        reads=[("ps", bnk), "KD"], writes=["Kf"])
                    P.op("act", lambda c0=c0, n8=n8, bnk=bnk, h=h: A.activation(
                        out=Kb[:, c0:c0 + n8, :], in_=ps_bf(bnk)[:, 0:n8 * 128].rearrange("p (c d) -> p c d", d=128),
                        func=AF.Copy, scale=KD[:, 1, h:h + 1]),
                        reads=[("ps", bnk), "KD", "Kf"], writes=["Kb"])
                P.op("dve", lambda h=h, nch=nch, N=N: V.tensor_tensor(
                    out=QfT[:, 0:nch, :], in0=QTh[:, 0:N].rearrange("p (c i) -> p c i", i=128),
                    in1=QDF[:, h:h + 1, :].to_broadcast([128, nch, 128]), op=ALU.mult),
                    reads=["QTh", "QD"], writes=["QfT"])
                P.op("pool", lambda h=h, nch=nch, N=N: G.tensor_tensor(
                    out=QbT[:, 0:nch, :], in0=QTh[:, 0:N].rearrange("p (c i) -> p c i", i=128),
                    in1=QDB[:, h:h + 1, :].to_broadcast([128, nch, 128]), op=ALU.mult),
                    reads=["QTh", "QD"], writes=["QbT"])
                P.op("act", lambda nch=nch: A.copy(out=SbAll[:, nch, :], in_=Sb32), reads=["Sb32"], writes=[("SbAll", nch)])
                for c in range(nch - 1, -1, -1):
                    bnk = 6 + (c % 2)
                    P.op("pe", lambda c=c, bnk=bnk: T.matmul(ps_f32(bnk, 256), lhsT=Kb[:, c, :], rhs=Vh[:, c, :],
                                                             start=True, stop=True),
                         reads=["Kb", "Vh"], writes=[("ps", bnk)])
                    P.op("dve", lambda bnk=bnk, h=h: V.scalar_tensor_tensor(
                        out=Sb32, in0=Sb32, scalar=CDEC[:, 8 + h:9 + h], in1=ps_f32(bnk, 256), op0=ALU.mult, op1=ALU.add),
                        reads=[("ps", bnk), "Sb32", "CDEC"], writes=["Sb32"])
                    P.op("act", lambda c=c: A.copy(out=SbAll[:, c, :], in_=Sb32), reads=["Sb32"], writes=[("SbAll", c)])
                if not latent:
                    P.dma("sp", lambda h=h, pidx=pidx: SP.dma_start(out=ns_out[pidx, l, 1, h], in_=Sb32),
                          reads=["Sb32"], writes=[("ns", pidx, 1, h)])
                P.op("act", lambda: A.copy(out=Sfbf[0], in_=Sf32), reads=["Sf32"], writes=[("Sfbf", 0)])
                for c in range(nch):
                    csl = slice(c * 128, (c + 1) * 128)
                    b_sc = c % 2
                    b_o = 2 + c % 2
                    sm, onx, ogx = scm[c % 2], onb[c % 2], ogb[c % 2]
                    P.op("pe", lambda csl=csl, b_sc=b_sc: T.matmul(ps_f32(b_sc, 128), lhsT=KTh[:, csl], rhs=QTh[:, csl],
                                                                 start=True, stop=True),
                         reads=["KTh", "QTh"], writes=[("ps", b_sc)])
                    P.op("dve", lambda b_sc=b_sc, sm=sm, h=h: V.tensor_tensor(out=sm, in0=ps_f32(b_sc, 128), in1=MC[:, h, :],
                                                                              op=ALU.mult),
                         reads=[("ps", b_sc), "MC"], writes=[("scm", c % 2)])
                    P.op("pe", lambda c=c, b_o=b_o, sm=sm: T.matmul(ps_f32(b_o, 256), lhsT=sm, rhs=Vh[:, c, :],
                                                                    start=True, stop=False),
                         reads=[("scm", c % 2), "Vh"], writes=[("ps", b_o)])
                    P.op("pe", lambda c=c, b_o=b_o: T.matmul(ps_f32(b_o, 256), lhsT=QfT[:, c, :], rhs=Sfbf[c % 2],
                                                             start=False, stop=False),
                         reads=["QfT", ("Sfbf", c % 2)], writes=[("ps", b_o)])
                    P.op("pe", lambda c=c, b_o=b_o: T.matmul(ps_f32(b_o, 256), lhsT=QbT[:, c, :], rhs=SbAll[:, c + 1, :],
                                                             start=False, stop=True),
                         reads=["QbT", ("SbAll", c + 1)], writes=[("ps", b_o)])
                    P.op("dve", lambda b_o=b_o: V.bn_stats(out=st6[:, 0:6], in_=ps_f32(b_o, 256)),
                         reads=[("ps", b_o)], writes=["st6"])
                    P.op("dve", lambda: V.bn_aggr(out=mv[:, 0:2], in_=st6[:, 0:6]), reads=["st6"], writes=["mv"])
                    P.op("act", lambda: A.activation(out=mv[:, 3:4], in_=mv[:, 1:2], func=AF.Sqrt, bias=epsT[:, 1:2]),
                         reads=["mv"], writes=["mv3"])
                    P.op("dve", lambda: V.reciprocal(out=mv[:, 2:3], in_=mv[:, 3:4]), reads=["mv3"], writes=["mv2"])
                    P.op("dve", lambda b_o=b_o, onx=onx: V.tensor_scalar(out=onx, in0=ps_f32(b_o, 256), scalar1=mv[:, 0:1],
                                                                         scalar2=mv[:, 2:3], op0=ALU.subtract, op1=ALU.mult),
                         reads=[("ps", b_o), "mv", "mv2"], writes=[("on", c % 2)])
                    P.op("pool", lambda c=c, onx=onx, ogx=ogx: G.tensor_tensor(out=ogx, in0=onx, in1=GRh[:, c, :], op=ALU.mult),
                         reads=[("on", c % 2), "GRh"], writes=[("og", c % 2)])
                    b_t = 4 + c % 2
                    for hf in range(2):
                        P.op("pe", lambda hf=hf, b_t=b_t, ogx=ogx: T.transpose(
                            ps_bf(b_t)[:, hf * 128:(hf + 1) * 128], ogx[:, hf * 128:(hf + 1) * 128], identb),
                            reads=[("og", c % 2)], writes=[("ps", b_t)])
                    P.op("act", lambda b_t=b_t, csl=csl: A.copy(
                        out=OGh[:, :, csl], in_=ps_bf(b_t)[:, 0:256].rearrange("p (a t) -> p a t", a=2)),
                        reads=[("ps", b_t)], writes=["OGh"])
                    b_s = 6 + c % 2
                    P.op("pe", lambda c=c, b_s=b_s: T.matmul(ps_f32(b_s, 256), lhsT=Kf[:, c, :], rhs=Vh[:, c, :],
                                                             start=True, stop=True),
                         reads=["Kf", "Vh"], writes=[("ps", b_s)])
                    P.op("dve", lambda b_s=b_s, h=h: V.scalar_tensor_tensor(
                        out=Sf32, in0=Sf32, scalar=CDEC[:, h:h + 1], in1=ps_f32(b_s, 256), op0=ALU.mult, op1=ALU.add),
                        reads=[("ps", b_s), "Sf32", "CDEC"], writes=["Sf32"])
                    P.op("act", lambda c=c: A.copy(out=Sfbf[(c + 1) % 2], in_=Sf32),
                         reads=["Sf32"], writes=[("Sfbf", (c + 1) % 2)])
                if not latent:
                    P.dma("sp", lambda h=h, pidx=pidx: SP.dma_start(out=ns_out[pidx, l, 0, h], in_=Sf32),
                          reads=["Sf32"], writes=[("ns", pidx, 0, h)])
                P.dma("sp", lambda h=h, r0=r0, N=N: SP.dma_start(
                    out=OGT[h * 256:(h + 1) * 256, r0:r0 + N].rearrange("(a p) t -> p a t", p=128), in_=OGh[:, :, 0:N]),
                    reads=["OGh"], writes=["OGT"])
        P.barrier()
        if stop_after == "p2b":
            return True

        al = Alloc(PBASE)
        dftc = al.get([128, 2, 512], BF16)
        dftp = al.get([128, 2, 2, 2, 128], BF16) if False else None
        dftp_t = [al.get([128, 2, 2, 128], BF16) for _ in range(2)]
        XFp = al.get([128, 4, NS], BF16)
        PQ = al.get([128, 2, 32, 512], BF16)
        Dp = [al.get([128, 2, 32, 128], BF16) for _ in range(2)]
        ufb = [al.get([128, 512], BF16) for _ in range(2)]
        P.dma("sp", lambda: SP.dma_start(out=dftc, in_=dftc_in), writes=["dftc"])
        for mt in range(2):
            P.dma("sp", lambda mt=mt: SP.dma_start(out=dftp_t[mt], in_=dftp_in[mt]), writes=["dftp"])
        di = 0
        for (r0, N, nch, latent, jt, pidx) in seqs:
            for pair in range(2):
                P.dma("sp", lambda pair=pair, r0=r0, N=N: SP.dma_start(
                    out=XFp[:, :, 0:N], in_=XFT[pair * 512:(pair + 1) * 512, r0:r0 + N].rearrange("(a p) t -> p a t", p=128)),
                    writes=["XFp"])
                for c in range(nch):
                    for gg in range(2):
                        bnk = (c * 2 + gg) % 2
                        for k2 in range(2):
                            P.op("pe", lambda c=c, gg=gg, k2=k2, bnk=bnk: T.matmul(
                                ps_f32(bnk), lhsT=XFp[:, gg * 2 + k2, c * 128:(c + 1) * 128], rhs=dftc[:, k2, :],
                                start=(k2 == 0), stop=(k2 == 1)), reads=["XFp", "dftc"], writes=[("ps", bnk)])
                        P.op("act", lambda c=c, gg=gg, bnk=bnk: A.copy(
                            out=PQ[:, :, c, gg * 256:(gg + 1) * 256], in_=ps_f32(bnk).rearrange("p (a f) -> p a f", a=2)),
                            reads=[("ps", bnk)], writes=["PQ"])
                for mt in range(nch):
                    if latent:
                        dp = Dp[di % 2]
                        dtok = ("Dp", di % 2)
                        di += 1
                        P.dma("sp", lambda mt=mt, dp=dp: SP.dma_start(out=dp, in_=dfts_in[mt]), writes=[dtok])
                    else:
                        dp = dftp_t[mt]
                        dtok = "dftp"
                    bnk = 2 + mt % 2
                    n_mm = 2 * nch
                    i_mm = 0
                    for cs in range(2):
                        for ncn in range(nch):
                            P.op("pe", lambda cs=cs, ncn=ncn, bnk=bnk, dp=dp, i_mm=i_mm, n_mm=n_mm: T.matmul(
                                ps_f32(bnk), lhsT=dp[:, cs, ncn, :], rhs=PQ[:, cs, ncn, :],
                                start=(i_mm == 0), stop=(i_mm == n_mm - 1)), reads=[dtok, "PQ"], writes=[("ps", bnk)])
                            i_mm += 1
                    ub = ufb[mt % 2]
                    P.op("act", lambda bnk=bnk, ub=ub: A.copy(out=ub, in_=ps_f32(bnk)), reads=[("ps", bnk)],
                         writes=[("ufb", mt % 2)])
                    bt = 4 + mt % 2
                    for q4 in range(4):
                        P.op("pe", lambda q4=q4, bt=bt, ub=ub: T.transpose(
                            ps_bf(bt)[:, q4 * 128:(q4 + 1) * 128], ub[:, q4 * 128:(q4 + 1) * 128], identb),
                            reads=[("ufb", mt % 2)], writes=[("ps", bt)])
                    P.op("dve", lambda mt=mt, bt=bt: V.tensor_copy(
                        out=XFp[:, :, mt * 128:(mt + 1) * 128], in_=ps_bf(bt)[:, 0:512].rearrange("p (a t) -> p a t", a=4)),
                        reads=[("ps", bt)], writes=["XFp"])
                P.dma("sp", lambda pair=pair, r0=r0, N=N: SP.dma_start(
                    out=UFT[pair * 512:(pair + 1) * 512, r0:r0 + N].rearrange("(a p) t -> p a t", p=128), in_=XFp[:, :, 0:N]),
                    reads=["XFp"], writes=["UFT"])
        P.barrier()
        if stop_after == "p2c":
            return True

        al = Alloc(PBASE)
        wro = al.get([128, 16, D], BF16)
        wfo = al.get([128, 8, D], BF16)
        wou = al.get([128, 8, D], BF16)
        wr = al.get([128, 8, NE], F32)
        brt = al.get([128, NE], F32)
        g2t = al.get([128, 8], F32)
        vtmp = al.get([128, 2, 8], F32)
        bct = al.get([128, 128], F32)
        G1 = [al.get([128, D], F32) for _ in range(2)]
        A2 = [al.get([128, D], F32) for _ in range(2)]
        B2 = [al.get([128, D], F32) for _ in range(2)]
        OGt = al.get([128, 16, 512], BF16)
        UFt = al.get([128, 8, 512], BF16)
        GAt = al.get([128, 8, 512], BF16)
        GBt = al.get([128, 8, 512], BF16)
        ta_ = [al.get([128, 512], F32)] * 2
        tb_ = [al.get([128, 512], F32)] * 2
        mT = al.get([128, 8, 512], BF16)
        xt2 = [al.get([128, D], F32) for _ in range(2)]
        yt2 = al.get([128, D], F32)
        h2 = al.get([128, D], F32)
        h2b = [al.get([128, D], BF16)] * 2
        h2T = al.get([128, 8, 128], F32)
        junk2 = yt2
        ss2 = al.get([128, 4], F32)
        Lsb = al.get([128, NT, NE], F32)
        P.dma("pool", lambda: G.dma_start(out=wro, in_=w_ret_o[l].rearrange("(kc p) c -> p kc c", p=128)), writes=["wro"])
        P.dma("pool", lambda: G.dma_start(out=wfo, in_=w_four[l].rearrange("(kc p) c -> p kc c", p=128)), writes=["wfo"])
        P.dma("pool", lambda: G.dma_start(out=wou, in_=w_out[l].rearrange("(kc p) c -> p kc c", p=128)), writes=["wou"])
        P.dma("sp", lambda: SP.dma_start(out=wr, in_=w_router[l].rearrange("(kc p) c -> p kc c", p=128)), writes=["wr"])
        P.dma("sp", lambda: SP.dma_start(out=brt, in_=b_router[l:l + 1, :].to_broadcast([128, NE])), writes=["brt"])
        P.dma("sp", lambda: SP.dma_start(out=g2t, in_=g2T[l]), writes=["g2t"])
        for j in range(2):
            P.op("dve", lambda j=j: V.tensor_copy(out=vtmp[:, 0, :], in_=modT[:, 16:24, j]), reads=["bc_dst"], writes=["vecsrc"])
            bcast_row(G1[j], vtmp[:, 0, :], bct, 7)
            P.op("dve", lambda j=j: V.scalar_tensor_tensor(out=vtmp[:, 0, :], in0=modT[:, 32:40, j], scalar=1.0, in1=g2t,
                                                           op0=ALU.add, op1=ALU.mult), reads=["bc_dst", "g2t"], writes=["vecsrc"])
            bcast_row(A2[j], vtmp[:, 0, :], bct, 7)
            P.op("dve", lambda j=j: V.tensor_copy(out=vtmp[:, 0, :], in_=modT[:, 24:32, j]), reads=["bc_dst"], writes=["vecsrc"])
            bcast_row(B2[j], vtmp[:, 0, :], bct, 7)
        for tg in range(10):
            j = 0 if tg < 8 else 1
            tsl = slice(tg * 512, (tg + 1) * 512)
            P.dma("sp", lambda tsl=tsl: SP.dma_start(out=OGt, in_=OGT[:, tsl].rearrange("(kc p) t -> p kc t", p=128)), writes=["OGt"])
            P.dma("sp", lambda tsl=tsl: SP.dma_start(out=UFt, in_=UFT[:, tsl].rearrange("(kc p) t -> p kc t", p=128)), writes=["UFt"])
            P.dma("sp", lambda tsl=tsl: SP.dma_start(out=GAt, in_=GAT[:, tsl].rearrange("(kc p) t -> p kc t", p=128)), writes=["GAt"])
            P.dma("sp", lambda tsl=tsl: SP.dma_start(out=GBt, in_=GBT[:, tsl].rearrange("(kc p) t -> p kc t", p=128)), writes=["GBt"])
            for dc in range(8):
                dsl = slice(dc * 128, (dc + 1) * 128)
                bA, bB = (dc % 2) * 2, (dc % 2) * 2 + 1
                for kc in range(16):
                    P.op("pe", lambda kc=kc, bA=bA, dsl=dsl: T.matmul(ps_f32(bA), lhsT=wro[:, kc, dsl], rhs=OGt[:, kc, :],
                                                                      start=(kc == 0), stop=(kc == 15)),
                         reads=["wro", "OGt"], writes=[("ps", bA)])
                for kc in range(8):
                    P.op("pe", lambda kc=kc, bB=bB, dsl=dsl: T.matmul(ps_f32(bB), lhsT=wfo[:, kc, dsl], rhs=UFt[:, kc, :],
                                                                      start=(kc == 0), stop=(kc == 7)),
                         reads=["wfo", "UFt"], writes=[("ps", bB)])
                ta, tb = ta_[dc % 2], tb_[dc % 2]
                P.op("dve", lambda bA=bA, ta=ta, dc=dc: V.tensor_tensor(out=ta, in0=ps_f32(bA), in1=GAt[:, dc, :], op=ALU.mult),
                     reads=[("ps", bA), "GAt"], writes=["ta"])
                P.op("dve", lambda bB=bB, tb=tb, dc=dc: V.tensor_tensor(out=tb, in0=ps_f32(bB), in1=GBt[:, dc, :], op=ALU.mult),
                     reads=[("ps", bB), "GBt"], writes=["tb"])
                P.op("pool", lambda ta=ta, tb=tb, dc=dc: G.tensor_tensor(out=mT[:, dc, :], in0=ta, in1=tb, op=ALU.add),
                     reads=["ta", "tb"], writes=["mT"])
            for ts in range(4):
                tt = tg * 4 + ts
                xb = xt2[tt % 2]
                hb = h2b[tt % 2]
                P.dma("sp", lambda tt=tt, xb=xb: SP.dma_start(out=xb, in_=Xsrc[tt * 128:(tt + 1) * 128, :]),
                      reads=[("Xs", tt)], writes=[("xt2", tt % 2)])
                for hf in range(2):
                    for kc in range(8):
                        P.op("pe", lambda kc=kc, hf=hf, ts=ts: T.matmul(
                            ps_f32(4 + hf), lhsT=mT[:, kc, ts * 128:(ts + 1) * 128], rhs=wou[:, kc, hf * 512:(hf + 1) * 512],
                            start=(kc == 0), stop=(kc == 7)), reads=["mT", "wou"], writes=[("ps", 4 + hf)])
                P.op("dve", lambda j=j: V.tensor_tensor(out=yt2.rearrange("p (a f) -> p a f", a=2), in0=psum[:, 4:6, :],
                                                        in1=G1[j].rearrange("p (a f) -> p a f", a=2), op=ALU.mult),
                     reads=[("ps", 4), ("ps", 5), "bc_dst"], writes=["yt2"])
                P.op("pool", lambda xb=xb: G.tensor_tensor(out=xb, in0=xb, in1=yt2, op=ALU.add),
                     reads=["yt2", ("xt2", tt % 2)], writes=[("xt2", tt % 2)])
                P.dma("sp", lambda tt=tt, xb=xb: SP.dma_start(out=Xs[tt * 128:(tt + 1) * 128, :], in_=xb),
                      reads=[("xt2", tt % 2)], writes=[("Xs", tt)])
                P.op("act", lambda xb=xb: A.activation(out=junk2, in_=xb, func=AF.Square, accum_out=ss2[:, 0:1]),
                     reads=[("xt2", tt % 2)], writes=["yt2", "ss2"])
                P.op("act", lambda: A.activation(out=ss2[:, 1:2], in_=ss2[:, 0:1], func=AF.Sqrt, scale=1.0 / D, bias=epsT[:, 0:1]),
                     reads=["ss2"], writes=["ss2b"])
                P.op("dve", lambda: V.reciprocal(out=ss2[:, 2:3], in_=ss2[:, 1:2]), reads=["ss2b"], writes=["ss2c"])
                P.op("dve", lambda xb=xb, j=j: V.scalar_tensor_tensor(out=h2, in0=xb, scalar=ss2[:, 2:3], in1=A2[j],
                                                                      op0=ALU.mult, op1=ALU.mult),
                     reads=[("xt2", tt % 2), "ss2c", "bc_dst"], writes=["h2"])
                P.op("pool", lambda j=j: G.tensor_tensor(out=h2, in0=h2, in1=B2[j], op=ALU.add),
                     reads=["h2", "bc_dst"], writes=["h2"])
                P.op("act", lambda hb=hb: A.copy(out=hb, in_=h2), reads=["h2"], writes=["h2b"])
                P.dma("sp", lambda tt=tt, hb=hb: SP.dma_start(out=H2[tt * 128:(tt + 1) * 128, :], in_=hb),
                      reads=["h2b"], writes=["H2"])
                for kc in range(8):
                    P.op("pe", lambda kc=kc: T.matmul(psum[:, 6 + kc // 4, (kc % 4) * 128:(kc % 4 + 1) * 128],
                                                      lhsT=h2[:, kc * 128:(kc + 1) * 128], rhs=ident, start=True, stop=True),
                         reads=["h2"], writes=[("ps", 6 + kc // 4)])
                P.op("dve", lambda: V.tensor_copy(out=h2T.rearrange("p (a k) t -> p a (k t)", a=2), in_=psum[:, 6:8, :]),
                     reads=[("ps", 6), ("ps", 7)], writes=["h2T"])
                for kc in range(8):
                    P.op("pe", lambda kc=kc: T.matmul(psum[:, 6, 0:NE], lhsT=h2T[:, kc, :], rhs=wr[:, kc, :],
                                                      start=(kc == 0), stop=(kc == 7)),
                         reads=["h2T", "wr"], writes=[("ps", 6)])
                P.op("dve", lambda tt=tt: V.tensor_tensor(out=Lsb[:, tt, :], in0=psum[:, 6, 0:NE], in1=brt, op=ALU.add),
                     reads=[("ps", 6), "brt"], writes=["Lsb"])
        P.dma("sp", lambda: SP.dma_start(out=LG, in_=Lsb.rearrange("p a b -> p (a b)")), reads=["Lsb"], writes=["LG"])
        P.barrier()
        if stop_after == "p2d":
            return True

        al = Alloc(PBASE)
        WK = al.get([128, NT, 4], F32)
        SLI = al.get([128, NT, 4], I32)
        BLKI = al.get([128, NBLK], I32)
        IDXW = al.get([128, NBLK, 8], I32)
        IDXB1 = al.get([128, NBLK], I32)
        IDXB2 = al.get([128, NBLK], I32)
        R3END = al.off
        idxf = al.get([128, NBLK, 8], F32)
        idxg = al.get([128, NBLK], F32)
        Lr = al.get([128, NT, NE], F32)
        v8 = al.get([128, NT, 8], F32)
        i8 = al.get([128, NT, 8], U32)
        i8f = al.get([128, NT, 8], F32)
        e4 = al.get([128, NT, 4], F32)
        s4 = al.get([128, NT], F32)
        mask = al.get([128, NT, NE], F32)
        pos = al.get([128, NT, NE], F32)
        tot = al.get([128, NT, NE], F32)
        carry = al.get([128, NT + 1, NE], F32)
        cntv = al.get([128, 4, NE], F32)
        pend = al.get([128, NE], F32)
        slot = al.get([128, NT, NE], F32)
        oh = al.get([128, NT, NE], F32)
        slk = al.get([128, NT, 4], F32)
        cmpb = al.get([128, NBLK, NE], F32)
        blkf = al.get([128, NBLK], F32)
        P.dma("sp", lambda: SP.dma_start(out=Lr.rearrange("p a b -> p (a b)"), in_=LG), writes=["Lr"])
        for tt in range(NT):
            P.op("dve", lambda tt=tt: V.max(out=v8[:, tt, :], in_=Lr[:, tt, :]), reads=["Lr"], writes=["v8"])
            P.op("dve", lambda tt=tt: V.max_index(out=i8[:, tt, :], in_max=v8[:, tt, :], in_values=Lr[:, tt, :]),
                 reads=["Lr", "v8"], writes=["i8"])
        P.op("dve", lambda: V.tensor_copy(out=i8f, in_=i8), reads=["i8"], writes=["i8f"])
        P.op("dve", lambda: V.tensor_tensor(out=e4, in0=v8[:, :, 0:4], in1=v8[:, :, 0:1].to_broadcast([128, NT, 4]),
                                            op=ALU.subtract), reads=["v8"], writes=["e4"])
        P.op("act", lambda: A.activation(out=e4, in_=e4, func=AF.Exp), reads=["e4"], writes=["e4"])
        P.op("dve", lambda: V.tensor_reduce(out=s4, in_=e4, axis=AX.X, op=ALU.add), reads=["e4"], writes=["s4"])
        P.op("dve", lambda: V.reciprocal(out=s4, in_=s4), reads=["s4"], writes=["s4"])
        P.op("dve", lambda: V.tensor_tensor(out=WK, in0=e4, in1=s4.unsqueeze(2).to_broadcast([128, NT, 4]), op=ALU.mult),
             reads=["e4", "s4"], writes=["WK"])
        P.op("dve", lambda: V.tensor_tensor(out=mask, in0=Lr, in1=v8[:, :, 3:4].to_broadcast([128, NT, NE]), op=ALU.is_ge),
             reads=["Lr", "v8"], writes=["mask"])
        mflat = mask.rearrange("p a b -> p (a b)")
        for (c0, cn, bnk) in ((0, 512, 0), (512, 512, 1), (1024, 256, 2)):
            P.op("pe", lambda c0=c0, cn=cn, bnk=bnk: T.matmul(ps_f32(bnk, cn), lhsT=cst[:, C_LTRI:C_LTRI + 128],
                                                              rhs=mflat[:, c0:c0 + cn], start=True, stop=True),
                 reads=["mask"], writes=[("ps", bnk)])
            P.op("act", lambda c0=c0, cn=cn, bnk=bnk: A.copy(out=pos.rearrange("p a b -> p (a b)")[:, c0:c0 + cn],
                                                             in_=ps_f32(bnk, cn)), reads=[("ps", bnk)], writes=["pos"])
            P.op("pe", lambda c0=c0, cn=cn, bnk=bnk: T.matmul(ps_f32(bnk + 3, cn), lhsT=ones, rhs=mflat[:, c0:c0 + cn],
                                                              start=True, stop=True),
                 reads=["mask"], writes=[("ps", bnk + 3)])
            P.op("act", lambda c0=c0, cn=cn, bnk=bnk: A.copy(out=tot.rearrange("p a b -> p (a b)")[:, c0:c0 + cn],
                                                             in_=ps_f32(bnk + 3, cn)), reads=[("ps", bnk + 3)], writes=["tot"])
        P.op("dve", lambda: V.memset(carry[:, 0, :], 0.0), writes=["carry"])
        for tt in range(NT):
            P.op("dve", lambda tt=tt: V.tensor_tensor(out=carry[:, tt + 1, :], in0=carry[:, tt, :], in1=tot[:, tt, :], op=ALU.add),
                 reads=["tot", "carry"], writes=["carry"])
        cnt_ = carry[:, NT, :]
        cnti = cntv.bitcast(I32)
        P.op("dve", lambda: V.tensor_copy(out=cnti[:, 0, :], in_=cnt_), reads=["carry"], writes=["cv0"])
        P.op("dve", lambda: V.tensor_scalar(out=cnti[:, 1, :], in0=cnti[:, 0, :], scalar1=BLK - 1, scalar2=None, op0=ALU.add),
             reads=["cv0"], writes=["cv1"])
        P.op("dve", lambda: V.tensor_scalar(out=cnti[:, 2, :], in0=cnti[:, 1, :], scalar1=9, scalar2=9,
                                            op0=ALU.arith_shift_right, op1=ALU.logical_shift_left), reads=["cv1"], writes=["cv2"])
        P.op("dve", lambda: V.tensor_copy(out=cntv[:, 3, :], in_=cnti[:, 2, :]), reads=["cv2"], writes=["cv3"])
        P.op("dve", lambda: V.tensor_copy(out=pend[:, 0:1], in_=cntv[:, 3, 0:1]), reads=["cv3"], writes=["pend"])
        for e in range(1, NE):
            P.op("dve", lambda e=e: V.tensor_tensor(out=pend[:, e:e + 1], in0=pend[:, e - 1:e], in1=cntv[:, 3, e:e + 1], op=ALU.add),
                 reads=["pend", "cv3"], writes=["pend"])
        P.op("dve", lambda: V.tensor_tensor(out=cntv[:, 0, :], in0=pend, in1=cntv[:, 3, :], op=ALU.subtract),
             reads=["pend", "cv3", "cv1"], writes=["cv0"])
        P.op("dve", lambda: V.tensor_tensor(out=pos, in0=pos, in1=carry[:, 0:NT, :], op=ALU.add),
             reads=["pos", "carry"], writes=["pos"])
        P.op("dve", lambda: V.tensor_tensor(out=slot, in0=pos, in1=cntv[:, 0:1, :].to_broadcast([128, NT, NE]), op=ALU.add),
             reads=["pos", "cv0"], writes=["slot"])
        for k in range(4):
            P.op("dve", lambda k=k: V.tensor_tensor(
                out=oh, in0=cst[:, C_IOTA:C_IOTA + NE].unsqueeze(1).to_broadcast([128, NT, NE]),
                in1=i8f[:, :, k:k + 1].to_broadcast([128, NT, NE]), op=ALU.is_equal), reads=["i8f", "slk"], writes=["oh"])
            P.op("dve", lambda: V.tensor_tensor(out=oh, in0=oh, in1=slot, op=ALU.mult), reads=["oh", "slot"], writes=["oh"])
            P.op("dve", lambda k=k: V.tensor_reduce(out=slk[:, :, k], in_=oh, axis=AX.X, op=ALU.add),
                 reads=["oh"], writes=["slk"])
        P.op("dve", lambda: V.tensor_copy(out=SLI, in_=slk), reads=["slk"], writes=["SLI"])
        P.op("dve", lambda: V.tensor_tensor(
            out=cmpb, in0=pend.unsqueeze(1).to_broadcast([128, NBLK, NE]),
            in1=cst[:, C_BC:C_BC + NBLK].unsqueeze(2).to_broadcast([128, NBLK, NE]), op=ALU.is_le),
            reads=["pend"], writes=["cmpb"])
        P.op("dve", lambda: V.tensor_reduce(out=blkf, in_=cmpb, axis=AX.X, op=ALU.add), reads=["cmpb"], writes=["blkf"])
        P.op("dve", lambda: V.tensor_scalar(out=blkf, in0=blkf, scalar1=float(NE - 1), scalar2=None, op0=ALU.min),
             reads=["blkf"], writes=["blkf"])
        P.op("dve", lambda: V.tensor_copy(out=BLKI, in_=blkf), reads=["blkf"], writes=["BLKI"])
        P.op("dve", lambda: V.tensor_scalar(out=idxg, in0=blkf, scalar1=1024.0, scalar2=float(l * NE * 1024),
                                            op0=ALU.mult, op1=ALU.add), reads=["blkf"], writes=["idxg"])
        P.op("dve", lambda: V.tensor_tensor(out=idxf, in0=idxg.unsqueeze(2).to_broadcast([128, NBLK, 8]),
                                            in1=cst[:, C_KCP:C_KCP + 8].unsqueeze(1).to_broadcast([128, NBLK, 8]), op=ALU.add),
             reads=["idxg"], writes=["idxf"])
        P.op("dve", lambda: V.tensor_copy(out=IDXW, in_=idxf), reads=["idxf"], writes=["IDXW"])
        P.op("dve", lambda: V.tensor_scalar(out=idxg, in0=blkf, scalar1=128.0, scalar2=float(l * NE * 128),
                                            op0=ALU.mult, op1=ALU.add), reads=["blkf", "idxf"], writes=["idxg"])
        P.op("dve", lambda: V.tensor_tensor(out=idxg, in0=idxg, in1=cst[:, C_KB:C_KB + 1].to_broadcast([128, NBLK]), op=ALU.add),
             reads=["idxg"], writes=["idxg"])
        P.op("dve", lambda: V.tensor_copy(out=IDXB1, in_=idxg), reads=["idxg"], writes=["IDXB1"])
        P.op("dve", lambda: V.tensor_scalar(out=idxg, in0=blkf, scalar1=float(l * NE), scalar2=None, op0=ALU.add),
             reads=["blkf", "IDXB1"], writes=["idxg"])
        P.op("dve", lambda: V.tensor_copy(out=IDXB2, in_=idxg), reads=["idxg"], writes=["IDXB2"])
        P.dma("sp", lambda: SP.dma_start(out=SLd, in_=SLI.rearrange("p a b -> p (a b)")), reads=["SLI"], writes=["SLd"])

        htk = [al.get([128, D], BF16) for _ in range(4)]
        R3C = R3END
        for tt in range(NT):
            hb = htk[tt % 4]
            P.dma("sp", lambda tt=tt, hb=hb: SP.dma_start(out=hb, in_=H2[tt * 128:(tt + 1) * 128, :]), writes=[("htk", tt % 4)])
            for k in range(4):
                P.dma("pool", lambda tt=tt, k=k, hb=hb: G.indirect_dma_start(
                    out=XP, out_offset=bass.IndirectOffsetOnAxis(ap=SLI[:, tt, k:k + 1], axis=0),
                    in_=hb, in_offset=None, bounds_check=REG["xp"], oob_is_err=False),
                    reads=[("htk", tt % 4), "SLI"], writes=["XP"])
        if stop_after in ("p3c", "p3d"):
            di = dbgo.bitcast(I32)
            P.dma("sp", lambda: SP.dma_start(out=dbgo[:, 0:160], in_=WK.rearrange("p a b -> p (a b)")), reads=["WK"], writes=["dbgo"])
            P.dma("sp", lambda: SP.dma_start(out=di[:, 256:256 + NBLK], in_=BLKI), reads=["BLKI"], writes=["dbgo1"])
            P.dma("sp", lambda: SP.dma_start(out=di[:, 512:512 + NBLK * 8], in_=IDXW.rearrange("p a b -> p (a b)")), reads=["IDXW"], writes=["dbgo2"])
            P.dma("sp", lambda: SP.dma_start(out=di[:, 1100:1100 + NBLK], in_=IDXB1), reads=["IDXB1"], writes=["dbgo3"])
            P.dma("sp", lambda: SP.dma_start(out=di[:, 1200:1200 + NBLK], in_=IDXB2), reads=["IDXB2"], writes=["dbgo4"])
            P.dma("sp", lambda: SP.dma_start(out=dbgo[:, 1300:1332], in_=pend), reads=["pend"], writes=["dbgo5"])
            P.dma("sp", lambda: SP.dma_start(out=dbgo[:, 1400:1432], in_=carry[:, NT, :]), reads=["carry"], writes=["dbgo6"])
        P.barrier()
        if stop_after == "p3c":
            return True

        al = Alloc(R3C)
        w1b = [al.get([128, 8, 2 * DE], BF16) for _ in range(2)]
        w2b = [al.get([128, 8, D], BF16) for _ in range(2)]
        b1t = [al.get([128, 16], F32) for _ in range(2)]
        b2t = [al.get([128, D], F32) for _ in range(2)]
        xtok2 = [al.get([128, 4, D], BF16) for _ in range(2)]
        xbT = al.get([128, 8, 512], BF16)
        actT = al.get([128, 8, 512], BF16)
        gA = [al.get([128, 512], F32) for _ in range(2)]
        sA = [al.get([128, 512], F32) for _ in range(2)]
        lA = [al.get([128, 512], F32) for _ in range(2)]
        ytk = al.get([128, 4, D], F32)
        def issue_loads(b):
            wi = b % 2
            for kc in range(8):
                P.dma("pool", lambda b=b, wi=wi, kc=kc: G.indirect_dma_start(
                    out=w1b[wi][:, kc, :], out_offset=None, in_=w1.rearrange("l e r c -> (l e r) c"),
                    in_offset=bass.IndirectOffsetOnAxis(ap=IDXW[:, b, kc:kc + 1], axis=0),
                    bounds_check=REG["w"], oob_is_err=False), writes=[("w1b", wi, kc // 2)])
            for kc in range(8):
                P.dma("pool", lambda b=b, wi=wi, kc=kc: G.indirect_dma_start(
                    out=w2b[wi][:, kc, :], out_offset=None, in_=w2.rearrange("l e r c -> (l e r) c"),
                    in_offset=bass.IndirectOffsetOnAxis(ap=IDXW[:, b, kc:kc + 1], axis=0),
                    bounds_check=REG["w"], oob_is_err=False), writes=[("w2b", wi, kc // 4)])
            P.dma("pool", lambda b=b, wi=wi: G.indirect_dma_start(
                out=b1t[wi], out_offset=None, in_=b1T.rearrange("l e p c -> (l e p) c"),
                in_offset=bass.IndirectOffsetOnAxis(ap=IDXB1[:, b:b + 1], axis=0),
                bounds_check=REG["b1"], oob_is_err=False), writes=[("b1t", wi)])
            P.dma("pool", lambda b=b, wi=wi: G.indirect_dma_start(
                out=b2t[wi], out_offset=None, in_=b2.rearrange("l e c -> (l e) c"),
                in_offset=bass.IndirectOffsetOnAxis(ap=IDXB2[:, b:b + 1], axis=0),
                bounds_check=REG["b2"], oob_is_err=False), writes=[("b2t", wi)])
            P.dma("sp", lambda b=b, wi=wi: SP.dma_start(
                out=xtok2[wi], in_=XP[b * BLK:(b + 1) * BLK, :].rearrange("(s p) d -> p s d", p=128)),
                writes=[("xtok", wi)])

        issue_loads(0)
        for b in range(NBLK):
            wi = b % 2
            xtok = xtok2[wi]
            if b + 1 < NBLK:
                issue_loads(b + 1)
            for s in range(4):
                bnk = s % 2
                for kc in range(8):
                    P.op("pe", lambda s=s, kc=kc, bnk=bnk, xtok=xtok: T.transpose(
                        ps_bf(bnk)[:, kc * 128:(kc + 1) * 128], xtok[:, s, kc * 128:(kc + 1) * 128], identb),
                        reads=[("xtok", wi)], writes=[("ps", bnk)])
                P.op("act", lambda s=s, bnk=bnk: A.copy(out=xbT[:, :, s * 128:(s + 1) * 128],
                                                        in_=ps_bf(bnk).rearrange("p (k t) -> p k t", k=8)),
                     reads=[("ps", bnk)], writes=["xbT"])
            w1r = [("w1b", wi, q) for q in range(4)]
            w2r = [("w2b", wi, q) for q in range(2)]
            for mc in range(8):
                bG, bL = 2 + (mc % 2) * 2, 3 + (mc % 2) * 2
                g_, s_, l_ = gA[mc % 2], sA[mc % 2], lA[mc % 2]
                for kc in range(8):
                    P.op("pe", lambda kc=kc, mc=mc, bG=bG, wi=wi: T.matmul(
                        ps_f32(bG), lhsT=w1b[wi][:, kc, mc * 128:(mc + 1) * 128], rhs=xbT[:, kc, :],
                        start=(kc == 0), stop=(kc == 7)), reads=w1r + ["xbT"], writes=[("ps", bG)])
                for kc in range(8):
                    P.op("pe", lambda kc=kc, mc=mc, bL=bL, wi=wi: T.matmul(
                        ps_f32(bL), lhsT=w1b[wi][:, kc, DE + mc * 128:DE + (mc + 1) * 128], rhs=xbT[:, kc, :],
                        start=(kc == 0), stop=(kc == 7)), reads=w1r + ["xbT"], writes=[("ps", bL)])
                P.op("dve", lambda mc=mc, bG=bG, g_=g_, wi=wi: V.tensor_scalar(
                    out=g_, in0=ps_f32(bG), scalar1=b1t[wi][:, mc:mc + 1], scalar2=7.0, op0=ALU.add, op1=ALU.min),
                    reads=[("ps", bG), ("b1t", wi)], writes=[("gA", mc % 2)])
                P.op("act", lambda g_=g_, s_=s_: A.activation(out=s_, in_=g_, func=AF.Sigmoid, scale=1.702),
                     reads=[("gA", mc % 2)], writes=[("sA", mc % 2)])
                P.op("dve", lambda g_=g_, s_=s_: V.tensor_tensor(out=s_, in0=g_, in1=s_, op=ALU.mult),
                     reads=[("gA", mc % 2), ("sA", mc % 2)], writes=[("sA", mc % 2)])
                P.op("dve", lambda mc=mc, bL=bL, l_=l_, wi=wi: V.tensor_scalar(
                    out=l_, in0=ps_f32(bL), scalar1=b1t[wi][:, 8 + mc:9 + mc], scalar2=7.0, op0=ALU.add, op1=ALU.min),
                    reads=[("ps", bL), ("b1t", wi)], writes=[("lA", mc % 2)])
                P.op("dve", lambda l_=l_: V.tensor_scalar(out=l_, in0=l_, scalar1=-7.0, scalar2=1.0, op0=ALU.max, op1=ALU.add),
                     reads=[("lA", mc % 2)], writes=[("lA", mc % 2)])
                P.op("dve", lambda mc=mc, l_=l_, s_=s_: V.tensor_tensor(out=actT[:, mc, :], in0=l_, in1=s_, op=ALU.mult),
                     reads=[("lA", mc % 2), ("sA", mc % 2)], writes=["actT"])
            for s in range(4):
                b0 = 6
                for hf in range(2):
                    for kc in range(8):
                        P.op("pe", lambda kc=kc, hf=hf, s=s, wi=wi: T.matmul(
                            ps_f32(6 + hf), lhsT=actT[:, kc, s * 128:(s + 1) * 128], rhs=w2b[wi][:, kc, hf * 512:(hf + 1) * 512],
                            start=(kc == 0), stop=(kc == 7)), reads=w2r + ["actT"], writes=[("ps", 6 + hf)])
                P.op("dve", lambda s=s, wi=wi: V.tensor_tensor(out=ytk[:, s, :].rearrange("p (a f) -> p a f", a=2),
                                                               in0=psum[:, 6:8, :],
                                                               in1=b2t[wi].rearrange("p (a f) -> p a f", a=2), op=ALU.add),
                     reads=[("ps", 6), ("ps", 7), ("b2t", wi)], writes=["ytk"])
            P.dma("sp", lambda b=b: SP.dma_start(out=Yd[b * BLK:(b + 1) * BLK, :].rearrange("(s p) d -> p s d", p=128), in_=ytk),
                  reads=["ytk"], writes=["Yd"])
        P.barrier()
        if stop_after == "p3d":
            return True

        al = Alloc(R3END)
        g2row = al.get([128, 2, 8], F32)
        bct2 = al.get([128, 128], F32)
        G2 = [al.get([128, D], F32) for _ in range(2)]
        FG = al.get([128, D], F32)
        yk = [al.get([128, 4, D], F32) for _ in range(2)]
        xt3 = [al.get([128, D], F32) for _ in range(2)]
        acc = al.get([128, D], F32)
        junk3 = al.get([128, D], F32)
        ss3 = al.get([128, 4], F32)
        for j in range(2):
            P.op("dve", lambda j=j: V.tensor_copy(out=g2row[:, 0, :], in_=modT[:, 40:48, j]), reads=["bc_dst"], writes=["vecsrc"])
            bcast_row(G2[j], g2row[:, 0, :], bct2, 7)
        if l == DEPTH - 1:
            P.dma("sp", lambda: SP.dma_start(out=FG, in_=final_g.to_broadcast([128, D])), writes=["FG"])
        for tt in range(NT):
            j = 0 if tt < 32 else 1
            ykb = yk[tt % 2]
            xb = xt3[tt % 2]
            for k in range(4):
                P.dma("pool", lambda tt=tt, k=k, ykb=ykb: G.indirect_dma_start(
                    out=ykb[:, k, :], out_offset=None, in_=Yd,
                    in_offset=bass.IndirectOffsetOnAxis(ap=SLI[:, tt, k:k + 1], axis=0),
                    bounds_check=REG["xp"], oob_is_err=False), writes=[("yk", tt % 2, k)])
            P.dma("sp", lambda tt=tt, xb=xb: SP.dma_start(out=xb, in_=Xs[tt * 128:(tt + 1) * 128, :]),
                  reads=[("Xs", tt)], writes=[("xt3", tt % 2)])
            P.op("dve", lambda tt=tt, ykb=ykb: V.tensor_scalar(out=acc, in0=ykb[:, 0, :], scalar1=WK[:, tt, 0:1], scalar2=None,
                                                               op0=ALU.mult), reads=[("yk", tt % 2, 0)], writes=["acc"])
            for k in range(1, 4):
                P.op("dve", lambda tt=tt, k=k, ykb=ykb: V.scalar_tensor_tensor(
                    out=acc, in0=ykb[:, k, :], scalar=WK[:, tt, k:k + 1], in1=acc, op0=ALU.mult, op1=ALU.add),
                    reads=[("yk", tt % 2, k), "acc"], writes=["acc"])
            P.op("pool", lambda j=j: G.tensor_tensor(out=acc, in0=acc, in1=G2[j], op=ALU.mult), reads=["acc", "bc_dst"], writes=["acc"])
            P.op("dve", lambda xb=xb: V.tensor_tensor(out=xb, in0=xb, in1=acc, op=ALU.add),
                 reads=["acc", ("xt3", tt % 2)], writes=[("xt3", tt % 2)])
            if l < DEPTH - 1:
                P.dma("sp", lambda tt=tt, xb=xb: SP.dma_start(out=Xs[tt * 128:(tt + 1) * 128, :], in_=xb),
                      reads=[("xt3", tt % 2)], writes=[("Xs", tt)])
            else:
                P.op("act", lambda xb=xb: A.activation(out=junk3, in_=xb, func=AF.Square, accum_out=ss3[:, 0:1]),
                     reads=[("xt3", tt % 2)], writes=["junk3", "ss3"])
                P.op("act", lambda: A.activation(out=ss3[:, 1:2], in_=ss3[:, 0:1], func=AF.Sqrt, scale=1.0 / D, bias=epsT[:, 0:1]),
                     reads=["ss3"], writes=["ss3b"])
                P.op("dve", lambda: V.reciprocal(out=ss3[:, 2:3], in_=ss3[:, 1:2]), reads=["ss3b"], writes=["ss3c"])
                P.op("dve", lambda xb=xb: V.scalar_tensor_tensor(out=xb, in0=xb, scalar=ss3[:, 2:3], in1=FG,
                                                                 op0=ALU.mult, op1=ALU.mult),
                     reads=[("xt3", tt % 2), "ss3c", "FG"], writes=[("xt3", tt % 2)])
                P.dma("sp", lambda tt=tt, xb=xb: SP.dma_start(out=y_out[tt * 128:(tt + 1) * 128, :], in_=xb),
                      reads=[("xt3", tt % 2)], writes=[("yo", tt)])
        P.barrier()
        return False

    for l in range(DEPTH):
        if do_layer(l):
            break
    P.barrier()
    P.emit()
    return nc, stack


_CONST_CACHE = {}


def _host_consts():
    if not _CONST_CACHE:
        _CONST_CACHE["cst"] = _consts()
        _CONST_CACHE["rot"] = _rot_tables()
        _CONST_CACHE["dftc"] = _dft_chan()
        _CONST_CACHE["dfts"] = _dft_seq(NS)
        _CONST_CACHE["dftp"] = _dft_seq(256)
    return _CONST_CACHE


def make_in_maps(inp, ncores=8):
    f = lambda a: np.ascontiguousarray(np.asarray(a, dtype=np.float32))
    hc = _host_consts()
    shared = {
        "w_ada": f(inp["w_ada"]),
        "b_adaT": f(np.asarray(inp["b_ada"]).reshape(DEPTH, 48, 128).transpose(0, 2, 1)),
        "g1T": f(np.asarray(inp["norm1_g"]).reshape(DEPTH, 8, 128).transpose(0, 2, 1)),
        "g2T": f(np.asarray(inp["norm2_g"]).reshape(DEPTH, 8, 128).transpose(0, 2, 1)),
        "w_in": f(inp["w_in"]),
        "decay": f(np.asarray(inp["ret_decay_logit"]).reshape(DEPTH, 16)),
        "w_ret_o": f(inp["w_ret_o"]),
        "w_four": f(inp["w_four"]),
        "w_out": f(inp["w_out"]),
        "w_router": f(inp["w_router"]),
        "b_router": f(inp["b_router"]),
        "w1": f(inp["w1"]),
        "b1T": f(np.asarray(inp["b1"]).reshape(DEPTH, NE, 16, 128).transpose(0, 1, 3, 2)),
        "w2": f(inp["w2"]),
        "b2": f(inp["b2"]),
        "final_g": f(np.asarray(inp["final_g"]).reshape(1, D)),
        "cst": hc["cst"], "rot": hc["rot"], "dftc": hc["dftc"], "dfts": hc["dfts"], "dftp": hc["dftp"],
    }
    xp = np.asarray(inp["x_prompt"], np.float32)
    xs = np.asarray(inp["x_sample"], np.float32)
    st = np.asarray(inp["state_ret"], np.float32)
    c = np.asarray(inp["c"], np.float32)
    cc = np.asarray(inp["c_ctx"], np.float32)
    maps = []
    for i in range(ncores):
        m = dict(shared)
        m["x_in"] = np.ascontiguousarray(np.concatenate([xs[i], xp[4 * i:4 * i + 4].reshape(1024, D)], axis=0))
        m["s0"] = np.ascontiguousarray(st[i])
        cT = np.stack([c[i].reshape(8, 128).T, cc.reshape(8, 128).T], axis=-1)
        m["cT"] = np.ascontiguousarray(cT.astype(np.float32))
        maps.append(m)
    return maps


def kernel(**inputs):
    nc, stack = build()
    try:
        maps = make_in_maps(inputs, 8)
        res = run_bass_kernel_spmd(nc, maps, core_ids=list(range(8)))
    finally:
        stack.close()
    y_prompt = np.zeros((32, 256, D), np.float32)
    y_sample = np.zeros((8, NS, D), np.float32)
    new_state = np.zeros((32, DEPTH, 2, NH, DK, DV), np.float32)
    for i, r in enumerate(res.results):
        yo = np.asarray(r["y_out"])
        y_sample[i] = yo[:NS]
        y_prompt[4 * i:4 * i + 4] = yo[NS:].reshape(4, 256, D)
        new_state[4 * i:4 * i + 4] = np.asarray(r["ns_out"])
    return (y_prompt, y_sample, new_state)
```

```python
import os
from contextlib import ExitStack
import numpy as np
import ml_dtypes
import concourse.bass as bass
import concourse.mybir as mybir
from concourse.bass_utils import run_bass_kernel_spmd

F32 = mybir.dt.float32
BF16 = mybir.dt.bfloat16
I32 = mybir.dt.int32
U32 = mybir.dt.uint32
AF = mybir.ActivationFunctionType
ALU = mybir.AluOpType
AX = mybir.AxisListType

D = 1024
NTOK = 5120
NT = 40
NS = 4096
NH = 8
DK = 128
DV = 256
INW = 9216
NE = 32
DE = 1024
BLK = 512
NBLK = 72
PT = NBLK * BLK
EPS = 1e-6
GN_EPS = 1e-5
DEPTH = 2

C_ID, C_DF, C_TF, C_DB, C_TB, C_IP1, C_IB, C_LTRI, C_ONES = [i * 128 for i in range(9)]
C_KF = 9 * 128
C_KB = C_KF + 1
C_IOTA = C_KB + 1
C_BC = C_IOTA + 32
C_KCP = C_BC + NBLK
C_N = C_KCP + 8


def _consts():
    c = np.zeros((128, C_N), np.float32)
    p = np.arange(128)
    j = p[:, None].astype(np.float64)
    i = p[None, :].astype(np.float64)
    c[:, C_ID:C_ID + 128] = np.eye(128)
    c[:, C_DF:C_DF + 128] = np.maximum(i - j, 0)
    c[:, C_TF:C_TF + 128] = (i >= j)
    c[:, C_DB:C_DB + 128] = np.maximum(j - i, 0)
    c[:, C_TB:C_TB + 128] = (j >= i)
    c[:, C_IP1:C_IP1 + 128] = i + 1
    c[:, C_IB:C_IB + 128] = 128 - i
    c[:, C_LTRI:C_LTRI + 128] = (j < i)
    c[:, C_ONES:C_ONES + 128] = 1.0
    c[:, C_KF] = 127 - p
    c[:, C_KB] = p
    c[:, C_IOTA:C_IOTA + 32] = np.arange(32)[None, :]
    c[:, C_BC:C_BC + NBLK] = (np.arange(NBLK) * BLK)[None, :]
    c[:, C_KCP:C_KCP + 8] = np.arange(8)[None, :] * 128 + p[:, None]
    return c


def _rot_tables():
    t = np.arange(NS)
    row = (t // 64).astype(np.float32)
    col = (t % 64).astype(np.float32)
    nf = 32
    inv = (np.float32(10000.0) ** (-(np.arange(nf, dtype=np.float32)) / np.float32(nf))).astype(np.float32)
    ang = np.concatenate([row[:, None] * inv[None, :], col[:, None] * inv[None, :]], axis=1)
    ang = ang.astype(np.float64)
    cs = np.cos(ang).T
    sn = np.sin(ang).T
    tab = np.zeros((128, 2, NS), np.float32)
    tab[:64, 0] = cs
    tab[64:, 0] = cs
    tab[:64, 1] = sn
    tab[64:, 1] = sn
    return tab


def _dft_chan():
    c = np.arange(256)
    m = (c[:, None] * c[None, :]) % 256
    a = 2.0 * np.pi * m / 256.0
    cs = np.cos(a) / 16.0
    sn = -np.sin(a) / 16.0
    full = np.concatenate([cs, sn], axis=1)
    return full.reshape(2, 128, 512).transpose(1, 0, 2).astype(ml_dtypes.bfloat16)


def _dft_seq(n):
    nch = n // 128
    idx = np.arange(n, dtype=np.int64)
    m = (idx[:, None] * idx[None, :]) % n
    a = 2.0 * np.pi * m.astype(np.float64) / n
    sc = 1.0 / np.sqrt(n)
    out = np.zeros((nch, 128, 2, nch, 128), ml_dtypes.bfloat16)
    for cs, f in ((0, np.cos), (1, np.sin)):
        mat = (f(a) * sc).astype(np.float32)
        out[:, :, cs] = mat.reshape(nch, 128, nch, 128).transpose(2, 1, 0, 3).astype(ml_dtypes.bfloat16)
    return out


class Prog:
    CE = ("pe", "act", "dve", "pool")
    ALL = ("pe", "act", "dve", "pool", "sp")
    R = 10

    def __init__(self, nc, stack):
        self.nc = nc
        self.stack = stack
        self.ops = {e: [] for e in self.ALL}
        self.sems = {}
        self.phase = 0
        self.cnt = {e: 0 for e in self.CE}
        self.dcnt = {"sp": 0, "pool": 0}
        self.lastw = {}
        self.readers = {}
        self.waited = {e: {} for e in self.ALL}
        self.nsem = 0
        self.pool_init = None
        for q in self.dcnt:
            for s in range(self.R):
                self._mk(("d", q, s))
        self._new_phase()

    def _mk(self, key):
        self.sems[key] = self.stack.enter_context(self.nc.semaphore("s%d" % self.nsem))
        self.nsem += 1

    def _new_phase(self):
        self.phase += 1
        for e in self.CE:
            self.cnt[e] = 0
            self._mk(("c", e, self.phase))

    def _ck(self, e):
        return ("c", e, self.phase)

    def _deps(self, eng, reads, writes):
        deps = {}

        def add(ev):
            if ev is None:
                return
            k, v = ev
            if deps.get(k, 0) < v:
                deps[k] = v
        for t in reads:
            add(self.lastw.get(t))
        for t in writes:
            add(self.lastw.get(t))
            for k, v in self.readers.get(t, {}).items():
                add((k, v))
        waits = []
        for k, v in deps.items():
            if eng == "pe" and k == self._ck("pe"):
                continue
            if self.waited[eng].get(k, 0) >= v:
                continue
            self.waited[eng][k] = v
            waits.append((k, v))
        return waits

    def _post(self, ev, reads, writes):
        k, v = ev
        for t in reads:
            r = self.readers.setdefault(t, {})
            if r.get(k, 0) < v:
                r[k] = v
        for t in writes:
            self.lastw[t] = ev
            self.readers[t] = {}

    def op(self, eng, fn, reads=(), writes=()):
        waits = self._deps(eng, reads, writes)
        self.cnt[eng] += 1
        ev = (self._ck(eng), self.cnt[eng])
        self.ops[eng].append((waits, fn, ev[0], 1))
        self._post(ev, reads, writes)

    def dma(self, q, fn, reads=(), writes=()):
        waits = self._deps(q, reads, writes)
        j = self.dcnt[q]
        self.dcnt[q] += 1
        slot, gen = j % self.R, j // self.R
        key = ("d", q, slot)
        if gen > 0 and self.waited[q].get(key, 0) < 16 * gen:
            self.waited[q][key] = 16 * gen
            waits.append((key, 16 * gen))
        ev = (key, 16 * (gen + 1))
        self.ops[q].append((waits, fn, key, 16))
        self._post(ev, reads, writes)

    def barrier(self):
        evs = []
        for e in self.CE:
            if self.cnt[e] > 0:
                evs.append((self._ck(e), self.cnt[e]))
        for q, n in self.dcnt.items():
            for s in range(self.R):
                if n > s:
                    last = ((n - 1 - s) // self.R) + 1
                    evs.append((("d", q, s), 16 * last))
        for e in self.ALL:
            waits = []
            for k, v in evs:
                if self.waited[e].get(k, 0) >= v:
                    continue
                self.waited[e][k] = v
                waits.append((k, v))
            if waits:
                self.ops[e].append((waits, None, None, 0))
        self.lastw = {}
        self.readers = {}
        self._new_phase()

    def emit(self):
        nc = self.nc
        engs = {"pe": nc.tensor, "act": nc.scalar, "dve": nc.vector, "pool": nc.gpsimd, "sp": nc.sync}
        with nc.Block() as block:
            def run(name):
                eng = engs[name]
                for waits, fn, key, inc in self.ops[name]:
                    for k, v in waits:
                        eng.wait_ge(self.sems[k], v)
                    if fn is not None:
                        ins = fn()
                        ins.then_inc(self.sems[key], inc)

            @block.tensor
            def _(e):
                run("pe")

            @block.scalar
            def _(e):
                run("act")

            @block.vector
            def _(e):
                run("dve")

            @block.gpsimd
            def _(e):
                if self.pool_init is not None:
                    self.pool_init()
                run("pool")

            @block.sync
            def _(e):
                run("sp")


def build(debug=False, stop_after=None, dbg_names=()):
    nc = bass.Bass("TRN2", target_bir_lowering=False)
    stack = ExitStack()
    dbg_kind = "ExternalOutput" if debug else "Internal"

    def din(name, shape, dt=F32):
        return nc.dram_tensor(name, list(shape), dt, kind="ExternalInput").ap()

    def dscr(name, shape, dt, dbg=False):
        return nc.dram_tensor(name, list(shape), dt, kind=("ExternalOutput" if name in dbg_names else "Internal")).ap()

    x_in = din("x_in", [NTOK, D])
    s0_in = din("s0", [DEPTH, 2, NH, DK, DV])
    cT_in = din("cT", [128, 8, 2])
    w_ada = din("w_ada", [DEPTH, D, 6 * D])
    b_adaT = din("b_adaT", [DEPTH, 128, 48])
    g1T = din("g1T", [DEPTH, 128, 8])
    g2T = din("g2T", [DEPTH, 128, 8])
    w_in = din("w_in", [DEPTH, D, INW])
    decay = din("decay", [DEPTH, 16])
    w_ret_o = din("w_ret_o", [DEPTH, 2048, D])
    w_four = din("w_four", [DEPTH, D, D])
    w_out = din("w_out", [DEPTH, D, D])
    w_router = din("w_router", [DEPTH, D, NE])
    b_router = din("b_router", [DEPTH, NE])
    w1 = din("w1", [DEPTH, NE, D, 2 * DE])
    b1T = din("b1T", [DEPTH, NE, 128, 16])
    w2 = din("w2", [DEPTH, NE, DE, D])
    b2 = din("b2", [DEPTH, NE, D])
    final_g = din("final_g", [1, D])
    cst_in = din("cst", [128, C_N])
    rot_in = din("rot", [128, 2, NS])
    dftc_in = din("dftc", [128, 2, 512], BF16)
    dfts_in = din("dfts", [32, 128, 2, 32, 128], BF16)
    dftp_in = din("dftp", [2, 128, 2, 2, 128], BF16)

    y_out = nc.dram_tensor("y_out", [NTOK, D], F32, kind="ExternalOutput").ap()
    ns_out = nc.dram_tensor("ns_out", [4, DEPTH, 2, NH, DK, DV], F32, kind="ExternalOutput").ap()

    Xs = dscr("Xs", [NTOK, D], F32, True)
    QT = dscr("QT", [1024, NTOK], BF16, True)
    KT = dscr("KT", [1024, NTOK], BF16, True)
    Vd = dscr("Vd", [NTOK, 2048], BF16, True)
    GR = dscr("GR", [NTOK, 2048], BF16, True)
    XFT = dscr("XFT", [1024, NTOK], BF16, True)
    GAT = dscr("GAT", [1024, NTOK], BF16, True)
    GBT = dscr("GBT", [1024, NTOK], BF16, True)
    OGT = dscr("OGT", [2048, NTOK], BF16, True)
    UFT = dscr("UFT", [1024, NTOK], BF16, True)
    H2 = dscr("H2", [NTOK, D], BF16, True)
    XP = dscr("XP", [PT, D], BF16)
    Yd = dscr("Yd", [PT, D], F32)
    LG = dscr("LGd", [128, NT * 32], F32, True)
    SLd = dscr("SLd", [128, NT * 4], I32, True)

    ARENA = 48640
    arena = stack.enter_context(nc.sbuf_tensor("arena", [128, ARENA], F32))
    psum = stack.enter_context(nc.psum_tensor("ps", [128, 8, 512], F32))

    class Alloc:
        def __init__(self, base=0):
            self.off = base

        def get(self, shape, dt):
            n = int(np.prod(shape[1:]))
            esz = 2 if dt == BF16 else 4
            words = (n * esz + 3) // 4
            words = (words + 7) // 8 * 8
            a = arena[:, self.off:self.off + words]
            self.off += words
            assert self.off <= ARENA, ("SBUF overflow", self.off)
            if dt != F32:
                a = a.bitcast(dt)
            a = a[:, 0:n]
            if len(shape) == 3:
                a = a.rearrange("p (a b) -> p a b", a=shape[1])
            elif len(shape) == 4:
                a = a.rearrange("p (a b c) -> p a b c", a=shape[1], b=shape[2])
            return a

    def ps_f32(b, n=512):
        return psum[:, b, 0:n]

    def ps_bf(b):
        return psum[:, b, :].bitcast(BF16)

    P = Prog(nc, stack)
    REG = {}

    def _pool_init():
        REG["xp"] = nc.gpsimd.to_reg(PT - 1)
        REG["w"] = nc.gpsimd.to_reg(DEPTH * NE * 1024 - 1)
        REG["b1"] = nc.gpsimd.to_reg(DEPTH * NE * 128 - 1)
        REG["b2"] = nc.gpsimd.to_reg(DEPTH * NE - 1)
    P.pool_init = _pool_init
    V, A, G, T, SP = nc.vector, nc.scalar, nc.gpsimd, nc.tensor, nc.sync

    pa = Alloc(0)
    cst = pa.get([128, C_N], F32)
    identb = pa.get([128, 128], BF16)
    scT = pa.get([128, 8, 2], F32)
    modT = pa.get([128, 48, 2], F32)
    A1 = pa.get([128, 2, 8], F32)
    epsT = pa.get([128, 2], F32)
    PBASE = pa.off

    ident = cst[:, C_ID:C_ID + 128]
    ones = cst[:, C_ONES:C_ONES + 128]

    P.dma("sp", lambda: SP.dma_start(out=cst, in_=cst_in), writes=["cst"])
    P.dma("sp", lambda: SP.dma_start(out=scT, in_=cT_in), writes=["scT"])
    P.op("act", lambda: A.activation(out=scT, in_=scT, func=AF.Silu), reads=["scT"], writes=["scT"])
    P.op("dve", lambda: V.tensor_copy(out=identb, in_=ident), reads=["cst"], writes=["identb"])
    P.op("dve", lambda: V.memset(epsT[:, 0:1], EPS), writes=["eps0"])
    P.op("dve", lambda: V.memset(epsT[:, 1:2], GN_EPS), writes=["eps1"])
    P.barrier()
    if debug or stop_after:
        dbgo = nc.dram_tensor("dbgo", [128, 4096], F32, kind="ExternalOutput").ap()
    if stop_after == "init":
        P.dma("sp", lambda: SP.dma_start(out=dbgo[:, 0:C_N], in_=cst), writes=["dbgo"])
        P.dma("sp", lambda: SP.dma_start(out=dbgo[:, 2048:2064], in_=scT.rearrange("p a b -> p (a b)")), writes=["dbgo2"])
        P.barrier()
        P.emit()
        return nc, stack

    seqs = [(0, NS, 32, True, 0, None)] + [(NS + 256 * s, 256, 2, False, 1, s) for s in range(4)]

    def bcast_row(dst, vecT, tmpd, psb):
        for kc in range(8):
            P.op("dve", lambda kc=kc: V.tensor_scalar(out=tmpd, in0=ident, scalar1=vecT[:, kc:kc + 1],
                                                      scalar2=None, op0=ALU.mult),
                 reads=["vecsrc"], writes=["bc_tmp"])
            P.op("pe", lambda kc=kc: T.matmul(ps_f32(psb, 128), lhsT=ones, rhs=tmpd, start=True, stop=True),
                 reads=["bc_tmp"], writes=[("ps", psb)])
            P.op("act", lambda kc=kc: A.copy(out=dst[:, kc * 128:(kc + 1) * 128], in_=ps_f32(psb, 128)),
                 reads=[("ps", psb)], writes=["bc_dst"])

    def do_layer(l):
        Xsrc = x_in if l == 0 else Xs
        al = Alloc(PBASE)
        hT = al.get([128, 8, NTOK], BF16)
        xt = [al.get([128, D], F32) for _ in range(2)]
        xn = [al.get([128, D], F32) for _ in range(2)]
        junk = al.get([128, D], F32)
        ss = al.get([128, 4], F32)
        P1END = al.off
        wa = [al.get([128, 8, 512], F32) for _ in range(2)]
        bada = al.get([128, 48], F32)
        g1t = al.get([128, 8], F32)
        P.dma("sp", lambda: SP.dma_start(out=bada, in_=b_adaT[l]), writes=["bada"])
        P.dma("sp", lambda: SP.dma_start(out=g1t, in_=g1T[l]), writes=["g1t"])
        for nb in range(12):
            wb = wa[nb % 2]
            P.dma("sp", lambda nb=nb, wb=wb: SP.dma_start(
                out=wb, in_=w_ada[l].rearrange("(kc p) c -> p kc c", p=128)[:, :, nb * 512:(nb + 1) * 512]),
                writes=[("wa", nb % 2)])
            for sub in range(4):
                ch = nb * 4 + sub
                for kc in range(8):
                    P.op("pe", lambda kc=kc, sub=sub, ch=ch, wb=wb: T.matmul(
                        psum[:, 0, ch * 2:ch * 2 + 2], lhsT=wb[:, kc, sub * 128:(sub + 1) * 128],
                        rhs=scT[:, kc, :], start=(kc == 0), stop=(kc == 7)),
                        reads=[("wa", nb % 2)], writes=[("ps", 0)])
        P.op("dve", lambda: V.tensor_tensor(
            out=modT, in0=psum[:, 0, 0:96].rearrange("p (c j) -> p c j", j=2),
            in1=bada.unsqueeze(2).to_broadcast([128, 48, 2]), op=ALU.add),
            reads=[("ps", 0), "bada"], writes=["modT"])
        for j in range(2):
            P.op("dve", lambda j=j: V.scalar_tensor_tensor(
                out=A1[:, j, :], in0=modT[:, 8:16, j], scalar=1.0, in1=g1t, op0=ALU.add, op1=ALU.mult),
                reads=["modT", "g1t"], writes=["A1"])

        if stop_after == "p0":
            P.dma("sp", lambda: SP.dma_start(out=dbgo[:, 0:96], in_=modT.rearrange("p a b -> p (a b)")), reads=["modT"], writes=["dbgo"])
            P.dma("sp", lambda: SP.dma_start(out=dbgo[:, 128:144], in_=A1.rearrange("p a b -> p (a b)")), reads=["A1"], writes=["dbgo2"])
            P.barrier()
            return True
        for tt in range(NT):
            j = 0 if tt < 32 else 1
            xb, xnb = xt[tt % 2], xn[tt % 2]
            P.dma("sp", lambda tt=tt, xb=xb: SP.dma_start(out=xb, in_=Xsrc[tt * 128:(tt + 1) * 128, :]),
                  writes=[("xt", tt % 2)])
            P.op("act", lambda xb=xb: A.activation(out=junk, in_=xb, func=AF.Square, accum_out=ss[:, 0:1]),
                 reads=[("xt", tt % 2)], writes=["junk", "ss"])
            P.op("act", lambda: A.activation(out=ss[:, 1:2], in_=ss[:, 0:1], func=AF.Sqrt, scale=1.0 / D, bias=epsT[:, 0:1]),
                 reads=["ss"], writes=["ss1"])
            P.op("dve", lambda: V.reciprocal(out=ss[:, 2:3], in_=ss[:, 1:2]), reads=["ss1"], writes=["ss2"])
            P.op("dve", lambda xb=xb, xnb=xnb: V.tensor_scalar(out=xnb, in0=xb, scalar1=ss[:, 2:3], scalar2=None,
                                                               op0=ALU.mult),
                 reads=[("xt", tt % 2), "ss2"], writes=[("xn", tt % 2)])
            for half in range(2):
                bnk = 1 + half + 2 * (tt % 2)
                for q in range(4):
                    kc = half * 4 + q
                    P.op("pe", lambda kc=kc, q=q, bnk=bnk, xnb=xnb: T.matmul(
                        psum[:, bnk, q * 128:(q + 1) * 128], lhsT=xnb[:, kc * 128:(kc + 1) * 128], rhs=ident,
                        start=True, stop=True),
                        reads=[("xn", tt % 2)], writes=[("ps", bnk)])
                for q in range(4):
                    kc = half * 4 + q
                    P.op("act", lambda kc=kc, q=q, bnk=bnk, tt=tt, j=j: A.activation(
                        out=hT[:, kc, tt * 128:(tt + 1) * 128], in_=psum[:, bnk, q * 128:(q + 1) * 128],
                        func=AF.Identity, scale=A1[:, j, kc:kc + 1], bias=modT[:, kc, j:j + 1]),
                        reads=[("ps", bnk), "A1", "modT"], writes=[("hT", tt)])
        if stop_after == "p1":
            P.dma("sp", lambda: SP.dma_start(out=dbgo.bitcast(BF16)[:, 0:8 * 128].rearrange("p (k t) -> p k t", k=8), in_=hT[:, :, 0:128]), reads=[("hT", 0)], writes=["dbgo"])
            P.dma("sp", lambda: SP.dma_start(out=dbgo.bitcast(BF16)[:, 4096:4096 + 8 * 128].rearrange("p (k t) -> p k t", k=8), in_=hT[:, :, 4992:5120]), reads=[("hT", 39)], writes=["dbgo1"])
        P.barrier()
        if stop_after == "p1":
            return True

        al = Alloc(P1END)
        wbf = [al.get([128, 8, 512], BF16) for _ in range(2)]
        wrot = al.get([128, 8, 512], BF16)
        rot = al.get([128, 2, NS], F32)
        osb = [al.get([128, 4, 512], BF16) for _ in range(2)]
        t1 = [al.get([128, 512], F32) for _ in range(2)]
        t2 = [al.get([128, 512], F32) for _ in range(2)]
        P.dma("sp", lambda: SP.dma_start(out=rot, in_=rot_in), writes=["rot"])
        oi = 0
        for cb in range(18):
            wb = wbf[cb % 2]
            wtok = ("wbf", cb % 2)
            P.dma("pool", lambda cb=cb, wb=wb: G.dma_start(
                out=wb, in_=w_in[l].rearrange("(kc p) c -> p kc c", p=128)[:, :, cb * 512:(cb + 1) * 512]),
                writes=[wtok])
            kind = ("q", "q", "k", "k", "v", "v", "v", "v", "g", "g", "g", "g", "x", "x", "a", "a", "b", "b")[cb]
            if kind in ("q", "k"):
                wv = wb.rearrange("p k (h two f) -> p k h two f", two=2, f=64)
                rv = wrot.rearrange("p k (h two f) -> p k h two f", two=2, f=64)
                P.op("act", lambda wv=wv, rv=rv: A.mul(out=rv[:, :, :, 0, :], in_=wv[:, :, :, 1, :], mul=-1.0),
                     reads=[wtok], writes=["wrot"])
                P.op("dve", lambda wv=wv, rv=rv: V.tensor_copy(out=rv[:, :, :, 1, :], in_=wv[:, :, :, 0, :]),
                     reads=[wtok], writes=["wrot2"])
            for tg in range(10):
                ob = osb[oi % 2]
                otok = ("osb", oi % 2)
                oi += 1
                tsl = slice(tg * 512, (tg + 1) * 512)
                for sub in range(4):
                    bA = (sub % 2) * 2
                    bB = bA + 1
                    if kind in ("v", "g"):
                        tk = tg * 512 + sub * 128
                        for kc in range(8):
                            P.op("pe", lambda kc=kc, bA=bA, tk=tk, wb=wb: T.matmul(
                                ps_f32(bA), lhsT=hT[:, kc, tk:tk + 128], rhs=wb[:, kc, :],
                                start=(kc == 0), stop=(kc == 7)), reads=[wtok], writes=[("ps", bA)])
                        fn = AF.Copy if kind == "v" else AF.Silu
                        P.op("act", lambda bA=bA, ob=ob, sub=sub, fn=fn: A.activation(
                            out=ob[:, sub, :], in_=ps_f32(bA), func=fn),
                            reads=[("ps", bA)], writes=[otok])
                    else:
                        csl = slice(sub * 128, (sub + 1) * 128)
                        for kc in range(8):
                            P.op("pe", lambda kc=kc, bA=bA, wb=wb, csl=csl, tsl=tsl: T.matmul(
                                ps_f32(bA), lhsT=wb[:, kc, csl], rhs=hT[:, kc, tsl],
                                start=(kc == 0), stop=(kc == 7)), reads=[wtok], writes=[("ps", bA)])
                        sc = (DK ** -0.5) if kind == "q" else 1.0
                        if kind in ("q", "k") and tg < 8:
                            for kc in range(8):
                                P.op("pe", lambda kc=kc, bB=bB, csl=csl, tsl=tsl: T.matmul(
                                    ps_f32(bB), lhsT=wrot[:, kc, csl], rhs=hT[:, kc, tsl],
                                    start=(kc == 0), stop=(kc == 7)), reads=["wrot", "wrot2"], writes=[("ps", bB)])
                            ta, tb = t1[sub % 2], t2[sub % 2]
                            P.op("dve", lambda bA=bA, ta=ta, tsl=tsl, sc=sc: V.scalar_tensor_tensor(
                                out=ta, in0=ps_f32(bA), scalar=sc, in1=rot[:, 0, tsl], op0=ALU.mult, op1=ALU.mult),
                                reads=[("ps", bA), "rot"], writes=[("t1", sub % 2)])
                            P.op("dve", lambda bB=bB, tb=tb, tsl=tsl, sc=sc: V.scalar_tensor_tensor(
                                out=tb, in0=ps_f32(bB), scalar=sc, in1=rot[:, 1, tsl], op0=ALU.mult, op1=ALU.mult),
                                reads=[("ps", bB), "rot"], writes=[("t2", sub % 2)])
                            P.op("pool", lambda ta=ta, tb=tb, ob=ob, sub=sub: G.tensor_tensor(
                                out=ob[:, sub, :], in0=ta, in1=tb, op=ALU.add),
                                reads=[("t1", sub % 2), ("t2", sub % 2)], writes=[otok])
                        else:
                            if kind in ("a", "b"):
                                P.op("act", lambda bA=bA, ob=ob, sub=sub: A.activation(
                                    out=ob[:, sub, :], in_=ps_f32(bA), func=AF.Sigmoid),
                                    reads=[("ps", bA)], writes=[otok])
                            else:
                                P.op("act", lambda bA=bA, ob=ob, sub=sub, sc=sc: A.activation(
                                    out=ob[:, sub, :], in_=ps_f32(bA), func=AF.Copy, scale=sc),
                                    reads=[("ps", bA)], writes=[otok])
                if kind in ("v", "g"):
                    dst = (Vd if kind == "v" else GR)
                    c0 = (cb - (4 if kind == "v" else 8)) * 512
                    P.dma("sp", lambda dst=dst, c0=c0, tg=tg, ob=ob: SP.dma_start(
                        out=dst[tg * 512:(tg + 1) * 512, c0:c0 + 512].rearrange("(s p) c -> p s c", p=128), in_=ob),
                        reads=[otok], writes=[("u", kind)])
                else:
                    dst, cb0 = {"q": (QT, 0), "k": (KT, 2), "x": (XFT, 12), "a": (GAT, 14), "b": (GBT, 16)}[kind]
                    r0 = (cb - cb0) * 512
                    P.dma("sp", lambda dst=dst, r0=r0, tsl=tsl, ob=ob: SP.dma_start(
                        out=dst[r0:r0 + 512, tsl].rearrange("(s p) t -> p s t", p=128), in_=ob),
                        reads=[otok], writes=[("u", kind)])
        P.barrier()
        if stop_after == "p2a":
            return True

        al = Alloc(PBASE)
        dl = al.get([128, 16], F32)
        MC = al.get([128, 8, 128], F32)
        QDF = al.get([128, 8, 128], F32)
        QDB = al.get([128, 8, 128], F32)
        KD = al.get([128, 2, 8], F32)
        CDEC = al.get([128, 16], F32)
        mtmp = al.get([128, 2, 128], F32)
        QTh = al.get([128, NS], BF16)
        KTh = al.get([128, NS], BF16)
        Vh = al.get([128, 32, 256], BF16)
        GRh = al.get([128, 32, 256], BF16)
        Kf = al.get([128, 32, 128], BF16)
        Kb = al.get([128, 32, 128], BF16)
        QfT = al.get([128, 32, 128], BF16)
        QbT = al.get([128, 32, 128], BF16)
        SbAll = al.get([128, 33, 256], BF16)
        OGh = al.get([128, 2, NS], BF16)
        Sf32 = al.get([128, 256], F32)
        Sb32 = al.get([128, 256], F32)
        Sfbf = [al.get([128, 256], BF16) for _ in range(2)]
        scm = [al.get([128, 128], BF16) for _ in range(2)]
        onb = [al.get([128, 256], F32) for _ in range(2)]
        ogb = [al.get([128, 256], BF16) for _ in range(2)]
        st6 = al.get([128, 16], F32)
        mv = al.get([128, 8], F32)

        P.dma("sp", lambda: SP.dma_start(out=dl, in_=decay[l:l + 1, :].to_broadcast([128, 16])), writes=["dl"])
        P.op("act", lambda: A.activation(out=dl, in_=dl, func=AF.Exp, scale=-1.0), reads=["dl"], writes=["dl"])
        P.op("act", lambda: A.activation(out=dl, in_=dl, func=AF.Ln, bias=1.0), reads=["dl"], writes=["dl"])
        P.op("dve", lambda: V.tensor_scalar(out=dl, in0=dl, scalar1=-1.0, scalar2=None, op0=ALU.mult),
             reads=["dl"], writes=["dl"])
        P.op("act", lambda: A.activation(out=CDEC, in_=dl, func=AF.Exp, scale=128.0), reads=["dl"], writes=["CDEC"])
        for h in range(NH):
            lf = dl[:, h:h + 1]
            lb = dl[:, 8 + h:9 + h]
            P.op("act", lambda lf=lf: A.activation(out=mtmp[:, 0, :], in_=cst[:, C_DF:C_DF + 128], func=AF.Exp, scale=lf),
                 reads=["dl"], writes=["mtmp0"])
            P.op("act", lambda lb=lb: A.activation(out=mtmp[:, 1, :], in_=cst[:, C_DB:C_DB + 128], func=AF.Exp, scale=lb),
                 reads=["dl"], writes=["mtmp1"])
            P.op("dve", lambda: V.tensor_tensor(out=mtmp[:, 0, :], in0=mtmp[:, 0, :], in1=cst[:, C_TF:C_TF + 128], op=ALU.mult),
                 reads=["mtmp0"], writes=["mtmp0"])
            P.op("dve", lambda: V.tensor_tensor(out=mtmp[:, 1, :], in0=mtmp[:, 1, :], in1=cst[:, C_TB:C_TB + 128], op=ALU.mult),
                 reads=["mtmp1"], writes=["mtmp1"])
            P.op("dve", lambda h=h: V.tensor_tensor(out=MC[:, h, :], in0=mtmp[:, 0, :], in1=mtmp[:, 1, :], op=ALU.add),
                 reads=["mtmp0", "mtmp1"], writes=["MC"])
            P.op("act", lambda h=h, lf=lf: A.activation(out=QDF[:, h, :], in_=cst[:, C_IP1:C_IP1 + 128], func=AF.Exp, scale=lf),
                 reads=["dl"], writes=["QD"])
            P.op("act", lambda h=h, lb=lb: A.activation(out=QDB[:, h, :], in_=cst[:, C_IB:C_IB + 128], func=AF.Exp, scale=lb),
                 reads=["dl"], writes=["QD"])
            P.op("act", lambda h=h, lf=lf: A.activation(out=KD[:, 0, h:h + 1], in_=cst[:, C_KF:C_KF + 1], func=AF.Exp, scale=lf),
                 reads=["dl"], writes=["KD"])
            P.op("act", lambda h=h, lb=lb: A.activation(out=KD[:, 1, h:h + 1], in_=cst[:, C_KB:C_KB + 1], func=AF.Exp, scale=lb),
                 reads=["dl"], writes=["KD"])

        for (r0, N, nch, latent, jt, pidx) in seqs:
            for h in range(NH):
                P.dma("sp", lambda h=h, r0=r0, N=N: SP.dma_start(out=QTh[:, 0:N], in_=QT[h * 128:(h + 1) * 128, r0:r0 + N]),
                      writes=["QTh"])
                P.dma("sp", lambda h=h, r0=r0, N=N: SP.dma_start(out=KTh[:, 0:N], in_=KT[h * 128:(h + 1) * 128, r0:r0 + N]),
                      writes=["KTh"])
                P.dma("sp", lambda h=h, r0=r0, N=N, nch=nch: SP.dma_start(
                    out=Vh[:, 0:nch, :], in_=Vd[r0:r0 + N, h * 256:(h + 1) * 256].rearrange("(c p) v -> p c v", p=128)),
                    writes=["Vh"])
                P.dma("sp", lambda h=h, r0=r0, N=N, nch=nch: SP.dma_start(
                    out=GRh[:, 0:nch, :], in_=GR[r0:r0 + N, h * 256:(h + 1) * 256].rearrange("(c p) v -> p c v", p=128)),
                    writes=["GRh"])
                if latent:
                    P.dma("sp", lambda h=h: SP.dma_start(out=Sf32, in_=s0_in[l, 0, h]), writes=["Sf32"])
                    P.dma("sp", lambda h=h: SP.dma_start(out=Sb32, in_=s0_in[l, 1, h]), writes=["Sb32"])
                else:
                    P.op("pool", lambda: G.memset(Sf32, 0.0), writes=["Sf32"])
                    P.op("pool", lambda: G.memset(Sb32, 0.0), writes=["Sb32"])
                for c0 in range(0, nch, 8):
                    n8 = min(8, nch - c0)
                    bnk = 4 + (c0 // 8) % 2
                    for cc in range(n8):
                        c = c0 + cc
                        P.op("pe", lambda c=c, cc=cc, bnk=bnk: T.transpose(
                            ps_bf(bnk)[:, cc * 128:(cc + 1) * 128], KTh[:, c * 128:(c + 1) * 128], identb),
                            reads=["KTh"], writes=[("ps", bnk)])
                    P.op("dve", lambda c0=c0, n8=n8, bnk=bnk, h=h: V.tensor_scalar(
                        out=Kf[:, c0:c0 + n8, :], in0=ps_bf(bnk)[:, 0:n8 * 128].rearrange("p (c d) -> p c d", d=128),
                        scalar1=KD[:, 0, h:h + 1], scalar2=None, op0=ALU.mult),
                        reads=[("ps", bnk), "KD"], writes=["Kf"])
                    P.op("act", lambda c0=c0, n8=n8, bnk=bnk, h=h: A.activation(
                        out=Kb[:, c0:c0 + n8, :], in_=ps_bf(bnk)[:, 0:n8 * 128].rearrange("p (c d) -> p c d", d=128),
                        func=AF.Copy, scale=KD[:, 1, h:h + 1]),
                        reads=[("ps", bnk), "KD", "Kf"], writes=["Kb"])
                P.op("dve", lambda h=h, nch=nch, N=N: V.tensor_tensor(
                    out=QfT[:, 0:nch, :], in0=QTh[:, 0:N].rearrange("p (c i) -> p c i", i=128),
                    in1=QDF[:, h:h + 1, :].to_broadcast([128, nch, 128]), op=ALU.mult),
                    reads=["QTh", "QD"], writes=["QfT"])
                P.op("pool", lambda h=h, nch=nch, N=N: G.tensor_tensor(
                    out=QbT[:, 0:nch, :], in0=QTh[:, 0:N].rearrange("p (c i) -> p c i", i=128),
                    in1=QDB[:, h:h + 1, :].to_broadcast([128, nch, 128]), op=ALU.mult),
                    reads=["QTh", "QD"], writes=["QbT"])
                P.op("act", lambda nch=nch: A.copy(out=SbAll[:, nch, :], in_=Sb32), reads=["Sb32"], writes=[("SbAll", nch)])
                for c in range(nch - 1, -1, -1):
                    bnk = 6 + (c % 2)
                    P.op("pe", lambda c=c, bnk=bnk: T.matmul(ps_f32(bnk, 256), lhsT=Kb[:, c, :], rhs=Vh[:, c, :],
                                                             start=True, stop=True),
                         reads=["Kb", "Vh"], writes=[("ps", bnk)])
                    P.op("dve", lambda bnk=bnk, h=h: V.scalar_tensor_tensor(
                        out=Sb32, in0=Sb32, scalar=CDEC[:, 8 + h:9 + h], in1=ps_f32(bnk, 256), op0=ALU.mult, op1=ALU.add),
                        reads=[("ps", bnk), "Sb32", "CDEC"], writes=["Sb32"])
                    P.op("act", lambda c=c: A.copy(out=SbAll[:, c, :], in_=Sb32), reads=["Sb32"], writes=[("SbAll", c)])
                if not latent:
                    P.dma("sp", lambda h=h, pidx=pidx: SP.dma_start(out=ns_out[pidx, l, 1, h], in_=Sb32),
                          reads=["Sb32"], writes=[("ns", pidx, 1, h)])
                P.op("act", lambda: A.copy(out=Sfbf[0], in_=Sf32), reads=["Sf32"], writes=[("Sfbf", 0)])
                def stage_a(c):
                    csl = slice(c * 128, (c + 1) * 128)
                    b_sc = c % 2
                    b_o = 2 + c % 2
                    sm = scm[c % 2]
                    P.op("pe", lambda csl=csl, b_sc=b_sc: T.matmul(ps_f32(b_sc, 128), lhsT=KTh[:, csl], rhs=QTh[:, csl],
                                                                 start=True, stop=True),
                         reads=["KTh", "QTh"], writes=[("ps", b_sc)])
                    P.op("dve", lambda b_sc=b_sc, sm=sm, h=h: V.tensor_tensor(out=sm, in0=ps_f32(b_sc, 128), in1=MC[:, h, :],
                                                                              op=ALU.mult),
                         reads=[("ps", b_sc), "MC"], writes=[("scm", c % 2)])
                    P.op("pe", lambda c=c, b_o=b_o, sm=sm: T.matmul(ps_f32(b_o, 256), lhsT=sm, rhs=Vh[:, c, :],
                                                                    start=True, stop=False),
                         reads=[("scm", c % 2), "Vh"], writes=[("ps", b_o)])
                    P.op("pe", lambda c=c, b_o=b_o: T.matmul(ps_f32(b_o, 256), lhsT=QfT[:, c, :], rhs=Sfbf[c % 2],
                                                             start=False, stop=False),
                         reads=["QfT", ("Sfbf", c % 2)], writes=[("ps", b_o)])
                    P.op("pe", lambda c=c, b_o=b_o: T.matmul(ps_f32(b_o, 256), lhsT=QbT[:, c, :], rhs=SbAll[:, c + 1, :],
                                                             start=False, stop=True),
                         reads=["QbT", ("SbAll", c + 1)], writes=[("ps", b_o)])
                    b_s = 6 + c % 2
                    P.op("pe", lambda c=c, b_s=b_s: T.matmul(ps_f32(b_s, 256), lhsT=Kf[:, c, :], rhs=Vh[:, c, :],
                                                             start=True, stop=True),
                         reads=["Kf", "Vh"], writes=[("ps", b_s)])
                    P.op("dve", lambda b_s=b_s, h=h: V.scalar_tensor_tensor(
                        out=Sf32, in0=Sf32, scalar=CDEC[:, h:h + 1], in1=ps_f32(b_s, 256), op0=ALU.mult, op1=ALU.add),
                        reads=[("ps", b_s), "Sf32", "CDEC"], writes=["Sf32"])
                    P.op("act", lambda c=c: A.copy(out=Sfbf[(c + 1) % 2], in_=Sf32),
                         reads=["Sf32"], writes=[("Sfbf", (c + 1) % 2)])
                    st_, mv_ = st6[:, (c % 2) * 8:(c % 2) * 8 + 6], mv[:, (c % 2) * 4:(c % 2) * 4 + 4]
                    P.op("dve", lambda b_o=b_o, st_=st_: V.bn_stats(out=st_, in_=ps_f32(b_o, 256)),
                         reads=[("ps", b_o)], writes=[("st6", c % 2)])
                    P.op("dve", lambda st_=st_, mv_=mv_: V.bn_aggr(out=mv_[:, 0:2], in_=st_), reads=[("st6", c % 2)],
                         writes=[("mv", c % 2)])
                    P.op("act", lambda mv_=mv_: A.activation(out=mv_[:, 3:4], in_=mv_[:, 1:2], func=AF.Sqrt, bias=epsT[:, 1:2]),
                         reads=[("mv", c % 2)], writes=[("mv3", c % 2)])

                def stage_b(c):
                    csl = slice(c * 128, (c + 1) * 128)
                    b_o = 2 + c % 2
                    onx, ogx = onb[c % 2], ogb[c % 2]
                    mv_ = mv[:, (c % 2) * 4:(c % 2) * 4 + 4]
                    P.op("dve", lambda mv_=mv_: V.reciprocal(out=mv_[:, 2:3], in_=mv_[:, 3:4]), reads=[("mv3", c % 2)],
                         writes=[("mv2", c % 2)])
                    P.op("dve", lambda b_o=b_o, onx=onx, mv_=mv_: V.tensor_scalar(
                        out=onx, in0=ps_f32(b_o, 256), scalar1=mv_[:, 0:1], scalar2=mv_[:, 2:3], op0=ALU.subtract, op1=ALU.mult),
                        reads=[("ps", b_o), ("mv", c % 2), ("mv2", c % 2)], writes=[("on", c % 2)])
                    P.op("pool", lambda c=c, onx=onx, ogx=ogx: G.tensor_tensor(out=ogx, in0=onx, in1=GRh[:, c, :], op=ALU.mult),
                         reads=[("on", c % 2), "GRh"], writes=[("og", c % 2)])
                    b_t = 4 + c % 2
                    for hf in range(2):
                        P.op("pe", lambda hf=hf, b_t=b_t, ogx=ogx: T.transpose(
                            ps_bf(b_t)[:, hf * 128:(hf + 1) * 128], ogx[:, hf * 128:(hf + 1) * 128], identb),
                            reads=[("og", c % 2)], writes=[("ps", b_t)])
                    P.op("act", lambda b_t=b_t, csl=csl: A.copy(
                        out=OGh[:, :, csl], in_=ps_bf(b_t)[:, 0:256].rearrange("p (a t) -> p a t", a=2)),
                        reads=[("ps", b_t)], writes=["OGh"])

                for c in range(nch):
                    stage_a(c)
                    if c >= 1:
                        stage_b(c - 1)
                stage_b(nch - 1)
                if not latent:
                    P.dma("sp", lambda h=h, pidx=pidx: SP.dma_start(out=ns_out[pidx, l, 0, h], in_=Sf32),
                          reads=["Sf32"], writes=[("ns", pidx, 0, h)])
                P.dma("sp", lambda h=h, r0=r0, N=N: SP.dma_start(
                    out=OGT[h * 256:(h + 1) * 256, r0:r0 + N].rearrange("(a p) t -> p a t", p=128), in_=OGh[:, :, 0:N]),
                    reads=["OGh"], writes=["OGT"])
        P.barrier()
        if stop_after == "p2b":
            return True

        al = Alloc(PBASE)
        dftc = al.get([128, 2, 512], BF16)
        dftp = al.get([128, 2, 2, 2, 128], BF16) if False else None
        dftp_t = [al.get([128, 2, 2, 128], BF16) for _ in range(2)]
        XFp = al.get([128, 4, NS], BF16)
        PQ = al.get([128, 2, 32, 512], BF16)
        Dp = [al.get([128, 2, 32, 128], BF16) for _ in range(2)]
        ufb = [al.get([128, 512], BF16) for _ in range(2)]
        P.dma("sp", lambda: SP.dma_start(out=dftc, in_=dftc_in), writes=["dftc"])
        for mt in range(2):
            P.dma("sp", lambda mt=mt: SP.dma_start(out=dftp_t[mt], in_=dftp_in[mt]), writes=["dftp"])
        di = 0
        for (r0, N, nch, latent, jt, pidx) in seqs:
            for pair in range(2):
                P.dma("sp", lambda pair=pair, r0=r0, N=N: SP.dma_start(
                    out=XFp[:, :, 0:N], in_=XFT[pair * 512:(pair + 1) * 512, r0:r0 + N].rearrange("(a p) t -> p a t", p=128)),
                    writes=["XFp"])
                for c in range(nch):
                    for gg in range(2):
                        bnk = (c * 2 + gg) % 2
                        for k2 in range(2):
                            P.op("pe", lambda c=c, gg=gg, k2=k2, bnk=bnk: T.matmul(
                                ps_f32(bnk), lhsT=XFp[:, gg * 2 + k2, c * 128:(c + 1) * 128], rhs=dftc[:, k2, :],
                                start=(k2 == 0), stop=(k2 == 1)), reads=["XFp", "dftc"], writes=[("ps", bnk)])
                        P.op("act", lambda c=c, gg=gg, bnk=bnk: A.copy(
                            out=PQ[:, :, c, gg * 256:(gg + 1) * 256], in_=ps_f32(bnk).rearrange("p (a f) -> p a f", a=2)),
                            reads=[("ps", bnk)], writes=["PQ"])
                for mt in range(nch):
                    if latent:
                        dp = Dp[di % 2]
                        dtok = ("Dp", di % 2)
                        di += 1
                        P.dma("sp", lambda mt=mt, dp=dp: SP.dma_start(out=dp, in_=dfts_in[mt]), writes=[dtok])
                    else:
                        dp = dftp_t[mt]
                        dtok = "dftp"
                    bnk = 2 + mt % 2
                    n_mm = 2 * nch
                    i_mm = 0
                    for cs in range(2):
                        for ncn in range(nch):
                            P.op("pe", lambda cs=cs, ncn=ncn, bnk=bnk, dp=dp, i_mm=i_mm, n_mm=n_mm: T.matmul(
                                ps_f32(bnk), lhsT=dp[:, cs, ncn, :], rhs=PQ[:, cs, ncn, :],
                                start=(i_mm == 0), stop=(i_mm == n_mm - 1)), reads=[dtok, "PQ"], writes=[("ps", bnk)])
                            i_mm += 1
                    ub = ufb[mt % 2]
                    P.op("act", lambda bnk=bnk, ub=ub: A.copy(out=ub, in_=ps_f32(bnk)), reads=[("ps", bnk)],
                         writes=[("ufb", mt % 2)])
                    bt = 4 + mt % 2
                    for q4 in range(4):
                        P.op("pe", lambda q4=q4, bt=bt, ub=ub: T.transpose(
                            ps_bf(bt)[:, q4 * 128:(q4 + 1) * 128], ub[:, q4 * 128:(q4 + 1) * 128], identb),
                            reads=[("ufb", mt % 2)], writes=[("ps", bt)])
                    P.op("dve", lambda mt=mt, bt=bt: V.tensor_copy(
                        out=XFp[:, :, mt * 128:(mt + 1) * 128], in_=ps_bf(bt)[:, 0:512].rearrange("p (a t) -> p a t", a=4)),
                        reads=[("ps", bt)], writes=["XFp"])
                P.dma("sp", lambda pair=pair, r0=r0, N=N: SP.dma_start(
                    out=UFT[pair * 512:(pair + 1) * 512, r0:r0 + N].rearrange("(a p) t -> p a t", p=128), in_=XFp[:, :, 0:N]),
                    reads=["XFp"], writes=["UFT"])
        P.barrier()
        if stop_after == "p2c":
            return True

        al = Alloc(PBASE)
        wro = al.get([128, 16, D], BF16)
        wfo = al.get([128, 8, D], BF16)
        wou = al.get([128, 8, D], BF16)
        wr = al.get([128, 8, NE], F32)
        brt = al.get([128, NE], F32)
        g2t = al.get([128, 8], F32)
        vtmp = al.get([128, 2, 8], F32)
        bct = al.get([128, 128], F32)
        G1 = [al.get([128, D], F32) for _ in range(2)]
        A2 = [al.get([128, D], F32) for _ in range(2)]
        B2 = [al.get([128, D], F32) for _ in range(2)]
        OGt = al.get([128, 16, 512], BF16)
        UFt = al.get([128, 8, 512], BF16)
        GAt = al.get([128, 8, 512], BF16)
        GBt = al.get([128, 8, 512], BF16)
        ta_ = [al.get([128, 512], F32)] * 2
        tb_ = [al.get([128, 512], F32)] * 2
        mT = al.get([128, 8, 512], BF16)
        xt2 = [al.get([128, D], F32) for _ in range(2)]
        yt2 = al.get([128, D], F32)
        h2 = al.get([128, D], F32)
        h2b = [al.get([128, D], BF16)] * 2
        h2T = al.get([128, 8, 128], F32)
        junk2 = yt2
        ss2 = al.get([128, 4], F32)
        Lsb = al.get([128, NT, NE], F32)
        P.dma("pool", lambda: G.dma_start(out=wro, in_=w_ret_o[l].rearrange("(kc p) c -> p kc c", p=128)), writes=["wro"])
        P.dma("pool", lambda: G.dma_start(out=wfo, in_=w_four[l].rearrange("(kc p) c -> p kc c", p=128)), writes=["wfo"])
        P.dma("pool", lambda: G.dma_start(out=wou, in_=w_out[l].rearrange("(kc p) c -> p kc c", p=128)), writes=["wou"])
        P.dma("sp", lambda: SP.dma_start(out=wr, in_=w_router[l].rearrange("(kc p) c -> p kc c", p=128)), writes=["wr"])
        P.dma("sp", lambda: SP.dma_start(out=brt, in_=b_router[l:l + 1, :].to_broadcast([128, NE])), writes=["brt"])
        P.dma("sp", lambda: SP.dma_start(out=g2t, in_=g2T[l]), writes=["g2t"])
        for j in range(2):
            P.op("dve", lambda j=j: V.tensor_copy(out=vtmp[:, 0, :], in_=modT[:, 16:24, j]), reads=["bc_dst"], writes=["vecsrc"])
            bcast_row(G1[j], vtmp[:, 0, :], bct, 7)
            P.op("dve", lambda j=j: V.scalar_tensor_tensor(out=vtmp[:, 0, :], in0=modT[:, 32:40, j], scalar=1.0, in1=g2t,
                                                           op0=ALU.add, op1=ALU.mult), reads=["bc_dst", "g2t"], writes=["vecsrc"])
            bcast_row(A2[j], vtmp[:, 0, :], bct, 7)
            P.op("dve", lambda j=j: V.tensor_copy(out=vtmp[:, 0, :], in_=modT[:, 24:32, j]), reads=["bc_dst"], writes=["vecsrc"])
            bcast_row(B2[j], vtmp[:, 0, :], bct, 7)
        for tg in range(10):
            j = 0 if tg < 8 else 1
            tsl = slice(tg * 512, (tg + 1) * 512)
            P.dma("sp", lambda tsl=tsl: SP.dma_start(out=OGt, in_=OGT[:, tsl].rearrange("(kc p) t -> p kc t", p=128)), writes=["OGt"])
            P.dma("sp", lambda tsl=tsl: SP.dma_start(out=UFt, in_=UFT[:, tsl].rearrange("(kc p) t -> p kc t", p=128)), writes=["UFt"])
            P.dma("sp", lambda tsl=tsl: SP.dma_start(out=GAt, in_=GAT[:, tsl].rearrange("(kc p) t -> p kc t", p=128)), writes=["GAt"])
            P.dma("sp", lambda tsl=tsl: SP.dma_start(out=GBt, in_=GBT[:, tsl].rearrange("(kc p) t -> p kc t", p=128)), writes=["GBt"])
            for dc in range(8):
                dsl = slice(dc * 128, (dc + 1) * 128)
                bA, bB = (dc % 2) * 2, (dc % 2) * 2 + 1
                for kc in range(16):
                    P.op("pe", lambda kc=kc, bA=bA, dsl=dsl: T.matmul(ps_f32(bA), lhsT=wro[:, kc, dsl], rhs=OGt[:, kc, :],
                                                                      start=(kc == 0), stop=(kc == 15)),
                         reads=["wro", "OGt"], writes=[("ps", bA)])
                for kc in range(8):
                    P.op("pe", lambda kc=kc, bB=bB, dsl=dsl: T.matmul(ps_f32(bB), lhsT=wfo[:, kc, dsl], rhs=UFt[:, kc, :],
                                                                      start=(kc == 0), stop=(kc == 7)),
                         reads=["wfo", "UFt"], writes=[("ps", bB)])
                ta, tb = ta_[dc % 2], tb_[dc % 2]
                P.op("dve", lambda bA=bA, ta=ta, dc=dc: V.tensor_tensor(out=ta, in0=ps_f32(bA), in1=GAt[:, dc, :], op=ALU.mult),
                     reads=[("ps", bA), "GAt"], writes=["ta"])
                P.op("dve", lambda bB=bB, tb=tb, dc=dc: V.tensor_tensor(out=tb, in0=ps_f32(bB), in1=GBt[:, dc, :], op=ALU.mult),
                     reads=[("ps", bB), "GBt"], writes=["tb"])
                P.op("pool", lambda ta=ta, tb=tb, dc=dc: G.tensor_tensor(out=mT[:, dc, :], in0=ta, in1=tb, op=ALU.add),
                     reads=["ta", "tb"], writes=["mT"])
            for ts in range(4):
                tt = tg * 4 + ts
                xb = xt2[tt % 2]
                hb = h2b[tt % 2]
                P.dma("sp", lambda tt=tt, xb=xb: SP.dma_start(out=xb, in_=Xsrc[tt * 128:(tt + 1) * 128, :]),
                      reads=[("Xs", tt)], writes=[("xt2", tt % 2)])
                for hf in range(2):
                    for kc in range(8):
                        P.op("pe", lambda kc=kc, hf=hf, ts=ts: T.matmul(
                            ps_f32(4 + hf), lhsT=mT[:, kc, ts * 128:(ts + 1) * 128], rhs=wou[:, kc, hf * 512:(hf + 1) * 512],
                            start=(kc == 0), stop=(kc == 7)), reads=["mT", "wou"], writes=[("ps", 4 + hf)])
                P.op("dve", lambda j=j: V.tensor_tensor(out=yt2.rearrange("p (a f) -> p a f", a=2), in0=psum[:, 4:6, :],
                                                        in1=G1[j].rearrange("p (a f) -> p a f", a=2), op=ALU.mult),
                     reads=[("ps", 4), ("ps", 5), "bc_dst"], writes=["yt2"])
                P.op("pool", lambda xb=xb: G.tensor_tensor(out=xb, in0=xb, in1=yt2, op=ALU.add),
                     reads=["yt2", ("xt2", tt % 2)], writes=[("xt2", tt % 2)])
                P.dma("sp", lambda tt=tt, xb=xb: SP.dma_start(out=Xs[tt * 128:(tt + 1) * 128, :], in_=xb),
                      reads=[("xt2", tt % 2)], writes=[("Xs", tt)])
                P.op("act", lambda xb=xb: A.activation(out=junk2, in_=xb, func=AF.Square, accum_out=ss2[:, 0:1]),
                     reads=[("xt2", tt % 2)], writes=["yt2", "ss2"])
                P.op("act", lambda: A.activation(out=ss2[:, 1:2], in_=ss2[:, 0:1], func=AF.Sqrt, scale=1.0 / D, bias=epsT[:, 0:1]),
                     reads=["ss2"], writes=["ss2b"])
                P.op("dve", lambda: V.reciprocal(out=ss2[:, 2:3], in_=ss2[:, 1:2]), reads=["ss2b"], writes=["ss2c"])
                P.op("dve", lambda xb=xb, j=j: V.scalar_tensor_tensor(out=h2, in0=xb, scalar=ss2[:, 2:3], in1=A2[j],
                                                                      op0=ALU.mult, op1=ALU.mult),
                     reads=[("xt2", tt % 2), "ss2c", "bc_dst"], writes=["h2"])
                P.op("pool", lambda j=j: G.tensor_tensor(out=h2, in0=h2, in1=B2[j], op=ALU.add),
                     reads=["h2", "bc_dst"], writes=["h2"])
                P.op("act", lambda hb=hb: A.copy(out=hb, in_=h2), reads=["h2"], writes=["h2b"])
                P.dma("sp", lambda tt=tt, hb=hb: SP.dma_start(out=H2[tt * 128:(tt + 1) * 128, :], in_=hb),
                      reads=["h2b"], writes=["H2"])
                for kc in range(8):
                    P.op("pe", lambda kc=kc: T.matmul(psum[:, 6 + kc // 4, (kc % 4) * 128:(kc % 4 + 1) * 128],
                                                      lhsT=h2[:, kc * 128:(kc + 1) * 128], rhs=ident, start=True, stop=True),
                         reads=["h2"], writes=[("ps", 6 + kc // 4)])
                P.op("dve", lambda: V.tensor_copy(out=h2T.rearrange("p (a k) t -> p a (k t)", a=2), in_=psum[:, 6:8, :]),
                     reads=[("ps", 6), ("ps", 7)], writes=["h2T"])
                for kc in range(8):
                    P.op("pe", lambda kc=kc: T.matmul(psum[:, 6, 0:NE], lhsT=h2T[:, kc, :], rhs=wr[:, kc, :],
                                                      start=(kc == 0), stop=(kc == 7)),
                         reads=["h2T", "wr"], writes=[("ps", 6)])
                P.op("dve", lambda tt=tt: V.tensor_tensor(out=Lsb[:, tt, :], in0=psum[:, 6, 0:NE], in1=brt, op=ALU.add),
                     reads=[("ps", 6), "brt"], writes=["Lsb"])
        P.dma("sp", lambda: SP.dma_start(out=LG, in_=Lsb.rearrange("p a b -> p (a b)")), reads=["Lsb"], writes=["LG"])
        P.barrier()
        if stop_after == "p2d":
            return True

        al = Alloc(PBASE)
        WK = al.get([128, NT, 4], F32)
        SLI = al.get([128, NT, 4], I32)
        BLKI = al.get([128, NBLK], I32)
        IDXW = al.get([128, NBLK, 8], I32)
        IDXB1 = al.get([128, NBLK], I32)
        IDXB2 = al.get([128, NBLK], I32)
        R3END = al.off
        idxf = al.get([128, NBLK, 8], F32)
        idxg = al.get([128, NBLK], F32)
        Lr = al.get([128, NT, NE], F32)
        v8 = al.get([128, NT, 8], F32)
        i8 = al.get([128, NT, 8], U32)
        i8f = al.get([128, NT, 8], F32)
        e4 = al.get([128, NT, 4], F32)
        s4 = al.get([128, NT], F32)
        mask = al.get([128, NT, NE], F32)
        pos = al.get([128, NT, NE], F32)
        tot = al.get([128, NT, NE], F32)
        carry = al.get([128, NT + 1, NE], F32)
        cntv = al.get([128, 4, NE], F32)
        pend = al.get([128, NE], F32)
        slot = al.get([128, NT, NE], F32)
        oh = al.get([128, NT, NE], F32)
        slk = al.get([128, NT, 4], F32)
        cmpb = al.get([128, NBLK, NE], F32)
        blkf = al.get([128, NBLK], F32)
        P.dma("sp", lambda: SP.dma_start(out=Lr.rearrange("p a b -> p (a b)"), in_=LG), writes=["Lr"])
        for tt in range(NT):
            P.op("dve", lambda tt=tt: V.max(out=v8[:, tt, :], in_=Lr[:, tt, :]), reads=["Lr"], writes=["v8"])
            P.op("dve", lambda tt=tt: V.max_index(out=i8[:, tt, :], in_max=v8[:, tt, :], in_values=Lr[:, tt, :]),
                 reads=["Lr", "v8"], writes=["i8"])
        P.op("dve", lambda: V.tensor_copy(out=i8f, in_=i8), reads=["i8"], writes=["i8f"])
        P.op("dve", lambda: V.tensor_tensor(out=e4, in0=v8[:, :, 0:4], in1=v8[:, :, 0:1].to_broadcast([128, NT, 4]),
                                            op=ALU.subtract), reads=["v8"], writes=["e4"])
        P.op("act", lambda: A.activation(out=e4, in_=e4, func=AF.Exp), reads=["e4"], writes=["e4"])
        P.op("dve", lambda: V.tensor_reduce(out=s4, in_=e4, axis=AX.X, op=ALU.add), reads=["e4"], writes=["s4"])
        P.op("dve", lambda: V.reciprocal(out=s4, in_=s4), reads=["s4"], writes=["s4"])
        P.op("dve", lambda: V.tensor_tensor(out=WK, in0=e4, in1=s4.unsqueeze(2).to_broadcast([128, NT, 4]), op=ALU.mult),
             reads=["e4", "s4"], writes=["WK"])
        P.op("dve", lambda: V.tensor_tensor(out=mask, in0=Lr, in1=v8[:, :, 3:4].to_broadcast([128, NT, NE]), op=ALU.is_ge),
             reads=["Lr", "v8"], writes=["mask"])
        mflat = mask.rearrange("p a b -> p (a b)")
        for (c0, cn, bnk) in ((0, 512, 0), (512, 512, 1), (1024, 256, 2)):
            P.op("pe", lambda c0=c0, cn=cn, bnk=bnk: T.matmul(ps_f32(bnk, cn), lhsT=cst[:, C_LTRI:C_LTRI + 128],
                                                              rhs=mflat[:, c0:c0 + cn], start=True, stop=True),
                 reads=["mask"], writes=[("ps", bnk)])
            P.op("act", lambda c0=c0, cn=cn, bnk=bnk: A.copy(out=pos.rearrange("p a b -> p (a b)")[:, c0:c0 + cn],
                                                             in_=ps_f32(bnk, cn)), reads=[("ps", bnk)], writes=["pos"])
            P.op("pe", lambda c0=c0, cn=cn, bnk=bnk: T.matmul(ps_f32(bnk + 3, cn), lhsT=ones, rhs=mflat[:, c0:c0 + cn],
                                                              start=True, stop=True),
                 reads=["mask"], writes=[("ps", bnk + 3)])
            P.op("act", lambda c0=c0, cn=cn, bnk=bnk: A.copy(out=tot.rearrange("p a b -> p (a b)")[:, c0:c0 + cn],
                                                             in_=ps_f32(bnk + 3, cn)), reads=[("ps", bnk + 3)], writes=["tot"])
        P.op("dve", lambda: V.memset(carry[:, 0, :], 0.0), writes=["carry"])
        for tt in range(NT):
            P.op("dve", lambda tt=tt: V.tensor_tensor(out=carry[:, tt + 1, :], in0=carry[:, tt, :], in1=tot[:, tt, :], op=ALU.add),
                 reads=["tot", "carry"], writes=["carry"])
        cnt_ = carry[:, NT, :]
        cnti = cntv.bitcast(I32)
        P.op("dve", lambda: V.tensor_copy(out=cnti[:, 0, :], in_=cnt_), reads=["carry"], writes=["cv0"])
        P.op("dve", lambda: V.tensor_scalar(out=cnti[:, 1, :], in0=cnti[:, 0, :], scalar1=BLK - 1, scalar2=None, op0=ALU.add),
             reads=["cv0"], writes=["cv1"])
        P.op("dve", lambda: V.tensor_scalar(out=cnti[:, 2, :], in0=cnti[:, 1, :], scalar1=9, scalar2=9,
                                            op0=ALU.arith_shift_right, op1=ALU.logical_shift_left), reads=["cv1"], writes=["cv2"])
        P.op("dve", lambda: V.tensor_copy(out=cntv[:, 3, :], in_=cnti[:, 2, :]), reads=["cv2"], writes=["cv3"])
        P.op("dve", lambda: V.tensor_copy(out=pend[:, 0:1], in_=cntv[:, 3, 0:1]), reads=["cv3"], writes=["pend"])
        for e in range(1, NE):
            P.op("dve", lambda e=e: V.tensor_tensor(out=pend[:, e:e + 1], in0=pend[:, e - 1:e], in1=cntv[:, 3, e:e + 1], op=ALU.add),
                 reads=["pend", "cv3"], writes=["pend"])
        P.op("dve", lambda: V.tensor_tensor(out=cntv[:, 0, :], in0=pend, in1=cntv[:, 3, :], op=ALU.subtract),
             reads=["pend", "cv3", "cv1"], writes=["cv0"])
        P.op("dve", lambda: V.tensor_tensor(out=pos, in0=pos, in1=carry[:, 0:NT, :], op=ALU.add),
             reads=["pos", "carry"], writes=["pos"])
        P.op("dve", lambda: V.tensor_tensor(out=slot, in0=pos, in1=cntv[:, 0:1, :].to_broadcast([128, NT, NE]), op=ALU.add),
             reads=["pos", "cv0"], writes=["slot"])
        for k in range(4):
            P.op("dve", lambda k=k: V.tensor_tensor(
                out=oh, in0=cst[:, C_IOTA:C_IOTA + NE].unsqueeze(1).to_broadcast([128, NT, NE]),
                in1=i8f[:, :, k:k + 1].to_broadcast([128, NT, NE]), op=ALU.is_equal), reads=["i8f", "slk"], writes=["oh"])
            P.op("dve", lambda: V.tensor_tensor(out=oh, in0=oh, in1=slot, op=ALU.mult), reads=["oh", "slot"], writes=["oh"])
            P.op("dve", lambda k=k: V.tensor_reduce(out=slk[:, :, k], in_=oh, axis=AX.X, op=ALU.add),
                 reads=["oh"], writes=["slk"])
        P.op("dve", lambda: V.tensor_copy(out=SLI, in_=slk), reads=["slk"], writes=["SLI"])
        P.op("dve", lambda: V.tensor_tensor(
            out=cmpb, in0=pend.unsqueeze(1).to_broadcast([128, NBLK, NE]),
            in1=cst[:, C_BC:C_BC + NBLK].unsqueeze(2).to_broadcast([128, NBLK, NE]), op=ALU.is_le),
            reads=["pend"], writes=["cmpb"])
        P.op("dve", lambda: V.tensor_reduce(out=blkf, in_=cmpb, axis=AX.X, op=ALU.add), reads=["cmpb"], writes=["blkf"])
        P.op("dve", lambda: V.tensor_scalar(out=blkf, in0=blkf, scalar1=float(NE - 1), scalar2=None, op0=ALU.min),
             reads=["blkf"], writes=["blkf"])
        P.op("dve", lambda: V.tensor_copy(out=BLKI, in_=blkf), reads=["blkf"], writes=["BLKI"])
        P.op("dve", lambda: V.tensor_scalar(out=idxg, in0=blkf, scalar1=1024.0, scalar2=float(l * NE * 1024),
                                            op0=ALU.mult, op1=ALU.add), reads=["blkf"], writes=["idxg"])
        P.op("dve", lambda: V.tensor_tensor(out=idxf, in0=idxg.unsqueeze(2).to_broadcast([128, NBLK, 8]),
                                            in1=cst[:, C_KCP:C_KCP + 8].unsqueeze(1).to_broadcast([128, NBLK, 8]), op=ALU.add),
             reads=["idxg"], writes=["idxf"])
        P.op("dve", lambda: V.tensor_copy(out=IDXW, in_=idxf), reads=["idxf"], writes=["IDXW"])
        P.op("dve", lambda: V.tensor_scalar(out=idxg, in0=blkf, scalar1=128.0, scalar2=float(l * NE * 128),
                                            op0=ALU.mult, op1=ALU.add), reads=["blkf", "idxf"], writes=["idxg"])
        P.op("dve", lambda: V.tensor_tensor(out=idxg, in0=idxg, in1=cst[:, C_KB:C_KB + 1].to_broadcast([128, NBLK]), op=ALU.add),
             reads=["idxg"], writes=["idxg"])
        P.op("dve", lambda: V.tensor_copy(out=IDXB1, in_=idxg), reads=["idxg"], writes=["IDXB1"])
        P.op("dve", lambda: V.tensor_scalar(out=idxg, in0=blkf, scalar1=float(l * NE), scalar2=None, op0=ALU.add),
             reads=["blkf", "IDXB1"], writes=["idxg"])
        P.op("dve", lambda: V.tensor_copy(out=IDXB2, in_=idxg), reads=["idxg"], writes=["IDXB2"])
        P.dma("sp", lambda: SP.dma_start(out=SLd, in_=SLI.rearrange("p a b -> p (a b)")), reads=["SLI"], writes=["SLd"])

        htk = [al.get([128, D], BF16) for _ in range(4)]
        R3C = R3END
        for tt in range(NT):
            hb = htk[tt % 4]
            P.dma("sp", lambda tt=tt, hb=hb: SP.dma_start(out=hb, in_=H2[tt * 128:(tt + 1) * 128, :]), writes=[("htk", tt % 4)])
            for k in range(4):
                P.dma("pool", lambda tt=tt, k=k, hb=hb: G.indirect_dma_start(
                    out=XP, out_offset=bass.IndirectOffsetOnAxis(ap=SLI[:, tt, k:k + 1], axis=0),
                    in_=hb, in_offset=None, bounds_check=REG["xp"], oob_is_err=False),
                    reads=[("htk", tt % 4), "SLI"], writes=["XP"])
        if stop_after in ("p3c", "p3d"):
            di = dbgo.bitcast(I32)
            P.dma("sp", lambda: SP.dma_start(out=dbgo[:, 0:160], in_=WK.rearrange("p a b -> p (a b)")), reads=["WK"], writes=["dbgo"])
            P.dma("sp", lambda: SP.dma_start(out=di[:, 256:256 + NBLK], in_=BLKI), reads=["BLKI"], writes=["dbgo1"])
            P.dma("sp", lambda: SP.dma_start(out=di[:, 512:512 + NBLK * 8], in_=IDXW.rearrange("p a b -> p (a b)")), reads=["IDXW"], writes=["dbgo2"])
            P.dma("sp", lambda: SP.dma_start(out=di[:, 1100:1100 + NBLK], in_=IDXB1), reads=["IDXB1"], writes=["dbgo3"])
            P.dma("sp", lambda: SP.dma_start(out=di[:, 1200:1200 + NBLK], in_=IDXB2), reads=["IDXB2"], writes=["dbgo4"])
            P.dma("sp", lambda: SP.dma_start(out=dbgo[:, 1300:1332], in_=pend), reads=["pend"], writes=["dbgo5"])
            P.dma("sp", lambda: SP.dma_start(out=dbgo[:, 1400:1432], in_=carry[:, NT, :]), reads=["carry"], writes=["dbgo6"])
        P.barrier()
        if stop_after == "p3c":
            return True

        al = Alloc(R3C)
        w1b = [al.get([128, 8, 2 * DE], BF16) for _ in range(2)]
        w2b = [al.get([128, 8, D], BF16) for _ in range(2)]
        b1t = [al.get([128, 16], F32) for _ in range(2)]
        b2t = [al.get([128, D], F32) for _ in range(2)]
        xtok2 = [al.get([128, 4, D], BF16) for _ in range(2)]
        xbT = al.get([128, 8, 512], BF16)
        actT = al.get([128, 8, 512], BF16)
        gA = [al.get([128, 512], F32) for _ in range(3)]
        sA = [al.get([128, 512], F32) for _ in range(3)]
        lA = [al.get([128, 512], F32) for _ in range(3)]
        ytk = al.get([128, 4, D], F32)
        def issue_loads(b):
            wi = b % 2
            for kc in range(8):
                P.dma("pool", lambda b=b, wi=wi, kc=kc: G.indirect_dma_start(
                    out=w1b[wi][:, kc, :], out_offset=None, in_=w1.rearrange("l e r c -> (l e r) c"),
                    in_offset=bass.IndirectOffsetOnAxis(ap=IDXW[:, b, kc:kc + 1], axis=0),
                    bounds_check=REG["w"], oob_is_err=False), writes=[("w1b", wi, kc // 2)])
            for kc in range(8):
                P.dma("pool", lambda b=b, wi=wi, kc=kc: G.indirect_dma_start(
                    out=w2b[wi][:, kc, :], out_offset=None, in_=w2.rearrange("l e r c -> (l e r) c"),
                    in_offset=bass.IndirectOffsetOnAxis(ap=IDXW[:, b, kc:kc + 1], axis=0),
                    bounds_check=REG["w"], oob_is_err=False), writes=[("w2b", wi, kc // 4)])
            P.dma("pool", lambda b=b, wi=wi: G.indirect_dma_start(
                out=b1t[wi], out_offset=None, in_=b1T.rearrange("l e p c -> (l e p) c"),
                in_offset=bass.IndirectOffsetOnAxis(ap=IDXB1[:, b:b + 1], axis=0),
                bounds_check=REG["b1"], oob_is_err=False), writes=[("b1t", wi)])
            P.dma("pool", lambda b=b, wi=wi: G.indirect_dma_start(
                out=b2t[wi], out_offset=None, in_=b2.rearrange("l e c -> (l e) c"),
                in_offset=bass.IndirectOffsetOnAxis(ap=IDXB2[:, b:b + 1], axis=0),
                bounds_check=REG["b2"], oob_is_err=False), writes=[("b2t", wi)])
            P.dma("sp", lambda b=b, wi=wi: SP.dma_start(
                out=xtok2[wi], in_=XP[b * BLK:(b + 1) * BLK, :].rearrange("(s p) d -> p s d", p=128)),
                writes=[("xtok", wi)])

        issue_loads(0)
        for b in range(NBLK):
            wi = b % 2
            xtok = xtok2[wi]
            if b + 1 < NBLK:
                issue_loads(b + 1)
            for s in range(4):
                bnk = s % 2
                for kc in range(8):
                    P.op("pe", lambda s=s, kc=kc, bnk=bnk, xtok=xtok: T.transpose(
                        ps_bf(bnk)[:, kc * 128:(kc + 1) * 128], xtok[:, s, kc * 128:(kc + 1) * 128], identb),
                        reads=[("xtok", wi)], writes=[("ps", bnk)])
                P.op("act", lambda s=s, bnk=bnk: A.copy(out=xbT[:, :, s * 128:(s + 1) * 128],
                                                        in_=ps_bf(bnk).rearrange("p (k t) -> p k t", k=8)),
                     reads=[("ps", bnk)], writes=["xbT"])
            w1r = [("w1b", wi, q) for q in range(4)]
            w2r = [("w2b", wi, q) for q in range(2)]
            for mc in range(8):
                bG, bL = ((2, 3), (4, 5), (0, 1))[mc % 3]
                g_, s_, l_ = gA[mc % 3], sA[mc % 3], lA[mc % 3]
                for kc in range(8):
                    P.op("pe", lambda kc=kc, mc=mc, bG=bG, wi=wi: T.matmul(
                        ps_f32(bG), lhsT=w1b[wi][:, kc, mc * 128:(mc + 1) * 128], rhs=xbT[:, kc, :],
                        start=(kc == 0), stop=(kc == 7)), reads=w1r + ["xbT"], writes=[("ps", bG)])
                for kc in range(8):
                    P.op("pe", lambda kc=kc, mc=mc, bL=bL, wi=wi: T.matmul(
                        ps_f32(bL), lhsT=w1b[wi][:, kc, DE + mc * 128:DE + (mc + 1) * 128], rhs=xbT[:, kc, :],
                        start=(kc == 0), stop=(kc == 7)), reads=w1r + ["xbT"], writes=[("ps", bL)])
                P.op("dve", lambda mc=mc, bG=bG, g_=g_, wi=wi: V.tensor_scalar(
                    out=g_, in0=ps_f32(bG), scalar1=b1t[wi][:, mc:mc + 1], scalar2=7.0, op0=ALU.add, op1=ALU.min),
                    reads=[("ps", bG), ("b1t", wi)], writes=[("gA", mc % 3)])
                P.op("act", lambda g_=g_, s_=s_: A.activation(out=s_, in_=g_, func=AF.Sigmoid, scale=1.702),
                     reads=[("gA", mc % 3)], writes=[("sA", mc % 3)])
                P.op("dve", lambda mc=mc, bL=bL, l_=l_, wi=wi: V.tensor_scalar(
                    out=l_, in0=ps_f32(bL), scalar1=b1t[wi][:, 8 + mc:9 + mc], scalar2=7.0, op0=ALU.add, op1=ALU.min),
                    reads=[("ps", bL), ("b1t", wi)], writes=[("lA", mc % 3)])
                P.op("dve", lambda l_=l_: V.tensor_scalar(out=l_, in0=l_, scalar1=-7.0, scalar2=1.0, op0=ALU.max, op1=ALU.add),
                     reads=[("lA", mc % 3)], writes=[("lA", mc % 3)])
                P.op("dve", lambda g_=g_, s_=s_: V.tensor_tensor(out=s_, in0=g_, in1=s_, op=ALU.mult),
                     reads=[("gA", mc % 3), ("sA", mc % 3)], writes=[("sA", mc % 3)])
                P.op("dve", lambda mc=mc, l_=l_, s_=s_: V.tensor_tensor(out=actT[:, mc, :], in0=l_, in1=s_, op=ALU.mult),
                     reads=[("lA", mc % 3), ("sA", mc % 3)], writes=["actT"])
            for s in range(4):
                b0 = 6
                for hf in range(2):
                    for kc in range(8):
                        P.op("pe", lambda kc=kc, hf=hf, s=s, wi=wi: T.matmul(
                            ps_f32(6 + hf), lhsT=actT[:, kc, s * 128:(s + 1) * 128], rhs=w2b[wi][:, kc, hf * 512:(hf + 1) * 512],
                            start=(kc == 0), stop=(kc == 7)), reads=w2r + ["actT"], writes=[("ps", 6 + hf)])
                P.op("dve", lambda s=s, wi=wi: V.tensor_tensor(out=ytk[:, s, :].rearrange("p (a f) -> p a f", a=2),
                                                               in0=psum[:, 6:8, :],
                                                               in1=b2t[wi].rearrange("p (a f) -> p a f", a=2), op=ALU.add),
                     reads=[("ps", 6), ("ps", 7), ("b2t", wi)], writes=["ytk"])
            P.dma("sp", lambda b=b: SP.dma_start(out=Yd[b * BLK:(b + 1) * BLK, :].rearrange("(s p) d -> p s d", p=128), in_=ytk),
                  reads=["ytk"], writes=["Yd"])
        P.barrier()
        if stop_after == "p3d":
            return True

        al = Alloc(R3END)
        g2row = al.get([128, 2, 8], F32)
        bct2 = al.get([128, 128], F32)
        G2 = [al.get([128, D], F32) for _ in range(2)]
        FG = al.get([128, D], F32)
        yk = [al.get([128, 4, D], F32) for _ in range(2)]
        xt3 = [al.get([128, D], F32) for _ in range(2)]
        acc = al.get([128, D], F32)
        junk3 = al.get([128, D], F32)
        ss3 = al.get([128, 4], F32)
        for j in range(2):
            P.op("dve", lambda j=j: V.tensor_copy(out=g2row[:, 0, :], in_=modT[:, 40:48, j]), reads=["bc_dst"], writes=["vecsrc"])
            bcast_row(G2[j], g2row[:, 0, :], bct2, 7)
        if l == DEPTH - 1:
            P.dma("sp", lambda: SP.dma_start(out=FG, in_=final_g.to_broadcast([128, D])), writes=["FG"])
        for tt in range(NT):
            j = 0 if tt < 32 else 1
            ykb = yk[tt % 2]
            xb = xt3[tt % 2]
            for k in range(4):
                P.dma("pool", lambda tt=tt, k=k, ykb=ykb: G.indirect_dma_start(
                    out=ykb[:, k, :], out_offset=None, in_=Yd,
                    in_offset=bass.IndirectOffsetOnAxis(ap=SLI[:, tt, k:k + 1], axis=0),
                    bounds_check=REG["xp"], oob_is_err=False), writes=[("yk", tt % 2, k)])
            P.dma("sp", lambda tt=tt, xb=xb: SP.dma_start(out=xb, in_=Xs[tt * 128:(tt + 1) * 128, :]),
                  reads=[("Xs", tt)], writes=[("xt3", tt % 2)])
            P.op("dve", lambda tt=tt, ykb=ykb: V.tensor_scalar(out=acc, in0=ykb[:, 0, :], scalar1=WK[:, tt, 0:1], scalar2=None,
                                                               op0=ALU.mult), reads=[("yk", tt % 2, 0)], writes=["acc"])
            for k in range(1, 4):
                P.op("dve", lambda tt=tt, k=k, ykb=ykb: V.scalar_tensor_tensor(
                    out=acc, in0=ykb[:, k, :], scalar=WK[:, tt, k:k + 1], in1=acc, op0=ALU.mult, op1=ALU.add),
                    reads=[("yk", tt % 2, k), "acc"], writes=["acc"])
            P.op("pool", lambda j=j: G.tensor_tensor(out=acc, in0=acc, in1=G2[j], op=ALU.mult), reads=["acc", "bc_dst"], writes=["acc"])
            P.op("dve", lambda xb=xb: V.tensor_tensor(out=xb, in0=xb, in1=acc, op=ALU.add),
                 reads=["acc", ("xt3", tt % 2)], writes=[("xt3", tt % 2)])
            if l < DEPTH - 1:
                P.dma("sp", lambda tt=tt, xb=xb: SP.dma_start(out=Xs[tt * 128:(tt + 1) * 128, :], in_=xb),
                      reads=[("xt3", tt % 2)], writes=[("Xs", tt)])
            else:
                P.op("act", lambda xb=xb: A.activation(out=junk3, in_=xb, func=AF.Square, accum_out=ss3[:, 0:1]),
                     reads=[("xt3", tt % 2)], writes=["junk3", "ss3"])
                P.op("act", lambda: A.activation(out=ss3[:, 1:2], in_=ss3[:, 0:1], func=AF.Sqrt, scale=1.0 / D, bias=epsT[:, 0:1]),
                     reads=["ss3"], writes=["ss3b"])
                P.op("dve", lambda: V.reciprocal(out=ss3[:, 2:3], in_=ss3[:, 1:2]), reads=["ss3b"], writes=["ss3c"])
                P.op("dve", lambda xb=xb: V.scalar_tensor_tensor(out=xb, in0=xb, scalar=ss3[:, 2:3], in1=FG,
                                                                 op0=ALU.mult, op1=ALU.mult),
                     reads=[("xt3", tt % 2), "ss3c", "FG"], writes=[("xt3", tt % 2)])
                P.dma("sp", lambda tt=tt, xb=xb: SP.dma_start(out=y_out[tt * 128:(tt + 1) * 128, :], in_=xb),
                      reads=[("xt3", tt % 2)], writes=[("yo", tt)])
        P.barrier()
        return False

    for l in range(DEPTH):
        if do_layer(l):
            break
    P.barrier()
    P.emit()
    return nc, stack


_CONST_CACHE = {}


def _host_consts():
    if not _CONST_CACHE:
        _CONST_CACHE["cst"] = _consts()
        _CONST_CACHE["rot"] = _rot_tables()
        _CONST_CACHE["dftc"] = _dft_chan()
        _CONST_CACHE["dfts"] = _dft_seq(NS)
        _CONST_CACHE["dftp"] = _dft_seq(256)
    return _CONST_CACHE


def make_in_maps(inp, ncores=8):
    f = lambda a: np.ascontiguousarray(np.asarray(a, dtype=np.float32))
    hc = _host_consts()
    shared = {
        "w_ada": f(inp["w_ada"]),
        "b_adaT": f(np.asarray(inp["b_ada"]).reshape(DEPTH, 48, 128).transpose(0, 2, 1)),
        "g1T": f(np.asarray(inp["norm1_g"]).reshape(DEPTH, 8, 128).transpose(0, 2, 1)),
        "g2T": f(np.asarray(inp["norm2_g"]).reshape(DEPTH, 8, 128).transpose(0, 2, 1)),
        "w_in": f(inp["w_in"]),
        "decay": f(np.asarray(inp["ret_decay_logit"]).reshape(DEPTH, 16)),
        "w_ret_o": f(inp["w_ret_o"]),
        "w_four": f(inp["w_four"]),
        "w_out": f(inp["w_out"]),
        "w_router": f(inp["w_router"]),
        "b_router": f(inp["b_router"]),
        "w1": f(inp["w1"]),
        "b1T": f(np.asarray(inp["b1"]).reshape(DEPTH, NE, 16, 128).transpose(0, 1, 3, 2)),
        "w2": f(inp["w2"]),
        "b2": f(inp["b2"]),
        "final_g": f(np.asarray(inp["final_g"]).reshape(1, D)),
        "cst": hc["cst"], "rot": hc["rot"], "dftc": hc["dftc"], "dfts": hc["dfts"], "dftp": hc["dftp"],
    }
    xp = np.asarray(inp["x_prompt"], np.float32)
    xs = np.asarray(inp["x_sample"], np.float32)
    st = np.asarray(inp["state_ret"], np.float32)
    c = np.asarray(inp["c"], np.float32)
    cc = np.asarray(inp["c_ctx"], np.float32)
    maps = []
    for i in range(ncores):
        m = dict(shared)
        m["x_in"] = np.ascontiguousarray(np.concatenate([xs[i], xp[4 * i:4 * i + 4].reshape(1024, D)], axis=0))
        m["s0"] = np.ascontiguousarray(st[i])
        cT = np.stack([c[i].reshape(8, 128).T, cc.reshape(8, 128).T], axis=-1)
        m["cT"] = np.ascontiguousarray(cT.astype(np.float32))
        maps.append(m)
    return maps


def kernel(**inputs):
    nc, stack = build()
    try:
        maps = make_in_maps(inputs, 8)
        res = run_bass_kernel_spmd(nc, maps, core_ids=list(range(8)))
    finally:
        stack.close()
    y_prompt = np.zeros((32, 256, D), np.float32)
    y_sample = np.zeros((8, NS, D), np.float32)
    new_state = np.zeros((32, DEPTH, 2, NH, DK, DV), np.float32)
    for i, r in enumerate(res.results):
        yo = np.asarray(r["y_out"])
        y_sample[i] = yo[:NS]
        y_prompt[4 * i:4 * i + 4] = yo[NS:].reshape(4, 256, D)
        new_state[4 * i:4 * i + 4] = np.asarray(r["ns_out"])
    return (y_prompt, y_sample, new_state)
```

```python
import os
from contextlib import ExitStack
import numpy as np
import ml_dtypes
import concourse.bass as bass
import concourse.mybir as mybir
from concourse.bass_utils import run_bass_kernel_spmd

F32 = mybir.dt.float32
BF16 = mybir.dt.bfloat16
I32 = mybir.dt.int32
U32 = mybir.dt.uint32
AF = mybir.ActivationFunctionType
ALU = mybir.AluOpType
AX = mybir.AxisListType

D = 1024
NTOK = 5120
NT = 40
NS = 4096
NH = 8
DK = 128
DV = 256
INW = 9216
NE = 32
DE = 1024
BLK = 512
NBLK = 72
PT = NBLK * BLK
EPS = 1e-6
GN_EPS = 1e-5
DEPTH = 2

C_ID, C_DF, C_TF, C_DB, C_TB, C_IP1, C_IB, C_LTRI, C_ONES = [i * 128 for i in range(9)]
C_KF = 9 * 128
C_KB = C_KF + 1
C_IOTA = C_KB + 1
C_BC = C_IOTA + 32
C_KCP = C_BC + NBLK
C_N = C_KCP + 8


def _consts():
    c = np.zeros((128, C_N), np.float32)
    p = np.arange(128)
    j = p[:, None].astype(np.float64)
    i = p[None, :].astype(np.float64)
    c[:, C_ID:C_ID + 128] = np.eye(128)
    c[:, C_DF:C_DF + 128] = np.maximum(i - j, 0)
    c[:, C_TF:C_TF + 128] = (i >= j)
    c[:, C_DB:C_DB + 128] = np.maximum(j - i, 0)
    c[:, C_TB:C_TB + 128] = (j >= i)
    c[:, C_IP1:C_IP1 + 128] = i + 1
    c[:, C_IB:C_IB + 128] = 128 - i
    c[:, C_LTRI:C_LTRI + 128] = (j < i)
    c[:, C_ONES:C_ONES + 128] = 1.0
    c[:, C_KF] = 127 - p
    c[:, C_KB] = p
    c[:, C_IOTA:C_IOTA + 32] = np.arange(32)[None, :]
    c[:, C_BC:C_BC + NBLK] = (np.arange(NBLK) * BLK)[None, :]
    c[:, C_KCP:C_KCP + 8] = np.arange(8)[None, :] * 128 + p[:, None]
    return c


def _rot_tables():
    t = np.arange(NS)
    row = (t // 64).astype(np.float32)
    col = (t % 64).astype(np.float32)
    nf = 32
    inv = (np.float32(10000.0) ** (-(np.arange(nf, dtype=np.float32)) / np.float32(nf))).astype(np.float32)
    ang = np.concatenate([row[:, None] * inv[None, :], col[:, None] * inv[None, :]], axis=1)
    ang = ang.astype(np.float64)
    cs = np.cos(ang).T
    sn = np.sin(ang).T
    tab = np.zeros((128, 2, NS), np.float32)
    tab[:64, 0] = cs
    tab[64:, 0] = cs
    tab[:64, 1] = sn
    tab[64:, 1] = sn
    return tab


def _dft_chan():
    c = np.arange(256)
    m = (c[:, None] * c[None, :]) % 256
    a = 2.0 * np.pi * m / 256.0
    cs = np.cos(a) / 16.0
    sn = -np.sin(a) / 16.0
    full = np.concatenate([cs, sn], axis=1)
    return full.reshape(2, 128, 512).transpose(1, 0, 2).astype(ml_dtypes.bfloat16)


def _dft_seq(n):
    nch = n // 128
    idx = np.arange(n, dtype=np.int64)
    m = (idx[:, None] * idx[None, :]) % n
    a = 2.0 * np.pi * m.astype(np.float64) / n
    sc = 1.0 / np.sqrt(n)
    out = np.zeros((nch, 128, 2, nch, 128), ml_dtypes.bfloat16)
    for cs, f in ((0, np.cos), (1, np.sin)):
        mat = (f(a) * sc).astype(np.float32)
        out[:, :, cs] = mat.reshape(nch, 128, nch, 128).transpose(2, 1, 0, 3).astype(ml_dtypes.bfloat16)
    return out


class Prog:
    CE = ("pe", "act", "dve", "pool")
    ALL = ("pe", "act", "dve", "pool", "sp")
    R = 10

    def __init__(self, nc, stack):
        self.nc = nc
        self.stack = stack
        self.ops = {e: [] for e in self.ALL}
        self.sems = {}
        self.phase = 0
        self.cnt = {e: 0 for e in self.CE}
        self.dcnt = {"sp": 0, "pool": 0}
        self.lastw = {}
        self.readers = {}
        self.waited = {e: {} for e in self.ALL}
        self.nsem = 0
        self.pool_init = None
        for q in self.dcnt:
            for s in range(self.R):
                self._mk(("d", q, s))
        self._new_phase()

    def _mk(self, key):
        self.sems[key] = self.stack.enter_context(self.nc.semaphore("s%d" % self.nsem))
        self.nsem += 1

    def _new_phase(self):
        self.phase += 1
        for e in self.CE:
            self.cnt[e] = 0
            self._mk(("c", e, self.phase))

    def _ck(self, e):
        return ("c", e, self.phase)

    def _deps(self, eng, reads, writes):
        deps = {}

        def add(ev):
            if ev is None:
                return
            k, v = ev
            if deps.get(k, 0) < v:
                deps[k] = v
        for t in reads:
            add(self.lastw.get(t))
        for t in writes:
            add(self.lastw.get(t))
            for k, v in self.readers.get(t, {}).items():
                add((k, v))
        waits = []
        for k, v in deps.items():
            if eng == "pe" and k == self._ck("pe"):
                continue
            if self.waited[eng].get(k, 0) >= v:
                continue
            self.waited[eng][k] = v
            waits.append((k, v))
        return waits

    def _post(self, ev, reads, writes):
        k, v = ev
        for t in reads:
            r = self.readers.setdefault(t, {})
            if r.get(k, 0) < v:
                r[k] = v
        for t in writes:
            self.lastw[t] = ev
            self.readers[t] = {}

    def op(self, eng, fn, reads=(), writes=()):
        waits = self._deps(eng, reads, writes)
        self.cnt[eng] += 1
        ev = (self._ck(eng), self.cnt[eng])
        self.ops[eng].append((waits, fn, ev[0], 1))
        self._post(ev, reads, writes)

    def dma(self, q, fn, reads=(), writes=()):
        waits = self._deps(q, reads, writes)
        j = self.dcnt[q]
        self.dcnt[q] += 1
        slot, gen = j % self.R, j // self.R
        key = ("d", q, slot)
        if gen > 0 and self.waited[q].get(key, 0) < 16 * gen:
            self.waited[q][key] = 16 * gen
            waits.append((key, 16 * gen))
        ev = (key, 16 * (gen + 1))
        self.ops[q].append((waits, fn, key, 16))
        self._post(ev, reads, writes)

    def barrier(self):
        evs = []
        for e in self.CE:
            if self.cnt[e] > 0:
                evs.append((self._ck(e), self.cnt[e]))
        for q, n in self.dcnt.items():
            for s in range(self.R):
                if n > s:
                    last = ((n - 1 - s) // self.R) + 1
                    evs.append((("d", q, s), 16 * last))
        for e in self.ALL:
            waits = []
            for k, v in evs:
                if self.waited[e].get(k, 0) >= v:
                    continue
                self.waited[e][k] = v
                waits.append((k, v))
            if waits:
                self.ops[e].append((waits, None, None, 0))
        self.lastw = {}
        self.readers = {}
        self._new_phase()

    def emit(self):
        nc = self.nc
        engs = {"pe": nc.tensor, "act": nc.scalar, "dve": nc.vector, "pool": nc.gpsimd, "sp": nc.sync}
        with nc.Block() as block:
            def run(name):
                eng = engs[name]
                for waits, fn, key, inc in self.ops[name]:
                    for k, v in waits:
                        eng.wait_ge(self.sems[k], v)
                    if fn is not None:
                        ins = fn()
                        ins.then_inc(self.sems[key], inc)

            @block.tensor
            def _(e):
                run("pe")

            @block.scalar
            def _(e):
                run("act")

            @block.vector
            def _(e):
                run("dve")

            @block.gpsimd
            def _(e):
                if self.pool_init is not None:
                    self.pool_init()
                run("pool")

            @block.sync
            def _(e):
                run("sp")


def build(debug=False, stop_after=None, dbg_names=()):
    nc = bass.Bass("TRN2", target_bir_lowering=False)
    stack = ExitStack()
    dbg_kind = "ExternalOutput" if debug else "Internal"

    def din(name, shape, dt=F32):
        return nc.dram_tensor(name, list(shape), dt, kind="ExternalInput").ap()

    def dscr(name, shape, dt, dbg=False):
        return nc.dram_tensor(name, list(shape), dt, kind=("ExternalOutput" if name in dbg_names else "Internal")).ap()

    x_in = din("x_in", [NTOK, D])
    s0_in = din("s0", [DEPTH, 2, NH, DK, DV])
    cT_in = din("cT", [128, 8, 2])
    w_ada = din("w_ada", [DEPTH, D, 6 * D])
    b_adaT = din("b_adaT", [DEPTH, 128, 48])
    g1T = din("g1T", [DEPTH, 128, 8])
    g2T = din("g2T", [DEPTH, 128, 8])
    w_in = din("w_in", [DEPTH, D, INW])
    decay = din("decay", [DEPTH, 16])
    w_ret_o = din("w_ret_o", [DEPTH, 2048, D])
    w_four = din("w_four", [DEPTH, D, D])
    w_out = din("w_out", [DEPTH, D, D])
    w_router = din("w_router", [DEPTH, D, NE])
    b_router = din("b_router", [DEPTH, NE])
    w1 = din("w1", [DEPTH, NE, D, 2 * DE])
    b1T = din("b1T", [DEPTH, NE, 128, 16])
    w2 = din("w2", [DEPTH, NE, DE, D])
    b2 = din("b2", [DEPTH, NE, D])
    final_g = din("final_g", [1, D])
    cst_in = din("cst", [128, C_N])
    rot_in = din("rot", [128, 2, NS])
    dftc_in = din("dftc", [128, 2, 512], BF16)
    dfts_in = din("dfts", [32, 128, 2, 32, 128], BF16)
    dftp_in = din("dftp", [2, 128, 2, 2, 128], BF16)

    y_out = nc.dram_tensor("y_out", [NTOK, D], F32, kind="ExternalOutput").ap()
    ns_out = nc.dram_tensor("ns_out", [4, DEPTH, 2, NH, DK, DV], F32, kind="ExternalOutput").ap()

    Xs = dscr("Xs", [NTOK, D], F32, True)
    QT = dscr("QT", [1024, NTOK], BF16, True)
    KT = dscr("KT", [1024, NTOK], BF16, True)
    Vd = dscr("Vd", [NTOK, 2048], BF16, True)
    GR = dscr("GR", [NTOK, 2048], BF16, True)
    XFT = dscr("XFT", [1024, NTOK], BF16, True)
    GAT = dscr("GAT", [1024, NTOK], BF16, True)
    GBT = dscr("GBT", [1024, NTOK], BF16, True)
    OGT = dscr("OGT", [2048, NTOK], BF16, True)
    UFT = dscr("UFT", [1024, NTOK], BF16, True)
    H2 = dscr("H2", [NTOK, D], BF16, True)
    XP = dscr("XP", [PT, D], BF16)
    Yd = dscr("Yd", [PT, D], F32)
    LG = dscr("LGd", [128, NT * 32], F32, True)
    SLd = dscr("SLd", [128, NT * 4], I32, True)

    ARENA = 48640
    arena = stack.enter_context(nc.sbuf_tensor("arena", [128, ARENA], F32))
    psum = stack.enter_context(nc.psum_tensor("ps", [128, 8, 512], F32))

    class Alloc:
        def __init__(self, base=0):
            self.off = base

        def get(self, shape, dt):
            n = int(np.prod(shape[1:]))
            esz = 2 if dt == BF16 else 4
            words = (n * esz + 3) // 4
            words = (words + 7) // 8 * 8
            a = arena[:, self.off:self.off + words]
            self.off += words
            assert self.off <= ARENA, ("SBUF overflow", self.off)
            if dt != F32:
                a = a.bitcast(dt)
            a = a[:, 0:n]
            if len(shape) == 3:
                a = a.rearrange("p (a b) -> p a b", a=shape[1])
            elif len(shape) == 4:
                a = a.rearrange("p (a b c) -> p a b c", a=shape[1], b=shape[2])
            return a

    def ps_f32(b, n=512):
        return psum[:, b, 0:n]

    def ps_bf(b):
        return psum[:, b, :].bitcast(BF16)

    P = Prog(nc, stack)
    REG = {}

    def _pool_init():
        REG["xp"] = nc.gpsimd.to_reg(PT - 1)
        REG["w"] = nc.gpsimd.to_reg(DEPTH * NE * 1024 - 1)
        REG["b1"] = nc.gpsimd.to_reg(DEPTH * NE * 128 - 1)
        REG["b2"] = nc.gpsimd.to_reg(DEPTH * NE - 1)
    P.pool_init = _pool_init
    V, A, G, T, SP = nc.vector, nc.scalar, nc.gpsimd, nc.tensor, nc.sync

    pa = Alloc(0)
    cst = pa.get([128, C_N], F32)
    identb = pa.get([128, 128], BF16)
    scT = pa.get([128, 8, 2], F32)
    modT = pa.get([128, 48, 2], F32)
    A1 = pa.get([128, 2, 8], F32)
    epsT = pa.get([128, 2], F32)
    PBASE = pa.off

    ident = cst[:, C_ID:C_ID + 128]
    ones = cst[:, C_ONES:C_ONES + 128]

    P.dma("sp", lambda: SP.dma_start(out=cst, in_=cst_in), writes=["cst"])
    P.dma("sp", lambda: SP.dma_start(out=scT, in_=cT_in), writes=["scT"])
    P.op("act", lambda: A.activation(out=scT, in_=scT, func=AF.Silu), reads=["scT"], writes=["scT"])
    P.op("dve", lambda: V.tensor_copy(out=identb, in_=ident), reads=["cst"], writes=["identb"])
    P.op("dve", lambda: V.memset(epsT[:, 0:1], EPS), writes=["eps0"])
    P.op("dve", lambda: V.memset(epsT[:, 1:2], GN_EPS), writes=["eps1"])
    P.barrier()
    if debug or stop_after:
        dbgo = nc.dram_tensor("dbgo", [128, 4096], F32, kind="ExternalOutput").ap()
    if stop_after == "init":
        P.dma("sp", lambda: SP.dma_start(out=dbgo[:, 0:C_N], in_=cst), writes=["dbgo"])
        P.dma("sp", lambda: SP.dma_start(out=dbgo[:, 2048:2064], in_=scT.rearrange("p a b -> p (a b)")), writes=["dbgo2"])
        P.barrier()
        P.emit()
        return nc, stack

    seqs = [(0, NS, 32, True, 0, None)] + [(NS + 256 * s, 256, 2, False, 1, s) for s in range(4)]

    def bcast_row(dst, vecT, tmpd, psb):
        for kc in range(8):
            P.op("dve", lambda kc=kc: V.tensor_scalar(out=tmpd, in0=ident, scalar1=vecT[:, kc:kc + 1],
                                                      scalar2=None, op0=ALU.mult),
                 reads=["vecsrc"], writes=["bc_tmp"])
            P.op("pe", lambda kc=kc: T.matmul(ps_f32(psb, 128), lhsT=ones, rhs=tmpd, start=True, stop=True),
                 reads=["bc_tmp"], writes=[("ps", psb)])
            P.op("act", lambda kc=kc: A.copy(out=dst[:, kc * 128:(kc + 1) * 128], in_=ps_f32(psb, 128)),
                 reads=[("ps", psb)], writes=["bc_dst"])

    def do_layer(l):
        Xsrc = x_in if l == 0 else Xs
        al = Alloc(PBASE)
        hT = al.get([128, 8, NTOK], BF16)
        xt = [al.get([128, D], F32) for _ in range(2)]
        xn = [al.get([128, D], F32) for _ in range(2)]
        junk = al.get([128, D], F32)
        ss = al.get([128, 4], F32)
        P1END = al.off
        wa = [al.get([128, 8, 512], F32) for _ in range(2)]
        bada = al.get([128, 48], F32)
        g1t = al.get([128, 8], F32)
        P.dma("sp", lambda: SP.dma_start(out=bada, in_=b_adaT[l]), writes=["bada"])
        P.dma("sp", lambda: SP.dma_start(out=g1t, in_=g1T[l]), writes=["g1t"])
        for nb in range(12):
            wb = wa[nb % 2]
            P.dma("sp", lambda nb=nb, wb=wb: SP.dma_start(
                out=wb, in_=w_ada[l].rearrange("(kc p) c -> p kc c", p=128)[:, :, nb * 512:(nb + 1) * 512]),
                writes=[("wa", nb % 2)])
            for sub in range(4):
                ch = nb * 4 + sub
                for kc in range(8):
                    P.op("pe", lambda kc=kc, sub=sub, ch=ch, wb=wb: T.matmul(
                        psum[:, 0, ch * 2:ch * 2 + 2], lhsT=wb[:, kc, sub * 128:(sub + 1) * 128],
                        rhs=scT[:, kc, :], start=(kc == 0), stop=(kc == 7)),
                        reads=[("wa", nb % 2)], writes=[("ps", 0)])
        P.op("dve", lambda: V.tensor_tensor(
            out=modT, in0=psum[:, 0, 0:96].rearrange("p (c j) -> p c j", j=2),
            in1=bada.unsqueeze(2).to_broadcast([128, 48, 2]), op=ALU.add),
            reads=[("ps", 0), "bada"], writes=["modT"])
        for j in range(2):
            P.op("dve", lambda j=j: V.scalar_tensor_tensor(
                out=A1[:, j, :], in0=modT[:, 8:16, j], scalar=1.0, in1=g1t, op0=ALU.add, op1=ALU.mult),
                reads=["modT", "g1t"], writes=["A1"])

        if stop_after == "p0":
            P.dma("sp", lambda: SP.dma_start(out=dbgo[:, 0:96], in_=modT.rearrange("p a b -> p (a b)")), reads=["modT"], writes=["dbgo"])
            P.dma("sp", lambda: SP.dma_start(out=dbgo[:, 128:144], in_=A1.rearrange("p a b -> p (a b)")), reads=["A1"], writes=["dbgo2"])
            P.barrier()
            return True
        for tt in range(NT):
            j = 0 if tt < 32 else 1
            xb, xnb = xt[tt % 2], xn[tt % 2]
            P.dma("sp", lambda tt=tt, xb=xb: SP.dma_start(out=xb, in_=Xsrc[tt * 128:(tt + 1) * 128, :]),
                  writes=[("xt", tt % 2)])
            P.op("act", lambda xb=xb: A.activation(out=junk, in_=xb, func=AF.Square, accum_out=ss[:, 0:1]),
                 reads=[("xt", tt % 2)], writes=["junk", "ss"])
            P.op("act", lambda: A.activation(out=ss[:, 1:2], in_=ss[:, 0:1], func=AF.Sqrt, scale=1.0 / D, bias=epsT[:, 0:1]),
                 reads=["ss"], writes=["ss1"])
            P.op("dve", lambda: V.reciprocal(out=ss[:, 2:3], in_=ss[:, 1:2]), reads=["ss1"], writes=["ss2"])
            P.op("dve", lambda xb=xb, xnb=xnb: V.tensor_scalar(out=xnb, in0=xb, scalar1=ss[:, 2:3], scalar2=None,
                                                               op0=ALU.mult),
                 reads=[("xt", tt % 2), "ss2"], writes=[("xn", tt % 2)])
            for half in range(2):
                bnk = 1 + half + 2 * (tt % 2)
                for q in range(4):
                    kc = half * 4 + q
                    P.op("pe", lambda kc=kc, q=q, bnk=bnk, xnb=xnb: T.matmul(
                        psum[:, bnk, q * 128:(q + 1) * 128], lhsT=xnb[:, kc * 128:(kc + 1) * 128], rhs=ident,
                        start=True, stop=True),
                        reads=[("xn", tt % 2)], writes=[("ps", bnk)])
                for q in range(4):
                    kc = half * 4 + q
                    P.op("act", lambda kc=kc, q=q, bnk=bnk, tt=tt, j=j: A.activation(
                        out=hT[:, kc, tt * 128:(tt + 1) * 128], in_=psum[:, bnk, q * 128:(q + 1) * 128],
                        func=AF.Identity, scale=A1[:, j, kc:kc + 1], bias=modT[:, kc, j:j + 1]),
                        reads=[("ps", bnk), "A1", "modT"], writes=[("hT", tt)])
        if stop_after == "p1":
            P.dma("sp", lambda: SP.dma_start(out=dbgo.bitcast(BF16)[:, 0:8 * 128].rearrange("p (k t) -> p k t", k=8), in_=hT[:, :, 0:128]), reads=[("hT", 0)], writes=["dbgo"])
            P.dma("sp", lambda: SP.dma_start(out=dbgo.bitcast(BF16)[:, 4096:4096 + 8 * 128].rearrange("p (k t) -> p k t", k=8), in_=hT[:, :, 4992:5120]), reads=[("hT", 39)], writes=["dbgo1"])
        P.barrier()
        if stop_after == "p1":
            return True

        al = Alloc(P1END)
        wbf = [al.get([128, 8, 512], BF16) for _ in range(2)]
        wrot = al.get([128, 8, 512], BF16)
        rot = al.get([128, 2, NS], F32)
        osb = [al.get([128, 4, 512], BF16) for _ in range(2)]
        t1 = [al.get([128, 512], F32) for _ in range(2)]
        t2 = [al.get([128, 512], F32) for _ in range(2)]
        P.dma("sp", lambda: SP.dma_start(out=rot, in_=rot_in), writes=["rot"])
        oi = 0
        for cb in range(18):
            wb = wbf[cb % 2]
            wtok = ("wbf", cb % 2)
            P.dma("pool", lambda cb=cb, wb=wb: G.dma_start(
                out=wb, in_=w_in[l].rearrange("(kc p) c -> p kc c", p=128)[:, :, cb * 512:(cb + 1) * 512]),
                writes=[wtok])
            kind = ("q", "q", "k", "k", "v", "v", "v", "v", "g", "g", "g", "g", "x", "x", "a", "a", "b", "b")[cb]
            if kind in ("q", "k"):
                wv = wb.rearrange("p k (h two f) -> p k h two f", two=2, f=64)
                rv = wrot.rearrange("p k (h two f) -> p k h two f", two=2, f=64)
                P.op("act", lambda wv=wv, rv=rv: A.mul(out=rv[:, :, :, 0, :], in_=wv[:, :, :, 1, :], mul=-1.0),
                     reads=[wtok], writes=["wrot"])
                P.op("dve", lambda wv=wv, rv=rv: V.tensor_copy(out=rv[:, :, :, 1, :], in_=wv[:, :, :, 0, :]),
                     reads=[wtok], writes=["wrot2"])
            for tg in range(10):
                ob = osb[oi % 2]
                otok = ("osb", oi % 2)
                oi += 1
                tsl = slice(tg * 512, (tg + 1) * 512)
                for sub in range(4):
                    bA = (sub % 2) * 2
                    bB = bA + 1
                    if kind in ("v", "g"):
                        tk = tg * 512 + sub * 128
                        for kc in range(8):
                            P.op("pe", lambda kc=kc, bA=bA, tk=tk, wb=wb: T.matmul(
                                ps_f32(bA), lhsT=hT[:, kc, tk:tk + 128], rhs=wb[:, kc, :],
                                start=(kc == 0), stop=(kc == 7)), reads=[wtok], writes=[("ps", bA)])
                        fn = AF.Copy if kind == "v" else AF.Silu
                        P.op("act", lambda bA=bA, ob=ob, sub=sub, fn=fn: A.activation(
                            out=ob[:, sub, :], in_=ps_f32(bA), func=fn),
                            reads=[("ps", bA)], writes=[otok])
                    else:
                        csl = slice(sub * 128, (sub + 1) * 128)
                        for kc in range(8):
                            P.op("pe", lambda kc=kc, bA=bA, wb=wb, csl=csl, tsl=tsl: T.matmul(
                                ps_f32(bA), lhsT=wb[:, kc, csl], rhs=hT[:, kc, tsl],
                                start=(kc == 0), stop=(kc == 7)), reads=[wtok], writes=[("ps", bA)])
                        sc = (DK ** -0.5) if kind == "q" else 1.0
                        if kind in ("q", "k") and tg < 8:
                            for kc in range(8):
                                P.op("pe", lambda kc=kc, bB=bB, csl=csl, tsl=tsl: T.matmul(
                                    ps_f32(bB), lhsT=wrot[:, kc, csl], rhs=hT[:, kc, tsl],
                                    start=(kc == 0), stop=(kc == 7)), reads=["wrot", "wrot2"], writes=[("ps", bB)])
                            ta, tb = t1[sub % 2], t2[sub % 2]
                            P.op("dve", lambda bA=bA, ta=ta, tsl=tsl, sc=sc: V.scalar_tensor_tensor(
                                out=ta, in0=ps_f32(bA), scalar=sc, in1=rot[:, 0, tsl], op0=ALU.mult, op1=ALU.mult),
                                reads=[("ps", bA), "rot"], writes=[("t1", sub % 2)])
                            P.op("dve", lambda bB=bB, tb=tb, tsl=tsl, sc=sc: V.scalar_tensor_tensor(
                                out=tb, in0=ps_f32(bB), scalar=sc, in1=rot[:, 1, tsl], op0=ALU.mult, op1=ALU.mult),
                                reads=[("ps", bB), "rot"], writes=[("t2", sub % 2)])
                            P.op("pool", lambda ta=ta, tb=tb, ob=ob, sub=sub: G.tensor_tensor(
                                out=ob[:, sub, :], in0=ta, in1=tb, op=ALU.add),
                                reads=[("t1", sub % 2), ("t2", sub % 2)], writes=[otok])
                        else:
                            if kind in ("a", "b"):
                                P.op("act", lambda bA=bA, ob=ob, sub=sub: A.activation(
                                    out=ob[:, sub, :], in_=ps_f32(bA), func=AF.Sigmoid),
                                    reads=[("ps", bA)], writes=[otok])
                            else:
                                P.op("act", lambda bA=bA, ob=ob, sub=sub, sc=sc: A.activation(
                                    out=ob[:, sub, :], in_=ps_f32(bA), func=AF.Copy, scale=sc),
                                    reads=[("ps", bA)], writes=[otok])
                if kind in ("v", "g"):
                    dst = (Vd if kind == "v" else GR)
                    c0 = (cb - (4 if kind == "v" else 8)) * 512
                    P.dma("sp", lambda dst=dst, c0=c0, tg=tg, ob=ob: SP.dma_start(
                        out=dst[tg * 512:(tg + 1) * 512, c0:c0 + 512].rearrange("(s p) c -> p s c", p=128), in_=ob),
                        reads=[otok], writes=[("u", kind)])
                else:
                    dst, cb0 = {"q": (QT, 0), "k": (KT, 2), "x": (XFT, 12), "a": (GAT, 14), "b": (GBT, 16)}[kind]
                    r0 = (cb - cb0) * 512
                    P.dma("sp", lambda dst=dst, r0=r0, tsl=tsl, ob=ob: SP.dma_start(
                        out=dst[r0:r0 + 512, tsl].rearrange("(s p) t -> p s t", p=128), in_=ob),
                        reads=[otok], writes=[("u", kind)])
        P.barrier()
        if stop_after == "p2a":
            return True

        al = Alloc(PBASE)
        dl = al.get([128, 16], F32)
        MC = al.get([128, 8, 128], F32)
        QDF = al.get([128, 8, 128], F32)
        QDB = al.get([128, 8, 128], F32)
        KD = al.get([128, 2, 8], F32)
        CDEC = al.get([128, 16], F32)
        mtmp = al.get([128, 2, 128], F32)
        QTh = al.get([128, NS], BF16)
        KTh = al.get([128, NS], BF16)
        Vh = al.get([128, 32, 256], BF16)
        GRh = al.get([128, 32, 256], BF16)
        Kf = al.get([128, 32, 128], BF16)
        Kb = al.get([128, 32, 128], BF16)
        QfT = al.get([128, 32, 128], BF16)
        QbT = al.get([128, 32, 128], BF16)
        SbAll = al.get([128, 33, 256], BF16)
        OGh = al.get([128, 2, NS], BF16)
        Sf32 = al.get([128, 256], F32)
        Sb32 = al.get([128, 256], F32)
        Sfbf = [al.get([128, 256], BF16) for _ in range(2)]
        scm = [al.get([128, 128], BF16) for _ in range(2)]
        onb = [al.get([128, 256], F32) for _ in range(2)]
        ogb = [al.get([128, 256], BF16) for _ in range(2)]
        st6 = al.get([128, 16], F32)
        mv = al.get([128, 8], F32)

        P.dma("sp", lambda: SP.dma_start(out=dl, in_=decay[l:l + 1, :].to_broadcast([128, 16])), writes=["dl"])
        P.op("act", lambda: A.activation(out=dl, in_=dl, func=AF.Exp, scale=-1.0), reads=["dl"], writes=["dl"])
        P.op("act", lambda: A.activation(out=dl, in_=dl, func=AF.Ln, bias=1.0), reads=["dl"], writes=["dl"])
        P.op("dve", lambda: V.tensor_scalar(out=dl, in0=dl, scalar1=-1.0, scalar2=None, op0=ALU.mult),
             reads=["dl"], writes=["dl"])
        P.op("act", lambda: A.activation(out=CDEC, in_=dl, func=AF.Exp, scale=128.0), reads=["dl"], writes=["CDEC"])
        for h in range(NH):
            lf = dl[:, h:h + 1]
            lb = dl[:, 8 + h:9 + h]
            P.op("act", lambda lf=lf: A.activation(out=mtmp[:, 0, :], in_=cst[:, C_DF:C_DF + 128], func=AF.Exp, scale=lf),
                 reads=["dl"], writes=["mtmp0"])
            P.op("act", lambda lb=lb: A.activation(out=mtmp[:, 1, :], in_=cst[:, C_DB:C_DB + 128], func=AF.Exp, scale=lb),
                 reads=["dl"], writes=["mtmp1"])
            P.op("dve", lambda: V.tensor_tensor(out=mtmp[:, 0, :], in0=mtmp[:, 0, :], in1=cst[:, C_TF:C_TF + 128], op=ALU.mult),
                 reads=["mtmp0"], writes=["mtmp0"])
            P.op("dve", lambda: V.tensor_tensor(out=mtmp[:, 1, :], in0=mtmp[:, 1, :], in1=cst[:, C_TB:C_TB + 128], op=ALU.mult),
                 reads=["mtmp1"], writes=["mtmp1"])
            P.op("dve", lambda h=h: V.tensor_tensor(out=MC[:, h, :], in0=mtmp[:, 0, :], in1=mtmp[:, 1, :], op=ALU.add),
                 reads=["mtmp0", "mtmp1"], writes=["MC"])
            P.op("act", lambda h=h, lf=lf: A.activation(out=QDF[:, h, :], in_=cst[:, C_IP1:C_IP1 + 128], func=AF.Exp, scale=lf),
                 reads=["dl"], writes=["QD"])
            P.op("act", lambda h=h, lb=lb: A.activation(out=QDB[:, h, :], in_=cst[:, C_IB:C_IB + 128], func=AF.Exp, scale=lb),
                 reads=["dl"], writes=["QD"])
            P.op("act", lambda h=h, lf=lf: A.activation(out=KD[:, 0, h:h + 1], in_=cst[:, C_KF:C_KF + 1], func=AF.Exp, scale=lf),
                 reads=["dl"], writes=["KD"])
            P.op("act", lambda h=h, lb=lb: A.activation(out=KD[:, 1, h:h + 1], in_=cst[:, C_KB:C_KB + 1], func=AF.Exp, scale=lb),
                 reads=["dl"], writes=["KD"])

        for (r0, N, nch, latent, jt, pidx) in seqs:
            for h in range(NH):
                P.dma("sp", lambda h=h, r0=r0, N=N: SP.dma_start(out=QTh[:, 0:N], in_=QT[h * 128:(h + 1) * 128, r0:r0 + N]),
                      writes=["QTh"])
                P.dma("sp", lambda h=h, r0=r0, N=N: SP.dma_start(out=KTh[:, 0:N], in_=KT[h * 128:(h + 1) * 128, r0:r0 + N]),
                      writes=["KTh"])
                P.dma("sp", lambda h=h, r0=r0, N=N, nch=nch: SP.dma_start(
                    out=Vh[:, 0:nch, :], in_=Vd[r0:r0 + N, h * 256:(h + 1) * 256].rearrange("(c p) v -> p c v", p=128)),
                    writes=["Vh"])
                P.dma("sp", lambda h=h, r0=r0, N=N, nch=nch: SP.dma_start(
                    out=GRh[:, 0:nch, :], in_=GR[r0:r0 + N, h * 256:(h + 1) * 256].rearrange("(c p) v -> p c v", p=128)),
                    writes=["GRh"])
                if latent:
                    P.dma("sp", lambda h=h: SP.dma_start(out=Sf32, in_=s0_in[l, 0, h]), writes=["Sf32"])
                    P.dma("sp", lambda h=h: SP.dma_start(out=Sb32, in_=s0_in[l, 1, h]), writes=["Sb32"])
                else:
                    P.op("pool", lambda: G.memset(Sf32, 0.0), writes=["Sf32"])
                    P.op("pool", lambda: G.memset(Sb32, 0.0), writes=["Sb32"])
                for c0 in range(0, nch, 8):
                    n8 = min(8, nch - c0)
                    bnk = 4 + (c0 // 8) % 2
                    for cc in range(n8):
                        c = c0 + cc
                        P.op("pe", lambda c=c, cc=cc, bnk=bnk: T.transpose(
                            ps_bf(bnk)[:, cc * 128:(cc + 1) * 128], KTh[:, c * 128:(c + 1) * 128], identb),
                            reads=["KTh"], writes=[("ps", bnk)])
                    P.op("dve", lambda c0=c0, n8=n8, bnk=bnk, h=h: V.tensor_scalar(
                        out=Kf[:, c0:c0 + n8, :], in0=ps_bf(bnk)[:, 0:n8 * 128].rearrange("p (c d) -> p c d", d=128),
                        scalar1=KD[:, 0, h:h + 1], scalar2=None, op0=ALU.mult),
                        reads=[("ps", bnk), "KD"], writes=["Kf"])
                    P.op("act", lambda c0=c0, n8=n8, bnk=bnk, h=h: A.activation(
                        out=Kb[:, c0:c0 + n8, :], in_=ps_bf(bnk)[:, 0:n8 * 128].rearrange("p (c d) -> p c d", d=128),
                        func=AF.Copy, scale=KD[:, 1, h:h + 1]),
                        reads=[("ps", bnk), "KD", "Kf"], writes=["Kb"])
                P.op("dve", lambda h=h, nch=nch, N=N: V.tensor_tensor(
                    out=QfT[:, 0:nch, :], in0=QTh[:, 0:N].rearrange("p (c i) -> p c i", i=128),
                    in1=QDF[:, h:h + 1, :].to_broadcast([128, nch, 128]), op=ALU.mult),
                    reads=["QTh", "QD"], writes=["QfT"])
                P.op("pool", lambda h=h, nch=nch, N=N: G.tensor_tensor(
                    out=QbT[:, 0:nch, :], in0=QTh[:, 0:N].rearrange("p (c i) -> p c i", i=128),
                    in1=QDB[:, h:h + 1, :].to_broadcast([128, nch, 128]), op=ALU.mult),
                    reads=["QTh", "QD"], writes=["QbT"])
                P.op("act", lambda nch=nch: A.copy(out=SbAll[:, nch, :], in_=Sb32), reads=["Sb32"], writes=[("SbAll", nch)])
                for c in range(nch - 1, -1, -1):
                    bnk = 6 + (c % 2)
                    P.op("pe", lambda c=c, bnk=bnk: T.matmul(ps_f32(bnk, 256), lhsT=Kb[:, c, :], rhs=Vh[:, c, :],
                                                             start=True, stop=True),
                         reads=["Kb", "Vh"], writes=[("ps", bnk)])
                    P.op("dve", lambda bnk=bnk, h=h: V.scalar_tensor_tensor(
                        out=Sb32, in0=Sb32, scalar=CDEC[:, 8 + h:9 + h], in1=ps_f32(bnk, 256), op0=ALU.mult, op1=ALU.add),
                        reads=[("ps", bnk), "Sb32", "CDEC"], writes=["Sb32"])
                    P.op("act", lambda c=c: A.copy(out=SbAll[:, c, :], in_=Sb32), reads=["Sb32"], writes=[("SbAll", c)])
                if not latent:
                    P.dma("sp", lambda h=h, pidx=pidx: SP.dma_start(out=ns_out[pidx, l, 1, h], in_=Sb32),
                          reads=["Sb32"], writes=[("ns", pidx, 1, h)])
                P.op("act", lambda: A.copy(out=Sfbf[0], in_=Sf32), reads=["Sf32"], writes=[("Sfbf", 0)])
                def stage_a(c):
                    csl = slice(c * 128, (c + 1) * 128)
                    b_sc = c % 2
                    b_o = 2 + c % 2
                    sm = scm[c % 2]
                    b_s = 6 + c % 2
                    P.op("pe", lambda c=c, b_s=b_s: T.matmul(ps_f32(b_s, 256), lhsT=Kf[:, c, :], rhs=Vh[:, c, :],
                                                             start=True, stop=True),
                         reads=["Kf", "Vh"], writes=[("ps", b_s)])
                    P.op("dve", lambda b_s=b_s, h=h: V.scalar_tensor_tensor(
                        out=Sf32, in0=Sf32, scalar=CDEC[:, h:h + 1], in1=ps_f32(b_s, 256), op0=ALU.mult, op1=ALU.add),
                        reads=[("ps", b_s), "Sf32", "CDEC"], writes=["Sf32"])
                    P.op("act", lambda c=c: A.copy(out=Sfbf[(c + 1) % 2], in_=Sf32),
                         reads=["Sf32"], writes=[("Sfbf", (c + 1) % 2)])
                    P.op("pe", lambda csl=csl, b_sc=b_sc: T.matmul(ps_f32(b_sc, 128), lhsT=KTh[:, csl], rhs=QTh[:, csl],
                                                                 start=True, stop=True),
                         reads=["KTh", "QTh"], writes=[("ps", b_sc)])
                    P.op("dve", lambda b_sc=b_sc, sm=sm, h=h: V.tensor_tensor(out=sm, in0=ps_f32(b_sc, 128), in1=MC[:, h, :],
                                                                              op=ALU.mult),
                         reads=[("ps", b_sc), "MC"], writes=[("scm", c % 2)])
                    P.op("pe", lambda c=c, b_o=b_o, sm=sm: T.matmul(ps_f32(b_o, 256), lhsT=sm, rhs=Vh[:, c, :],
                                                                    start=True, stop=False),
                         reads=[("scm", c % 2), "Vh"], writes=[("ps", b_o)])
                    P.op("pe", lambda c=c, b_o=b_o: T.matmul(ps_f32(b_o, 256), lhsT=QfT[:, c, :], rhs=Sfbf[c % 2],
                                                             start=False, stop=False),
                         reads=["QfT", ("Sfbf", c % 2)], writes=[("ps", b_o)])
                    P.op("pe", lambda c=c, b_o=b_o: T.matmul(ps_f32(b_o, 256), lhsT=QbT[:, c, :], rhs=SbAll[:, c + 1, :],
                                                             start=False, stop=True),
                         reads=["QbT", ("SbAll", c + 1)], writes=[("ps", b_o)])
                    st_, mv_ = st6[:, (c % 2) * 8:(c % 2) * 8 + 6], mv[:, (c % 2) * 4:(c % 2) * 4 + 4]
                    P.op("dve", lambda b_o=b_o, st_=st_: V.bn_stats(out=st_, in_=ps_f32(b_o, 256)),
                         reads=[("ps", b_o)], writes=[("st6", c % 2)])
                    P.op("dve", lambda st_=st_, mv_=mv_: V.bn_aggr(out=mv_[:, 0:2], in_=st_), reads=[("st6", c % 2)],
                         writes=[("mv", c % 2)])
                    P.op("act", lambda mv_=mv_: A.activation(out=mv_[:, 3:4], in_=mv_[:, 1:2], func=AF.Sqrt, bias=epsT[:, 1:2]),
                         reads=[("mv", c % 2)], writes=[("mv3", c % 2)])

                def stage_b(c):
                    csl = slice(c * 128, (c + 1) * 128)
                    b_o = 2 + c % 2
                    onx, ogx = onb[c % 2], ogb[c % 2]
                    mv_ = mv[:, (c % 2) * 4:(c % 2) * 4 + 4]
                    P.op("dve", lambda mv_=mv_: V.reciprocal(out=mv_[:, 2:3], in_=mv_[:, 3:4]), reads=[("mv3", c % 2)],
                         writes=[("mv2", c % 2)])
                    P.op("dve", lambda b_o=b_o, onx=onx, mv_=mv_: V.tensor_scalar(
                        out=onx, in0=ps_f32(b_o, 256), scalar1=mv_[:, 0:1], scalar2=mv_[:, 2:3], op0=ALU.subtract, op1=ALU.mult),
                        reads=[("ps", b_o), ("mv", c % 2), ("mv2", c % 2)], writes=[("on", c % 2)])
                    P.op("pool", lambda c=c, onx=onx, ogx=ogx: G.tensor_tensor(out=ogx, in0=onx, in1=GRh[:, c, :], op=ALU.mult),
                         reads=[("on", c % 2), "GRh"], writes=[("og", c % 2)])
                    b_t = 4 + c % 2
                    for hf in range(2):
                        P.op("pe", lambda hf=hf, b_t=b_t, ogx=ogx: T.transpose(
                            ps_bf(b_t)[:, hf * 128:(hf + 1) * 128], ogx[:, hf * 128:(hf + 1) * 128], identb),
                            reads=[("og", c % 2)], writes=[("ps", b_t)])
                    P.op("act", lambda b_t=b_t, csl=csl: A.copy(
                        out=OGh[:, :, csl], in_=ps_bf(b_t)[:, 0:256].rearrange("p (a t) -> p a t", a=2)),
                        reads=[("ps", b_t)], writes=["OGh"])

                for c in range(nch):
                    stage_a(c)
                    if c >= 1:
                        stage_b(c - 1)
                stage_b(nch - 1)
                if not latent:
                    P.dma("sp", lambda h=h, pidx=pidx: SP.dma_start(out=ns_out[pidx, l, 0, h], in_=Sf32),
                          reads=["Sf32"], writes=[("ns", pidx, 0, h)])
                P.dma("sp", lambda h=h, r0=r0, N=N: SP.dma_start(
                    out=OGT[h * 256:(h + 1) * 256, r0:r0 + N].rearrange("(a p) t -> p a t", p=128), in_=OGh[:, :, 0:N]),
                    reads=["OGh"], writes=["OGT"])
        P.barrier()
        if stop_after == "p2b":
            return True

        al = Alloc(PBASE)
        dftc = al.get([128, 2, 512], BF16)
        dftp = al.get([128, 2, 2, 2, 128], BF16) if False else None
        dftp_t = [al.get([128, 2, 2, 128], BF16) for _ in range(2)]
        XFp = al.get([128, 4, NS], BF16)
        PQ = al.get([128, 2, 32, 512], BF16)
        Dp = [al.get([128, 2, 32, 128], BF16) for _ in range(2)]
        ufb = [al.get([128, 512], BF16) for _ in range(2)]
        P.dma("sp", lambda: SP.dma_start(out=dftc, in_=dftc_in), writes=["dftc"])
        for mt in range(2):
            P.dma("sp", lambda mt=mt: SP.dma_start(out=dftp_t[mt], in_=dftp_in[mt]), writes=["dftp"])
        di = 0
        for (r0, N, nch, latent, jt, pidx) in seqs:
            for pair in range(2):
                P.dma("sp", lambda pair=pair, r0=r0, N=N: SP.dma_start(
                    out=XFp[:, :, 0:N], in_=XFT[pair * 512:(pair + 1) * 512, r0:r0 + N].rearrange("(a p) t -> p a t", p=128)),
                    writes=["XFp"])
                for c in range(nch):
                    for gg in range(2):
                        bnk = (c * 2 + gg) % 2
                        for k2 in range(2):
                            P.op("pe", lambda c=c, gg=gg, k2=k2, bnk=bnk: T.matmul(
                                ps_f32(bnk), lhsT=XFp[:, gg * 2 + k2, c * 128:(c + 1) * 128], rhs=dftc[:, k2, :],
                                start=(k2 == 0), stop=(k2 == 1)), reads=["XFp", "dftc"], writes=[("ps", bnk)])
                        P.op("act", lambda c=c, gg=gg, bnk=bnk: A.copy(
                            out=PQ[:, :, c, gg * 256:(gg + 1) * 256], in_=ps_f32(bnk).rearrange("p (a f) -> p a f", a=2)),
                            reads=[("ps", bnk)], writes=["PQ"])
                for mt in range(nch):
                    if latent:
                        dp = Dp[di % 2]
                        dtok = ("Dp", di % 2)
                        di += 1
                        P.dma("sp", lambda mt=mt, dp=dp: SP.dma_start(out=dp, in_=dfts_in[mt]), writes=[dtok])
                    else:
                        dp = dftp_t[mt]
                        dtok = "dftp"
                    bnk = 2 + mt % 2
                    n_mm = 2 * nch
                    i_mm = 0
                    for cs in range(2):
                        for ncn in range(nch):
                            P.op("pe", lambda cs=cs, ncn=ncn, bnk=bnk, dp=dp, i_mm=i_mm, n_mm=n_mm: T.matmul(
                                ps_f32(bnk), lhsT=dp[:, cs, ncn, :], rhs=PQ[:, cs, ncn, :],
                                start=(i_mm == 0), stop=(i_mm == n_mm - 1)), reads=[dtok, "PQ"], writes=[("ps", bnk)])
                            i_mm += 1
                    ub = ufb[mt % 2]
                    P.op("act", lambda bnk=bnk, ub=ub: A.copy(out=ub, in_=ps_f32(bnk)), reads=[("ps", bnk)],
                         writes=[("ufb", mt % 2)])
                    bt = 4 + mt % 2
                    for q4 in range(4):
                        P.op("pe", lambda q4=q4, bt=bt, ub=ub: T.transpose(
                            ps_bf(bt)[:, q4 * 128:(q4 + 1) * 128], ub[:, q4 * 128:(q4 + 1) * 128], identb),
                            reads=[("ufb", mt % 2)], writes=[("ps", bt)])
                    P.op("dve", lambda mt=mt, bt=bt: V.tensor_copy(
                        out=XFp[:, :, mt * 128:(mt + 1) * 128], in_=ps_bf(bt)[:, 0:512].rearrange("p (a t) -> p a t", a=4)),
                        reads=[("ps", bt)], writes=["XFp"])
                P.dma("sp", lambda pair=pair, r0=r0, N=N: SP.dma_start(
                    out=UFT[pair * 512:(pair + 1) * 512, r0:r0 + N].rearrange("(a p) t -> p a t", p=128), in_=XFp[:, :, 0:N]),
                    reads=["XFp"], writes=["UFT"])
        P.barrier()
        if stop_after == "p2c":
            return True

        al = Alloc(PBASE)
        wro = al.get([128, 16, D], BF16)
        wfo = al.get([128, 8, D], BF16)
        wou = al.get([128, 8, D], BF16)
        wr = al.get([128, 8, NE], F32)
        brt = al.get([128, NE], F32)
        g2t = al.get([128, 8], F32)
        vtmp = al.get([128, 2, 8], F32)
        bct = al.get([128, 128], F32)
        G1 = [al.get([128, D], F32) for _ in range(2)]
        A2 = [al.get([128, D], F32) for _ in range(2)]
        B2 = [al.get([128, D], F32) for _ in range(2)]
        OGt = al.get([128, 16, 512], BF16)
        UFt = al.get([128, 8, 512], BF16)
        GAt = al.get([128, 8, 512], BF16)
        GBt = al.get([128, 8, 512], BF16)
        ta_ = [al.get([128, 512], F32)] * 2
        tb_ = [al.get([128, 512], F32)] * 2
        mT = al.get([128, 8, 512], BF16)
        xt2 = [al.get([128, D], F32) for _ in range(2)]
        yt2 = al.get([128, D], F32)
        h2 = al.get([128, D], F32)
        h2b = [al.get([128, D], BF16)] * 2
        h2T = al.get([128, 8, 128], F32)
        junk2 = yt2
        ss2 = al.get([128, 4], F32)
        Lsb = al.get([128, NT, NE], F32)
        P.dma("pool", lambda: G.dma_start(out=wro, in_=w_ret_o[l].rearrange("(kc p) c -> p kc c", p=128)), writes=["wro"])
        P.dma("pool", lambda: G.dma_start(out=wfo, in_=w_four[l].rearrange("(kc p) c -> p kc c", p=128)), writes=["wfo"])
        P.dma("pool", lambda: G.dma_start(out=wou, in_=w_out[l].rearrange("(kc p) c -> p kc c", p=128)), writes=["wou"])
        P.dma("sp", lambda: SP.dma_start(out=wr, in_=w_router[l].rearrange("(kc p) c -> p kc c", p=128)), writes=["wr"])
        P.dma("sp", lambda: SP.dma_start(out=brt, in_=b_router[l:l + 1, :].to_broadcast([128, NE])), writes=["brt"])
        P.dma("sp", lambda: SP.dma_start(out=g2t, in_=g2T[l]), writes=["g2t"])
        for j in range(2):
            P.op("dve", lambda j=j: V.tensor_copy(out=vtmp[:, 0, :], in_=modT[:, 16:24, j]), reads=["bc_dst"], writes=["vecsrc"])
            bcast_row(G1[j], vtmp[:, 0, :], bct, 7)
            P.op("dve", lambda j=j: V.scalar_tensor_tensor(out=vtmp[:, 0, :], in0=modT[:, 32:40, j], scalar=1.0, in1=g2t,
                                                           op0=ALU.add, op1=ALU.mult), reads=["bc_dst", "g2t"], writes=["vecsrc"])
            bcast_row(A2[j], vtmp[:, 0, :], bct, 7)
            P.op("dve", lambda j=j: V.tensor_copy(out=vtmp[:, 0, :], in_=modT[:, 24:32, j]), reads=["bc_dst"], writes=["vecsrc"])
            bcast_row(B2[j], vtmp[:, 0, :], bct, 7)
        for tg in range(10):
            j = 0 if tg < 8 else 1
            tsl = slice(tg * 512, (tg + 1) * 512)
            P.dma("sp", lambda tsl=tsl: SP.dma_start(out=OGt, in_=OGT[:, tsl].rearrange("(kc p) t -> p kc t", p=128)), writes=["OGt"])
            P.dma("sp", lambda tsl=tsl: SP.dma_start(out=UFt, in_=UFT[:, tsl].rearrange("(kc p) t -> p kc t", p=128)), writes=["UFt"])
            P.dma("sp", lambda tsl=tsl: SP.dma_start(out=GAt, in_=GAT[:, tsl].rearrange("(kc p) t -> p kc t", p=128)), writes=["GAt"])
            P.dma("sp", lambda tsl=tsl: SP.dma_start(out=GBt, in_=GBT[:, tsl].rearrange("(kc p) t -> p kc t", p=128)), writes=["GBt"])
            for dc in range(8):
                dsl = slice(dc * 128, (dc + 1) * 128)
                bA, bB = (dc % 2) * 2, (dc % 2) * 2 + 1
                for kc in range(16):
                    P.op("pe", lambda kc=kc, bA=bA, dsl=dsl: T.matmul(ps_f32(bA), lhsT=wro[:, kc, dsl], rhs=OGt[:, kc, :],
                                                                      start=(kc == 0), stop=(kc == 15)),
                         reads=["wro", "OGt"], writes=[("ps", bA)])
                for kc in range(8):
                    P.op("pe", lambda kc=kc, bB=bB, dsl=dsl: T.matmul(ps_f32(bB), lhsT=wfo[:, kc, dsl], rhs=UFt[:, kc, :],
                                                                      start=(kc == 0), stop=(kc == 7)),
                         reads=["wfo", "UFt"], writes=[("ps", bB)])
                ta, tb = ta_[dc % 2], tb_[dc % 2]
                P.op("dve", lambda bA=bA, ta=ta, dc=dc: V.tensor_tensor(out=ta, in0=ps_f32(bA), in1=GAt[:, dc, :], op=ALU.mult),
                     reads=[("ps", bA), "GAt"], writes=["ta"])
                P.op("dve", lambda bB=bB, tb=tb, dc=dc: V.tensor_tensor(out=tb, in0=ps_f32(bB), in1=GBt[:, dc, :], op=ALU.mult),
                     reads=[("ps", bB), "GBt"], writes=["tb"])
                P.op("pool", lambda ta=ta, tb=tb, dc=dc: G.tensor_tensor(out=mT[:, dc, :], in0=ta, in1=tb, op=ALU.add),
                     reads=["ta", "tb"], writes=["mT"])
            for ts in range(4):
                tt = tg * 4 + ts
                xb = xt2[tt % 2]
                hb = h2b[tt % 2]
                P.dma("sp", lambda tt=tt, xb=xb: SP.dma_start(out=xb, in_=Xsrc[tt * 128:(tt + 1) * 128, :]),
                      reads=[("Xs", tt)], writes=[("xt2", tt % 2)])
                for hf in range(2):
                    for kc in range(8):
                        P.op("pe", lambda kc=kc, hf=hf, ts=ts: T.matmul(
                            ps_f32(4 + hf), lhsT=mT[:, kc, ts * 128:(ts + 1) * 128], rhs=wou[:, kc, hf * 512:(hf + 1) * 512],
                            start=(kc == 0), stop=(kc == 7)), reads=["mT", "wou"], writes=[("ps", 4 + hf)])
                P.op("dve", lambda j=j: V.tensor_tensor(out=yt2.rearrange("p (a f) -> p a f", a=2), in0=psum[:, 4:6, :],
                                                        in1=G1[j].rearrange("p (a f) -> p a f", a=2), op=ALU.mult),
                     reads=[("ps", 4), ("ps", 5), "bc_dst"], writes=["yt2"])
                P.op("pool", lambda xb=xb: G.tensor_tensor(out=xb, in0=xb, in1=yt2, op=ALU.add),
                     reads=["yt2", ("xt2", tt % 2)], writes=[("xt2", tt % 2)])
                P.dma("sp", lambda tt=tt, xb=xb: SP.dma_start(out=Xs[tt * 128:(tt + 1) * 128, :], in_=xb),
                      reads=[("xt2", tt % 2)], writes=[("Xs", tt)])
                P.op("act", lambda xb=xb: A.activation(out=junk2, in_=xb, func=AF.Square, accum_out=ss2[:, 0:1]),
                     reads=[("xt2", tt % 2)], writes=["yt2", "ss2"])
                P.op("act", lambda: A.activation(out=ss2[:, 1:2], in_=ss2[:, 0:1], func=AF.Sqrt, scale=1.0 / D, bias=epsT[:, 0:1]),
                     reads=["ss2"], writes=["ss2b"])
                P.op("dve", lambda: V.reciprocal(out=ss2[:, 2:3], in_=ss2[:, 1:2]), reads=["ss2b"], writes=["ss2c"])
                P.op("dve", lambda xb=xb, j=j: V.scalar_tensor_tensor(out=h2, in0=xb, scalar=ss2[:, 2:3], in1=A2[j],
                                                                      op0=ALU.mult, op1=ALU.mult),
                     reads=[("xt2", tt % 2), "ss2c", "bc_dst"], writes=["h2"])
                P.op("pool", lambda j=j: G.tensor_tensor(out=h2, in0=h2, in1=B2[j], op=ALU.add),
                     reads=["h2", "bc_dst"], writes=["h2"])
                P.op("act", lambda hb=hb: A.copy(out=hb, in_=h2), reads=["h2"], writes=["h2b"])
                P.dma("sp", lambda tt=tt, hb=hb: SP.dma_start(out=H2[tt * 128:(tt + 1) * 128, :], in_=hb),
                      reads=["h2b"], writes=["H2"])
                for kc in range(8):
                    P.op("pe", lambda kc=kc: T.matmul(psum[:, 6 + kc // 4, (kc % 4) * 128:(kc % 4 + 1) * 128],
                                                      lhsT=h2[:, kc * 128:(kc + 1) * 128], rhs=ident, start=True, stop=True),
                         reads=["h2"], writes=[("ps", 6 + kc // 4)])
                P.op("dve", lambda: V.tensor_copy(out=h2T.rearrange("p (a k) t -> p a (k t)", a=2), in_=psum[:, 6:8, :]),
                     reads=[("ps", 6), ("ps", 7)], writes=["h2T"])
                for kc in range(8):
                    P.op("pe", lambda kc=kc: T.matmul(psum[:, 6, 0:NE], lhsT=h2T[:, kc, :], rhs=wr[:, kc, :],
                                                      start=(kc == 0), stop=(kc == 7)),
                         reads=["h2T", "wr"], writes=[("ps", 6)])
                P.op("dve", lambda tt=tt: V.tensor_tensor(out=Lsb[:, tt, :], in0=psum[:, 6, 0:NE], in1=brt, op=ALU.add),
                     reads=[("ps", 6), "brt"], writes=["Lsb"])
        P.dma("sp", lambda: SP.dma_start(out=LG, in_=Lsb.rearrange("p a b -> p (a b)")), reads=["Lsb"], writes=["LG"])
        P.barrier()
        if stop_after == "p2d":
            return True

        al = Alloc(PBASE)
        WK = al.get([128, NT, 4], F32)
        SLI = al.get([128, NT, 4], I32)
        BLKI = al.get([128, NBLK], I32)
        IDXW = al.get([128, NBLK, 8], I32)
        IDXB1 = al.get([128, NBLK], I32)
        IDXB2 = al.get([128, NBLK], I32)
        R3END = al.off
        idxf = al.get([128, NBLK, 8], F32)
        idxg = al.get([128, NBLK], F32)
        Lr = al.get([128, NT, NE], F32)
        v8 = al.get([128, NT, 8], F32)
        i8 = al.get([128, NT, 8], U32)
        i8f = al.get([128, NT, 8], F32)
        e4 = al.get([128, NT, 4], F32)
        s4 = al.get([128, NT], F32)
        mask = al.get([128, NT, NE], F32)
        pos = al.get([128, NT, NE], F32)
        tot = al.get([128, NT, NE], F32)
        carry = al.get([128, NT + 1, NE], F32)
        cntv = al.get([128, 4, NE], F32)
        pend = al.get([128, NE], F32)
        slot = al.get([128, NT, NE], F32)
        oh = al.get([128, NT, NE], F32)
        slk = al.get([128, NT, 4], F32)
        cmpb = al.get([128, NBLK, NE], F32)
        blkf = al.get([128, NBLK], F32)
        P.dma("sp", lambda: SP.dma_start(out=Lr.rearrange("p a b -> p (a b)"), in_=LG), writes=["Lr"])
        for tt in range(NT):
            P.op("dve", lambda tt=tt: V.max(out=v8[:, tt, :], in_=Lr[:, tt, :]), reads=["Lr"], writes=["v8"])
            P.op("dve", lambda tt=tt: V.max_index(out=i8[:, tt, :], in_max=v8[:, tt, :], in_values=Lr[:, tt, :]),
                 reads=["Lr", "v8"], writes=["i8"])
        P.op("dve", lambda: V.tensor_copy(out=i8f, in_=i8), reads=["i8"], writes=["i8f"])
        P.op("dve", lambda: V.tensor_tensor(out=e4, in0=v8[:, :, 0:4], in1=v8[:, :, 0:1].to_broadcast([128, NT, 4]),
                                            op=ALU.subtract), reads=["v8"], writes=["e4"])
        P.op("act", lambda: A.activation(out=e4, in_=e4, func=AF.Exp), reads=["e4"], writes=["e4"])
        P.op("dve", lambda: V.tensor_reduce(out=s4, in_=e4, axis=AX.X, op=ALU.add), reads=["e4"], writes=["s4"])
        P.op("dve", lambda: V.reciprocal(out=s4, in_=s4), reads=["s4"], writes=["s4"])
        P.op("dve", lambda: V.tensor_tensor(out=WK, in0=e4, in1=s4.unsqueeze(2).to_broadcast([128, NT, 4]), op=ALU.mult),
             reads=["e4", "s4"], writes=["WK"])
        P.op("dve", lambda: V.tensor_tensor(out=mask, in0=Lr, in1=v8[:, :, 3:4].to_broadcast([128, NT, NE]), op=ALU.is_ge),
             reads=["Lr", "v8"], writes=["mask"])
        mflat = mask.rearrange("p a b -> p (a b)")
        for (c0, cn, bnk) in ((0, 512, 0), (512, 512, 1), (1024, 256, 2)):
            P.op("pe", lambda c0=c0, cn=cn, bnk=bnk: T.matmul(ps_f32(bnk, cn), lhsT=cst[:, C_LTRI:C_LTRI + 128],
                                                              rhs=mflat[:, c0:c0 + cn], start=True, stop=True),
                 reads=["mask"], writes=[("ps", bnk)])
            P.op("act", lambda c0=c0, cn=cn, bnk=bnk: A.copy(out=pos.rearrange("p a b -> p (a b)")[:, c0:c0 + cn],
                                                             in_=ps_f32(bnk, cn)), reads=[("ps", bnk)], writes=["pos"])
            P.op("pe", lambda c0=c0, cn=cn, bnk=bnk: T.matmul(ps_f32(bnk + 3, cn), lhsT=ones, rhs=mflat[:, c0:c0 + cn],
                                                              start=True, stop=True),
                 reads=["mask"], writes=[("ps", bnk + 3)])
            P.op("act", lambda c0=c0, cn=cn, bnk=bnk: A.copy(out=tot.rearrange("p a b -> p (a b)")[:, c0:c0 + cn],
                                                             in_=ps_f32(bnk + 3, cn)), reads=[("ps", bnk + 3)], writes=["tot"])
        P.op("dve", lambda: V.memset(carry[:, 0, :], 0.0), writes=["carry"])
        for tt in range(NT):
            P.op("dve", lambda tt=tt: V.tensor_tensor(out=carry[:, tt + 1, :], in0=carry[:, tt, :], in1=tot[:, tt, :], op=ALU.add),
                 reads=["tot", "carry"], writes=["carry"])
        cnt_ = carry[:, NT, :]
        cnti = cntv.bitcast(I32)
        P.op("dve", lambda: V.tensor_copy(out=cnti[:, 0, :], in_=cnt_), reads=["carry"], writes=["cv0"])
        P.op("dve", lambda: V.tensor_scalar(out=cnti[:, 1, :], in0=cnti[:, 0, :], scalar1=BLK - 1, scalar2=None, op0=ALU.add),
             reads=["cv0"], writes=["cv1"])
        P.op("dve", lambda: V.tensor_scalar(out=cnti[:, 2, :], in0=cnti[:, 1, :], scalar1=9, scalar2=9,
                                            op0=ALU.arith_shift_right, op1=ALU.logical_shift_left), reads=["cv1"], writes=["cv2"])
        P.op("dve", lambda: V.tensor_copy(out=cntv[:, 3, :], in_=cnti[:, 2, :]), reads=["cv2"], writes=["cv3"])
        P.op("dve", lambda: V.tensor_copy(out=pend[:, 0:1], in_=cntv[:, 3, 0:1]), reads=["cv3"], writes=["pend"])
        for e in range(1, NE):
            P.op("dve", lambda e=e: V.tensor_tensor(out=pend[:, e:e + 1], in0=pend[:, e - 1:e], in1=cntv[:, 3, e:e + 1], op=ALU.add),
                 reads=["pend", "cv3"], writes=["pend"])
        P.op("dve", lambda: V.tensor_tensor(out=cntv[:, 0, :], in0=pend, in1=cntv[:, 3, :], op=ALU.subtract),
             reads=["pend", "cv3", "cv1"], writes=["cv0"])
        P.op("dve", lambda: V.tensor_tensor(out=pos, in0=pos, in1=carry[:, 0:NT, :], op=ALU.add),
             reads=["pos", "carry"], writes=["pos"])
        P.op("dve", lambda: V.tensor_tensor(out=slot, in0=pos, in1=cntv[:, 0:1, :].to_broadcast([128, NT, NE]), op=ALU.add),
             reads=["pos", "cv0"], writes=["slot"])
        for k in range(4):
            P.op("dve", lambda k=k: V.tensor_tensor(
                out=oh, in0=cst[:, C_IOTA:C_IOTA + NE].unsqueeze(1).to_broadcast([128, NT, NE]),
                in1=i8f[:, :, k:k + 1].to_broadcast([128, NT, NE]), op=ALU.is_equal), reads=["i8f", "slk"], writes=["oh"])
            P.op("dve", lambda: V.tensor_tensor(out=oh, in0=oh, in1=slot, op=ALU.mult), reads=["oh", "slot"], writes=["oh"])
            P.op("dve", lambda k=k: V.tensor_reduce(out=slk[:, :, k], in_=oh, axis=AX.X, op=ALU.add),
                 reads=["oh"], writes=["slk"])
        P.op("dve", lambda: V.tensor_copy(out=SLI, in_=slk), reads=["slk"], writes=["SLI"])
        P.op("dve", lambda: V.tensor_tensor(
            out=cmpb, in0=pend.unsqueeze(1).to_broadcast([128, NBLK, NE]),
            in1=cst[:, C_BC:C_BC + NBLK].unsqueeze(2).to_broadcast([128, NBLK, NE]), op=ALU.is_le),
            reads=["pend"], writes=["cmpb"])
        P.op("dve", lambda: V.tensor_reduce(out=blkf, in_=cmpb, axis=AX.X, op=ALU.add), reads=["cmpb"], writes=["blkf"])
        P.op("dve", lambda: V.tensor_scalar(out=blkf, in0=blkf, scalar1=float(NE - 1), scalar2=None, op0=ALU.min),
             reads=["blkf"], writes=["blkf"])
        P.op("dve", lambda: V.tensor_copy(out=BLKI, in_=blkf), reads=["blkf"], writes=["BLKI"])
        P.op("dve", lambda: V.tensor_scalar(out=idxg, in0=blkf, scalar1=1024.0, scalar2=float(l * NE * 1024),
                                            op0=ALU.mult, op1=ALU.add), reads=["blkf"], writes=["idxg"])
        P.op("dve", lambda: V.tensor_tensor(out=idxf, in0=idxg.unsqueeze(2).to_broadcast([128, NBLK, 8]),
                                            in1=cst[:, C_KCP:C_KCP + 8].unsqueeze(1).to_broadcast([128, NBLK, 8]), op=ALU.add),
             reads=["idxg"], writes=["idxf"])
        P.op("dve", lambda: V.tensor_copy(out=IDXW, in_=idxf), reads=["idxf"], writes=["IDXW"])
        P.op("dve", lambda: V.tensor_scalar(out=idxg, in0=blkf, scalar1=128.0, scalar2=float(l * NE * 128),
                                            op0=ALU.mult, op1=ALU.add), reads=["blkf", "idxf"], writes=["idxg"])
        P.op("dve", lambda: V.tensor_tensor(out=idxg, in0=idxg, in1=cst[:, C_KB:C_KB + 1].to_broadcast([128, NBLK]), op=ALU.add),
             reads=["idxg"], writes=["idxg"])
        P.op("dve", lambda: V.tensor_copy(out=IDXB1, in_=idxg), reads=["idxg"], writes=["IDXB1"])
        P.op("dve", lambda: V.tensor_scalar(out=idxg, in0=blkf, scalar1=float(l * NE), scalar2=None, op0=ALU.add),
             reads=["blkf", "IDXB1"], writes=["idxg"])
        P.op("dve", lambda: V.tensor_copy(out=IDXB2, in_=idxg), reads=["idxg"], writes=["IDXB2"])
        P.dma("sp", lambda: SP.dma_start(out=SLd, in_=SLI.rearrange("p a b -> p (a b)")), reads=["SLI"], writes=["SLd"])

        htk = [al.get([128, D], BF16) for _ in range(4)]
        R3C = R3END
        for tt in range(NT):
            hb = htk[tt % 4]
            P.dma("sp", lambda tt=tt, hb=hb: SP.dma_start(out=hb, in_=H2[tt * 128:(tt + 1) * 128, :]), writes=[("htk", tt % 4)])
            for k in range(4):
                P.dma("pool", lambda tt=tt, k=k, hb=hb: G.indirect_dma_start(
                    out=XP, out_offset=bass.IndirectOffsetOnAxis(ap=SLI[:, tt, k:k + 1], axis=0),
                    in_=hb, in_offset=None, bounds_check=REG["xp"], oob_is_err=False),
                    reads=[("htk", tt % 4), "SLI"], writes=["XP"])
        if stop_after in ("p3c", "p3d"):
            di = dbgo.bitcast(I32)
            P.dma("sp", lambda: SP.dma_start(out=dbgo[:, 0:160], in_=WK.rearrange("p a b -> p (a b)")), reads=["WK"], writes=["dbgo"])
            P.dma("sp", lambda: SP.dma_start(out=di[:, 256:256 + NBLK], in_=BLKI), reads=["BLKI"], writes=["dbgo1"])
            P.dma("sp", lambda: SP.dma_start(out=di[:, 512:512 + NBLK * 8], in_=IDXW.rearrange("p a b -> p (a b)")), reads=["IDXW"], writes=["dbgo2"])
            P.dma("sp", lambda: SP.dma_start(out=di[:, 1100:1100 + NBLK], in_=IDXB1), reads=["IDXB1"], writes=["dbgo3"])
            P.dma("sp", lambda: SP.dma_start(out=di[:, 1200:1200 + NBLK], in_=IDXB2), reads=["IDXB2"], writes=["dbgo4"])
            P.dma("sp", lambda: SP.dma_start(out=dbgo[:, 1300:1332], in_=pend), reads=["pend"], writes=["dbgo5"])
            P.dma("sp", lambda: SP.dma_start(out=dbgo[:, 1400:1432], in_=carry[:, NT, :]), reads=["carry"], writes=["dbgo6"])
        P.barrier()
        if stop_after == "p3c":
            return True

        al = Alloc(R3C)
        w1b = [al.get([128, 8, 2 * DE], BF16) for _ in range(2)]
        w2b = [al.get([128, 8, D], BF16) for _ in range(2)]
        b1t = [al.get([128, 16], F32) for _ in range(2)]
        b2t = [al.get([128, D], F32) for _ in range(2)]
        xtok2 = [al.get([128, 4, D], BF16) for _ in range(2)]
        xbT = al.get([128, 8, 512], BF16)
        actT = al.get([128, 8, 512], BF16)
        gA = [al.get([128, 512], F32) for _ in range(3)]
        sA = [al.get([128, 512], F32) for _ in range(3)]
        lA = [al.get([128, 512], F32) for _ in range(3)]
        ytk = al.get([128, 4, D], F32)
        def issue_loads(b):
            wi = b % 2
            for kc in range(8):
                P.dma("pool", lambda b=b, wi=wi, kc=kc: G.indirect_dma_start(
                    out=w1b[wi][:, kc, :], out_offset=None, in_=w1.rearrange("l e r c -> (l e r) c"),
                    in_offset=bass.IndirectOffsetOnAxis(ap=IDXW[:, b, kc:kc + 1], axis=0),
                    bounds_check=REG["w"], oob_is_err=False), writes=[("w1b", wi, kc // 2)])
            for kc in range(8):
                P.dma("pool", lambda b=b, wi=wi, kc=kc: G.indirect_dma_start(
                    out=w2b[wi][:, kc, :], out_offset=None, in_=w2.rearrange("l e r c -> (l e r) c"),
                    in_offset=bass.IndirectOffsetOnAxis(ap=IDXW[:, b, kc:kc + 1], axis=0),
                    bounds_check=REG["w"], oob_is_err=False), writes=[("w2b", wi, kc // 4)])
            P.dma("pool", lambda b=b, wi=wi: G.indirect_dma_start(
                out=b1t[wi], out_offset=None, in_=b1T.rearrange("l e p c -> (l e p) c"),
                in_offset=bass.IndirectOffsetOnAxis(ap=IDXB1[:, b:b + 1], axis=0),
                bounds_check=REG["b1"], oob_is_err=False), writes=[("b1t", wi)])
            P.dma("pool", lambda b=b, wi=wi: G.indirect_dma_start(
                out=b2t[wi], out_offset=None, in_=b2.rearrange("l e c -> (l e) c"),
                in_offset=bass.IndirectOffsetOnAxis(ap=IDXB2[:, b:b + 1], axis=0),
                bounds_check=REG["b2"], oob_is_err=False), writes=[("b2t", wi)])
            P.dma("sp", lambda b=b, wi=wi: SP.dma_start(
                out=xtok2[wi], in_=XP[b * BLK:(b + 1) * BLK, :].rearrange("(s p) d -> p s d", p=128)),
                writes=[("xtok", wi)])

        issue_loads(0)
        for b in range(NBLK):
            wi = b % 2
            xtok = xtok2[wi]
            if b + 1 < NBLK:
                issue_loads(b + 1)
            for s in range(4):
                bnk = s % 2
                for kc in range(8):
                    P.op("pe", lambda s=s, kc=kc, bnk=bnk, xtok=xtok: T.transpose(
                        ps_bf(bnk)[:, kc * 128:(kc + 1) * 128], xtok[:, s, kc * 128:(kc + 1) * 128], identb),
                        reads=[("xtok", wi)], writes=[("ps", bnk)])
                P.op("act", lambda s=s, bnk=bnk: A.copy(out=xbT[:, :, s * 128:(s + 1) * 128],
                                                        in_=ps_bf(bnk).rearrange("p (k t) -> p k t", k=8)),
                     reads=[("ps", bnk)], writes=["xbT"])
            w1r = [("w1b", wi, q) for q in range(4)]
            w2r = [("w2b", wi, q) for q in range(2)]
            for mc in range(8):
                bG, bL = ((2, 3), (4, 5), (0, 1))[mc % 3]
                g_, s_, l_ = gA[mc % 3], sA[mc % 3], lA[mc % 3]
                for kc in range(8):
                    P.op("pe", lambda kc=kc, mc=mc, bG=bG, wi=wi: T.matmul(
                        ps_f32(bG), lhsT=w1b[wi][:, kc, mc * 128:(mc + 1) * 128], rhs=xbT[:, kc, :],
                        start=(kc == 0), stop=(kc == 7)), reads=w1r + ["xbT"], writes=[("ps", bG)])
                for kc in range(8):
                    P.op("pe", lambda kc=kc, mc=mc, bL=bL, wi=wi: T.matmul(
                        ps_f32(bL), lhsT=w1b[wi][:, kc, DE + mc * 128:DE + (mc + 1) * 128], rhs=xbT[:, kc, :],
                        start=(kc == 0), stop=(kc == 7)), reads=w1r + ["xbT"], writes=[("ps", bL)])
                P.op("dve", lambda mc=mc, bG=bG, g_=g_, wi=wi: V.tensor_scalar(
                    out=g_, in0=ps_f32(bG), scalar1=b1t[wi][:, mc:mc + 1], scalar2=7.0, op0=ALU.add, op1=ALU.min),
                    reads=[("ps", bG), ("b1t", wi)], writes=[("gA", mc % 3)])
                P.op("act", lambda g_=g_, s_=s_: A.activation(out=s_, in_=g_, func=AF.Sigmoid, scale=1.702),
                     reads=[("gA", mc % 3)], writes=[("sA", mc % 3)])
                P.op("dve", lambda mc=mc, bL=bL, l_=l_, wi=wi: V.tensor_scalar(
                    out=l_, in0=ps_f32(bL), scalar1=b1t[wi][:, 8 + mc:9 + mc], scalar2=7.0, op0=ALU.add, op1=ALU.min),
                    reads=[("ps", bL), ("b1t", wi)], writes=[("lA", mc % 3)])
                P.op("dve", lambda l_=l_: V.tensor_scalar(out=l_, in0=l_, scalar1=-7.0, scalar2=1.0, op0=ALU.max, op1=ALU.add),
                     reads=[("lA", mc % 3)], writes=[("lA", mc % 3)])
                P.op("dve", lambda g_=g_, s_=s_: V.tensor_tensor(out=s_, in0=g_, in1=s_, op=ALU.mult),
                     reads=[("gA", mc % 3), ("sA", mc % 3)], writes=[("sA", mc % 3)])
                P.op("dve", lambda mc=mc, l_=l_, s_=s_: V.tensor_tensor(out=actT[:, mc, :], in0=l_, in1=s_, op=ALU.mult),
                     reads=[("lA", mc % 3), ("sA", mc % 3)], writes=["actT"])
            for s in range(4):
                b0 = 6
                for hf in range(2):
                    for kc in range(8):
                        P.op("pe", lambda kc=kc, hf=hf, s=s, wi=wi: T.matmul(
                            ps_f32(6 + hf), lhsT=actT[:, kc, s * 128:(s + 1) * 128], rhs=w2b[wi][:, kc, hf * 512:(hf + 1) * 512],
                            start=(kc == 0), stop=(kc == 7)), reads=w2r + ["actT"], writes=[("ps", 6 + hf)])
                P.op("dve", lambda s=s, wi=wi: V.tensor_tensor(out=ytk[:, s, :].rearrange("p (a f) -> p a f", a=2),
                                                               in0=psum[:, 6:8, :],
                                                               in1=b2t[wi].rearrange("p (a f) -> p a f", a=2), op=ALU.add),
                     reads=[("ps", 6), ("ps", 7), ("b2t", wi)], writes=["ytk"])
            P.dma("sp", lambda b=b: SP.dma_start(out=Yd[b * BLK:(b + 1) * BLK, :].rearrange("(s p) d -> p s d", p=128), in_=ytk),
                  reads=["ytk"], writes=["Yd"])
        P.barrier()
        if stop_after == "p3d":
            return True

        al = Alloc(R3END)
        g2row = al.get([128, 2, 8], F32)
        bct2 = al.get([128, 128], F32)
        G2 = [al.get([128, D], F32) for _ in range(2)]
        FG = al.get([128, D], F32)
        yk = [al.get([128, 4, D], F32) for _ in range(2)]
        xt3 = [al.get([128, D], F32) for _ in range(2)]
        acc = al.get([128, D], F32)
        junk3 = al.get([128, D], F32)
        ss3 = al.get([128, 4], F32)
        for j in range(2):
            P.op("dve", lambda j=j: V.tensor_copy(out=g2row[:, 0, :], in_=modT[:, 40:48, j]), reads=["bc_dst"], writes=["vecsrc"])
            bcast_row(G2[j], g2row[:, 0, :], bct2, 7)
        if l == DEPTH - 1:
            P.dma("sp", lambda: SP.dma_start(out=FG, in_=final_g.to_broadcast([128, D])), writes=["FG"])
        for tt in range(NT):
            j = 0 if tt < 32 else 1
            ykb = yk[tt % 2]
            xb = xt3[tt % 2]
            for k in range(4):
                P.dma("pool", lambda tt=tt, k=k, ykb=ykb: G.indirect_dma_start(
                    out=ykb[:, k, :], out_offset=None, in_=Yd,
                    in_offset=bass.IndirectOffsetOnAxis(ap=SLI[:, tt, k:k + 1], axis=0),
                    bounds_check=REG["xp"], oob_is_err=False), writes=[("yk", tt % 2, k)])
            P.dma("sp", lambda tt=tt, xb=xb: SP.dma_start(out=xb, in_=Xs[tt * 128:(tt + 1) * 128, :]),
                  reads=[("Xs", tt)], writes=[("xt3", tt % 2)])
            P.op("dve", lambda tt=tt, ykb=ykb: V.tensor_scalar(out=acc, in0=ykb[:, 0, :], scalar1=WK[:, tt, 0:1], scalar2=None,
                                                               op0=ALU.mult), reads=[("yk", tt % 2, 0)], writes=["acc"])
            for k in range(1, 4):
                P.op("dve", lambda tt=tt, k=k, ykb=ykb: V.scalar_tensor_tensor(
                    out=acc, in0=ykb[:, k, :], scalar=WK[:, tt, k:k + 1], in1=acc, op0=ALU.mult, op1=ALU.add),
                    reads=[("yk", tt % 2, k), "acc"], writes=["acc"])
            P.op("pool", lambda j=j: G.tensor_tensor(out=acc, in0=acc, in1=G2[j], op=ALU.mult), reads=["acc", "bc_dst"], writes=["acc"])
            P.op("dve", lambda xb=xb: V.tensor_tensor(out=xb, in0=xb, in1=acc, op=ALU.add),
                 reads=["acc", ("xt3", tt % 2)], writes=[("xt3", tt % 2)])
            if l < DEPTH - 1:
                P.dma("sp", lambda tt=tt, xb=xb: SP.dma_start(out=Xs[tt * 128:(tt + 1) * 128, :], in_=xb),
                      reads=[("xt3", tt % 2)], writes=[("Xs", tt)])
            else:
                P.op("act", lambda xb=xb: A.activation(out=junk3, in_=xb, func=AF.Square, accum_out=ss3[:, 0:1]),
                     reads=[("xt3", tt % 2)], writes=["junk3", "ss3"])
                P.op("act", lambda: A.activation(out=ss3[:, 1:2], in_=ss3[:, 0:1], func=AF.Sqrt, scale=1.0 / D, bias=epsT[:, 0:1]),
                     reads=["ss3"], writes=["ss3b"])
                P.op("dve", lambda: V.reciprocal(out=ss3[:, 2:3], in_=ss3[:, 1:2]), reads=["ss3b"], writes=["ss3c"])
                P.op("dve", lambda xb=xb: V.scalar_tensor_tensor(out=xb, in0=xb, scalar=ss3[:, 2:3], in1=FG,
                                                                 op0=ALU.mult, op1=ALU.mult),
                     reads=[("xt3", tt % 2), "ss3c", "FG"], writes=[("xt3", tt % 2)])
                P.dma("sp", lambda tt=tt, xb=xb: SP.dma_start(out=y_out[tt * 128:(tt + 1) * 128, :], in_=xb),
                      reads=[("xt3", tt % 2)], writes=[("yo", tt)])
        P.barrier()
        return False

    for l in range(DEPTH):
        if do_layer(l):
            break
    P.barrier()
    P.emit()
    return nc, stack


_CONST_CACHE = {}


def _host_consts():
    if not _CONST_CACHE:
        _CONST_CACHE["cst"] = _consts()
        _CONST_CACHE["rot"] = _rot_tables()
        _CONST_CACHE["dftc"] = _dft_chan()
        _CONST_CACHE["dfts"] = _dft_seq(NS)
        _CONST_CACHE["dftp"] = _dft_seq(256)
    return _CONST_CACHE


def make_in_maps(inp, ncores=8):
    f = lambda a: np.ascontiguousarray(np.asarray(a, dtype=np.float32))
    hc = _host_consts()
    shared = {
        "w_ada": f(inp["w_ada"]),
        "b_adaT": f(np.asarray(inp["b_ada"]).reshape(DEPTH, 48, 128).transpose(0, 2, 1)),
        "g1T": f(np.asarray(inp["norm1_g"]).reshape(DEPTH, 8, 128).transpose(0, 2, 1)),
        "g2T": f(np.asarray(inp["norm2_g"]).reshape(DEPTH, 8, 128).transpose(0, 2, 1)),
        "w_in": f(inp["w_in"]),
        "decay": f(np.asarray(inp["ret_decay_logit"]).reshape(DEPTH, 16)),
        "w_ret_o": f(inp["w_ret_o"]),
        "w_four": f(inp["w_four"]),
        "w_out": f(inp["w_out"]),
        "w_router": f(inp["w_router"]),
        "b_router": f(inp["b_router"]),
        "w1": f(inp["w1"]),
        "b1T": f(np.asarray(inp["b1"]).reshape(DEPTH, NE, 16, 128).transpose(0, 1, 3, 2)),
        "w2": f(inp["w2"]),
        "b2": f(inp["b2"]),
        "final_g": f(np.asarray(inp["final_g"]).reshape(1, D)),
        "cst": hc["cst"], "rot": hc["rot"], "dftc": hc["dftc"], "dfts": hc["dfts"], "dftp": hc["dftp"],
    }
    xp = np.asarray(inp["x_prompt"], np.float32)
    xs = np.asarray(inp["x_sample"], np.float32)
    st = np.asarray(inp["state_ret"], np.float32)
    c = np.asarray(inp["c"], np.float32)
    cc = np.asarray(inp["c_ctx"], np.float32)
    maps = []
    for i in range(ncores):
        m = dict(shared)
        m["x_in"] = np.ascontiguousarray(np.concatenate([xs[i], xp[4 * i:4 * i + 4].reshape(1024, D)], axis=0))
        m["s0"] = np.ascontiguousarray(st[i])
        cT = np.stack([c[i].reshape(8, 128).T, cc.reshape(8, 128).T], axis=-1)
        m["cT"] = np.ascontiguousarray(cT.astype(np.float32))
        maps.append(m)
    return maps


def kernel(**inputs):
    nc, stack = build()
    try:
        maps = make_in_maps(inputs, 8)
        res = run_bass_kernel_spmd(nc, maps, core_ids=list(range(8)))
    finally:
        stack.close()
    y_prompt = np.zeros((32, 256, D), np.float32)
    y_sample = np.zeros((8, NS, D), np.float32)
    new_state = np.zeros((32, DEPTH, 2, NH, DK, DV), np.float32)
    for i, r in enumerate(res.results):
        yo = np.asarray(r["y_out"])
        y_sample[i] = yo[:NS]
        y_prompt[4 * i:4 * i + 4] = yo[NS:].reshape(4, 256, D)
        new_state[4 * i:4 * i + 4] = np.asarray(r["ns_out"])
    return (y_prompt, y_sample, new_state)
```
